# Optimizing a Trainium2 kernel written in Bass

```python
import jax
import jax.numpy as jnp
from jax import lax
import numpy as np


D_MODEL = 1024
BATCH = 4
SEQ = 8192
DEPTH = 2

GRID_W = 64
CTX_LEN = 256

D_A = D_MODEL // 4
CONV_A = 3
H_B = 4
DK_B = D_MODEL // 16
DV_B = D_MODEL // 16
D_B = H_B * DV_B
C_GROUPS = 4
C_GDIM = D_MODEL // 16
D_C = C_GROUPS * C_GDIM
H_D = 4
P_D = D_MODEL // 16
D_D = H_D * P_D
G_D = 2
N_D = D_MODEL // 16
CONV_D = 3
D_XBC = D_D + 2 * G_D * N_D

N_BRANCH = 4
CHUNK = 64
IN_SIZES = (D_A, D_A, D_A, H_B * DK_B, 2 * H_B * DK_B, D_B, D_B, D_C, D_D, D_XBC, 2 * H_D, N_BRANCH * D_MODEL)
N_IN = sum(IN_SIZES)

N_GROUPS = 4
EXPERTS_PER_GROUP = 8
N_EXPERTS = N_GROUPS * EXPERTS_PER_GROUP
TOP_K = 2
D_FF = D_MODEL // 2
MOE_BLOCK = 128

EPS = 1e-6

kernel_name = 'hybrid_prefix_flow_block'


def rms_norm(x, w):
    xf = x.astype(jnp.float32)
    y = xf * lax.rsqrt(jnp.mean(xf * xf, axis=-1, keepdims=True) + EPS)
    return (y * w.astype(jnp.float32)).astype(x.dtype)


def modulate(h, shift, scale):
    return h * (1 + scale) + shift


def depthwise_conv(x, w, b):
    k = w.shape[0]
    y = lax.conv_general_dilated(x, w[:, None, :].astype(x.dtype), window_strides=(1,),
                                 padding=((k // 2, k // 2),), dimension_numbers=('NWC', 'WIO', 'NWC'),
                                 feature_group_count=x.shape[-1])
    return y + b.astype(x.dtype)


def chunk_scan(q, k, v, log_f, s0):
    bsz, nh, t_len, _ = q.shape
    n_chunks = t_len // CHUNK

    def to_chunks(a):
        return jnp.moveaxis(a.reshape(bsz, nh, n_chunks, CHUNK, a.shape[-1]), 2, 0)

    incl = jnp.tril(jnp.ones((CHUNK, CHUNK), dtype=bool))[:, :, None]

    def step(state, inp):
        qc, kc, vc, gc = inp
        cum = jnp.cumsum(gc, axis=-2)
        rel = cum[..., :, None, :] - cum[..., None, :, :]
        decay = jnp.exp(jnp.where(incl, rel, -jnp.inf))
        if gc.shape[-1] == 1:
            scores = jnp.einsum('bhtd,bhsd->bhts', qc, kc) * decay[..., 0]
        else:
            scores = jnp.einsum('bhtd,bhsd,bhtsd->bhts', qc, kc, decay)
        out = (jnp.einsum('bhts,bhse->bhte', scores, vc)
               + jnp.einsum('bhtd,bhde->bhte', qc * jnp.exp(cum), state))
        last = cum[..., -1:, :]
        state = (jnp.exp(last[..., 0, :])[..., None] * state
                 + jnp.einsum('bhsd,bhse->bhde', kc * jnp.exp(last - cum), vc))
        return state, out

    state, outs = lax.scan(step, s0, (to_chunks(q), to_chunks(k), to_chunks(v), to_chunks(log_f)))
    return jnp.moveaxis(outs, 0, 2).reshape(bsz, nh, t_len, v.shape[-1]), state


def to_heads(a, n_heads):
    bsz, t_len, _ = a.shape
    return a.reshape(bsz, t_len, n_heads, -1).transpose(0, 2, 1, 3).astype(jnp.float32)


def flip_t(a):
    return jnp.flip(a, axis=2)


def hgrn_log_forget(f_raw, lb):
    lb = lb.reshape(H_B, 1, DK_B)
    return jnp.logaddexp(jnp.log(lb), jnp.log1p(-lb) + jax.nn.log_sigmoid(f_raw))


def token_mixer(h, lp, lb, init, need_out):
    bsz, t_len, _ = h.shape
    split_at = np.cumsum(IN_SIZES)[:-1].tolist()
    (a_b, a_c, a_x, q_raw, f_raw, i_raw, g_raw, four, z, xbc, dt_raw, gate_raw) = jnp.split(
        h @ lp['w_in'], split_at, axis=-1)
    if init is None:
        zb = jnp.zeros((bsz, H_B, DK_B, DV_B), jnp.float32)
        zd = jnp.zeros((bsz, H_D, N_D, P_D), jnp.float32)
        init = (zb, zb, zd, zd)
    s_hf, s_hb, s_mf, s_mb = init

    y_a = a_b * depthwise_conv(a_c * a_x, lp['conv_a_w'], lp['conv_a_b'])

    q = to_heads(jax.nn.silu(q_raw), H_B)
    v = to_heads(i_raw, H_B)
    ff_raw, fb_raw = jnp.split(f_raw, 2, axis=-1)
    log_ff = hgrn_log_forget(to_heads(ff_raw, H_B), lb[0])
    log_fb = hgrn_log_forget(to_heads(fb_raw, H_B), lb[1])
    o_f, s_hf = chunk_scan(q, -jnp.expm1(log_ff), v, log_ff, s_hf)
    o_b, s_hb = chunk_scan(flip_t(q), flip_t(-jnp.expm1(log_fb)), flip_t(v), flip_t(log_fb), s_hb)
    o = (o_f + flip_t(o_b)).transpose(0, 2, 1, 3)
    y_b = (rms_norm(o, lp['hgrn_norm_w'])
           * jax.nn.silu(g_raw.reshape(bsz, t_len, H_B, DV_B).astype(jnp.float32)))
    y_b = y_b.reshape(bsz, t_len, D_B).astype(h.dtype)

    y_c = jnp.fft.fft2(four.reshape(bsz, t_len, C_GROUPS, C_GDIM).astype(jnp.float32),
                       axes=(1, 3), norm='ortho').real
    y_c = y_c.reshape(bsz, t_len, D_C).astype(h.dtype)

    xbc = jax.nn.silu(depthwise_conv(xbc, lp['ssm_conv_w'], lp['ssm_conv_b']))
    xs, b_in, c_in = jnp.split(xbc, [D_D, D_D + G_D * N_D], axis=-1)
    xh = to_heads(xs, H_D)

    def group_heads(a):
        a = jnp.repeat(a.reshape(bsz, t_len, G_D, N_D), H_D // G_D, axis=2)
        return a.transpose(0, 2, 1, 3).astype(jnp.float32)

    bh, ch = group_heads(b_in), group_heads(c_in)
    dt = jax.nn.softplus(dt_raw.reshape(bsz, t_len, 2, H_D).astype(jnp.float32)
                         + lp['ssm_dt_bias'].astype(jnp.float32)).transpose(2, 0, 3, 1)
    log_a = dt * (-jnp.exp(lp['ssm_A_log'].astype(jnp.float32)))[:, None, :, None]
    y_f, s_mf = chunk_scan(ch, bh * dt[0][..., None], xh, log_a[0][..., None], s_mf)
    y_r, s_mb = chunk_scan(flip_t(ch), flip_t(bh * dt[1][..., None]), flip_t(xh),
                           flip_t(log_a[1][..., None]), s_mb)
    y = y_f + flip_t(y_r) + lp['ssm_D'].astype(jnp.float32)[:, None, None] * xh
    y = (y.transpose(0, 2, 1, 3).reshape(bsz, t_len, G_D, D_D // G_D)
         * jax.nn.silu(z.astype(jnp.float32)).reshape(bsz, t_len, G_D, D_D // G_D))
    y_d = rms_norm(y, lp['ssm_norm_w'].reshape(G_D, D_D // G_D)).reshape(bsz, t_len, D_D).astype(h.dtype)

    states = (s_hf, s_hb, s_mf, s_mb)
    if not need_out:
        return None, states

    gates = jax.nn.sigmoid(gate_raw.reshape(bsz, t_len, N_BRANCH, D_MODEL))
    branches = (y_a, y_b, y_c, y_d)
    merged = gates[:, :, 0] * (branches[0] @ lp['w_branch'][0])
    for kb in range(1, N_BRANCH):
        merged = merged + gates[:, :, kb] * (branches[kb] @ lp['w_branch'][kb])
    return merged @ lp['w_out'], states


def grouped_experts(h, expert_ids, weights, w1, w3, w2):
    n_tok, d = h.shape
    n_assign = expert_ids.shape[0]
    token_ids = jnp.arange(n_assign, dtype=jnp.int32) // (n_assign // n_tok)
    order = jnp.argsort(expert_ids)
    e_sorted = expert_ids[order]
    tok_sorted = token_ids[order]
    counts = jnp.bincount(expert_ids, length=N_EXPERTS)
    starts = jnp.cumsum(counts) - counts
    padded = (counts + MOE_BLOCK - 1) // MOE_BLOCK * MOE_BLOCK
    pad_ends = jnp.cumsum(padded)
    pad_starts = pad_ends - padded
    dest = pad_starts[e_sorted] + (jnp.arange(n_assign, dtype=jnp.int32) - starts[e_sorted])
    n_blocks = -(-n_assign // MOE_BLOCK) + N_EXPERTS
    buf = jnp.zeros((n_blocks * MOE_BLOCK, d), h.dtype).at[dest].set(h[tok_sorted])
    block_start = jnp.arange(n_blocks, dtype=jnp.int32) * MOE_BLOCK
    block_expert = jnp.minimum(jnp.sum(block_start[:, None] >= pad_ends[None, :], axis=-1), N_EXPERTS - 1)

    def expert_block(args):
        xb, e = args
        return (jax.nn.silu(xb @ w1[e]) * (xb @ w3[e])) @ w2[e]

    out = lax.map(expert_block, (buf.reshape(n_blocks, MOE_BLOCK, d), block_expert)).reshape(-1, d)
    y_assign = out[dest].astype(jnp.float32) * weights[order][:, None]
    return jax.ops.segment_sum(y_assign, tok_sorted, num_segments=n_tok).astype(h.dtype)


def hier_moe(h, rg_w, rg_b, re_w, re_b, w1, w3, w2):
    n_tok = h.shape[0]
    hf = h.astype(jnp.float32)
    g_logits = hf @ rg_w.astype(jnp.float32) + rg_b.astype(jnp.float32)
    g_prob = jax.nn.softmax(g_logits, axis=-1)
    g_top = jnp.argmax(g_logits, axis=-1).astype(jnp.int32)
    p_group = jnp.take_along_axis(g_prob, g_top[:, None], axis=-1)
    e_logits = (hf @ re_w.astype(jnp.float32) + re_b.astype(jnp.float32)).reshape(n_tok, N_GROUPS, EXPERTS_PER_GROUP)
    e_in = jnp.take_along_axis(e_logits, g_top[:, None, None], axis=1)[:, 0]
    top_v, top_i = lax.top_k(e_in, TOP_K)
    w = p_group * jax.nn.softmax(top_v, axis=-1)
    expert = g_top[:, None] * EXPERTS_PER_GROUP + top_i.astype(jnp.int32)
    return grouped_experts(h, expert.reshape(-1), w.reshape(-1), w1, w3, w2)


def setup_inputs(seed: int = 0) -> dict:
    key = jax.random.key(seed)
    ks = jax.random.split(key, 32)
    f32 = jnp.float32
    nrm = lambda k, s, sc: jax.random.normal(k, s, f32) * sc
    dt0 = jnp.exp(jax.random.uniform(ks[14], (DEPTH, 2, H_D), f32) * (jnp.log(0.1) - jnp.log(0.001)) + jnp.log(0.001))
    return {
        'x': nrm(ks[0], (BATCH, SEQ, D_MODEL), 1.0),
        'c': nrm(ks[1], (BATCH, D_MODEL), 1.0),
        'ctx': nrm(ks[2], (BATCH, CTX_LEN, D_MODEL), 1.0),
        'c_ctx': nrm(ks[3], (D_MODEL,), 1.0),
        'ada_w': nrm(ks[4], (DEPTH, D_MODEL, 6 * D_MODEL), 0.5 * D_MODEL ** -0.5),
        'ada_b': nrm(ks[5], (DEPTH, 6 * D_MODEL), 0.01),
        'norm_mix_w': 1.0 + nrm(ks[6], (DEPTH, D_MODEL), 0.01),
        'norm_ffn_w': 1.0 + nrm(ks[7], (DEPTH, D_MODEL), 0.01),
        'w_in': nrm(ks[8], (DEPTH, D_MODEL, N_IN), D_MODEL ** -0.5),
        'conv_a_w': nrm(ks[9], (DEPTH, CONV_A, D_A), CONV_A ** -0.5),
        'conv_a_b': nrm(ks[10], (DEPTH, D_A), 0.01),
        'hgrn_lb': nrm(ks[11], (2, DEPTH, H_B * DK_B), 1.0),
        'hgrn_norm_w': 1.0 + nrm(ks[12], (DEPTH, DV_B), 0.01),
        'ssm_conv_w': nrm(ks[13], (DEPTH, CONV_D, D_XBC), CONV_D ** -0.5),
        'ssm_conv_b': nrm(ks[15], (DEPTH, D_XBC), 0.01),
        'ssm_A_log': jnp.log(jax.random.uniform(ks[16], (DEPTH, 2, H_D), f32, 1.0, 16.0)),
        'ssm_dt_bias': dt0 + jnp.log(-jnp.expm1(-dt0)),
        'ssm_D': 1.0 + nrm(ks[17], (DEPTH, H_D), 0.01),
        'ssm_norm_w': 1.0 + nrm(ks[18], (DEPTH, D_D), 0.01),
        'w_branch': nrm(ks[19], (DEPTH, N_BRANCH, D_A, D_MODEL), D_A ** -0.5),
        'w_out': nrm(ks[20], (DEPTH, D_MODEL, D_MODEL), D_MODEL ** -0.5),
        'router_group_w': nrm(ks[21], (DEPTH, D_MODEL, N_GROUPS), D_MODEL ** -0.5),
        'router_group_b': nrm(ks[22], (DEPTH, N_GROUPS), 0.01),
        'router_expert_w': nrm(ks[23], (DEPTH, D_MODEL, N_EXPERTS), D_MODEL ** -0.5),
        'router_expert_b': nrm(ks[24], (DEPTH, N_EXPERTS), 0.01),
        'moe_w1': nrm(ks[25], (DEPTH, N_EXPERTS, D_MODEL, D_FF), D_MODEL ** -0.5),
        'moe_w3': nrm(ks[26], (DEPTH, N_EXPERTS, D_MODEL, D_FF), D_MODEL ** -0.5),
        'moe_w2': nrm(ks[27], (DEPTH, N_EXPERTS, D_FF, D_MODEL), D_FF ** -0.5),
        'final_norm_w': 1.0 + nrm(ks[28], (D_MODEL,), 0.01),
    }


def reference(x, c, ctx, c_ctx, ada_w, ada_b, norm_mix_w, norm_ffn_w, w_in, conv_a_w, conv_a_b,
              hgrn_lb, hgrn_norm_w, ssm_conv_w, ssm_conv_b, ssm_A_log, ssm_dt_bias, ssm_D, ssm_norm_w,
              w_branch, w_out, router_group_w, router_group_b, router_expert_w, router_expert_b,
              moe_w1, moe_w3, moe_w2, final_norm_w):
    lb_all = jnp.cumsum(jax.nn.softmax(hgrn_lb.astype(jnp.float32), axis=1), axis=1)
    lb_all = lb_all - lb_all[:, :1]
    s_c = jax.nn.silu(c)
    s_ctx = jax.nn.silu(c_ctx)
    for l in range(DEPTH):
        last = l == DEPTH - 1
        lp = {'w_in': w_in[l], 'conv_a_w': conv_a_w[l], 'conv_a_b': conv_a_b[l],
              'hgrn_norm_w': hgrn_norm_w[l], 'ssm_conv_w': ssm_conv_w[l], 'ssm_conv_b': ssm_conv_b[l],
              'ssm_A_log': ssm_A_log[l], 'ssm_dt_bias': ssm_dt_bias[l], 'ssm_D': ssm_D[l],
              'ssm_norm_w': ssm_norm_w[l], 'w_branch': w_branch[l], 'w_out': w_out[l]}
        m_l = jnp.split((s_c @ ada_w[l] + ada_b[l])[:, None, :], 6, axis=-1)
        m_c = jnp.split(s_ctx @ ada_w[l] + ada_b[l], 6, axis=-1)

        h_c = modulate(rms_norm(ctx, norm_mix_w[l]), m_c[0], m_c[1])
        mix_c, ctx_states = token_mixer(h_c, lp, lb_all[:, l], None, not last)
        h_l = modulate(rms_norm(x, norm_mix_w[l]), m_l[0], m_l[1])
        mix_l, _ = token_mixer(h_l, lp, lb_all[:, l], ctx_states, True)
        x = x + m_l[2] * mix_l

        moe_args = (router_group_w[l], router_group_b[l], router_expert_w[l], router_expert_b[l],
                    moe_w1[l], moe_w3[l], moe_w2[l])
        f_l = modulate(rms_norm(x, norm_ffn_w[l]), m_l[3], m_l[4]).reshape(-1, D_MODEL)
        if last:
            y_l = hier_moe(f_l, *moe_args)
        else:
            ctx = ctx + m_c[2] * mix_c
            f_c = modulate(rms_norm(ctx, norm_ffn_w[l]), m_c[3], m_c[4]).reshape(-1, D_MODEL)
            y_all = hier_moe(jnp.concatenate([f_l, f_c], axis=0), *moe_args)
            n_lat = f_l.shape[0]
            y_l = y_all[:n_lat]
            ctx = ctx + m_c[5] * y_all[n_lat:].reshape(ctx.shape)
        x = x + m_l[5] * y_l.reshape(x.shape)
    return rms_norm(x, final_norm_w)
```

```python
import contextlib
import numpy as np
import ml_dtypes
import concourse.bass as bass
import concourse.mybir as mybir
from concourse.bass_utils import run_bass_kernel_spmd

F32 = mybir.dt.float32
BF16 = mybir.dt.bfloat16
AF = mybir.ActivationFunctionType
ALU = mybir.AluOpType
AX = mybir.AxisListType

COMPUTE = ('pe', 'act', 'dve', 'pool')
ALLENG = ('pe', 'act', 'dve', 'pool', 'sp')
NDMASEM = 12

D = 1024
NIN = 7176
NPJ = 3080
TC = 256
DEPTH = 2
EPS = 1e-6
NEXP = 32
DFF = 512
SPARSE_MOE = True


_UID = [0]


class Op:
    __slots__ = ('eng', 'idx', 'fn', 'waits', 'signal', 'dma', 'dsem', 'dval', 'vc', 'dwait')


class Prog:
    def __init__(self, nc):
        self.nc = nc
        self.ops = {e: [] for e in ALLENG}
        self.lastw = {}
        self.readers = {}
        self.vc = {e: {f: -1 for f in COMPUTE} for e in ALLENG}
        self.selfk = {e: -1 for e in ALLENG}
        self.dma_known = {e: set() for e in ALLENG}
        self.dma_cnt = {e: 0 for e in ALLENG}
        self.dma_ops = {e: [] for e in ALLENG}

    def add(self, eng, fn, reads=(), writes=(), dma=False):
        op = Op()
        op.eng = eng
        op.fn = fn
        op.idx = len(self.ops[eng])
        op.signal = False
        op.dma = dma
        op.waits = []
        op.dwait = None
        op.dsem = None
        op.dval = None
        deps = []
        for r in reads:
            w = self.lastw.get(r)
            if w is not None:
                deps.append((w, True))
        for w_ in writes:
            w = self.lastw.get(w_)
            if w is not None:
                deps.append((w, False))
            for rd in self.readers.get(w_, ()):
                deps.append((rd, False))
        vc = self.vc[eng]
        for d, raw in deps:
            if d is op:
                continue
            if d.dma:
                if id(d) in self.dma_known[eng]:
                    continue
                self.dma_known[eng].add(id(d))
                op.waits.append(d)
            elif d.eng == eng:
                if not dma:
                    if eng == 'pe':
                        continue
                if self.selfk[eng] >= d.idx:
                    continue
                self.selfk[eng] = d.idx
                d.signal = True
                op.waits.append(d)
            else:
                if vc[d.eng] >= d.idx:
                    continue
                d.signal = True
                op.waits.append(d)
                for f in COMPUTE:
                    if d.vc[f] > vc[f]:
                        vc[f] = d.vc[f]
        if dma:
            i = self.dma_cnt[eng]
            self.dma_cnt[eng] += 1
            op.dsem = i % NDMASEM
            op.dval = 16 * (i // NDMASEM + 1)
            if i >= NDMASEM:
                op.dwait = (op.dsem, 16 * (i // NDMASEM))
                prev = self.dma_ops[eng][i - NDMASEM]
                self.dma_known[eng].add(id(prev))
            self.dma_ops[eng].append(op)
            op.vc = None
        else:
            if eng in COMPUTE:
                vc[eng] = op.idx
            op.vc = {f: vc[f] for f in COMPUTE}
        for r in reads:
            self.readers.setdefault(r, []).append(op)
        for w_ in writes:
            self.lastw[w_] = op
            self.readers[w_] = []
        self.ops[eng].append(op)
        return op

    def emit(self):
        nc = self.nc
        with contextlib.ExitStack() as st:
            _UID[0] += 1
            u = _UID[0]
            esem = {e: nc.alloc_semaphore('s%d_%s' % (u, e)) for e in COMPUTE}
            dsem = {e: [nc.alloc_semaphore('d%d_%s_%d' % (u, e, i)) for i in range(NDMASEM)]
                    for e in ALLENG if self.dma_cnt[e] > 0}
            self.allsems = list(esem.values()) + [x for v in dsem.values() for x in v]
            cnts = {}
            for e in COMPUTE:
                c = 0
                for op in self.ops[e]:
                    if op.signal and not op.dma:
                        c += 1
                    cnts[id(op)] = c
            block = st.enter_context(nc.Block())
            final = []
            for e in ALLENG:
                for i in range(min(NDMASEM, self.dma_cnt[e])):
                    n = (self.dma_cnt[e] - 1 - i) // NDMASEM + 1
                    final.append((dsem[e][i], 16 * n))

            def run(e, eng):
                for op in self.ops[e]:
                    if op.dwait is not None:
                        eng.wait_ge(dsem[e][op.dwait[0]], op.dwait[1])
                    for d in op.waits:
                        if d.dma:
                            eng.wait_ge(dsem[d.eng][d.dsem], d.dval)
                        else:
                            eng.wait_ge(esem[d.eng], cnts[id(d)])
                    ins = op.fn(eng)
                    if op.dma:
                        ins.then_inc(dsem[e][op.dsem], 16)
                    elif op.signal:
                        ins.then_inc(esem[e], 1)
                if e == 'sp':
                    for s, v in final:
                        eng.wait_ge(s, v)

            @block.tensor
            def _(eng):
                run('pe', eng)

            @block.scalar
            def _(eng):
                run('act', eng)

            @block.vector
            def _(eng):
                run('dve', eng)

            @block.gpsimd
            def _(eng):
                run('pool', eng)

            @block.sync
            def _(eng):
                run('sp', eng)


class Ring:
    def __init__(self, tiles):
        self.tiles = tiles
        self.i = 0

    def next(self):
        t = self.tiles[self.i % len(self.tiles)]
        self.i += 1
        return t


def _keys(aps):
    out = []
    for a in aps:
        if a is None or isinstance(a, (float, int)):
            continue
        out.append(a if isinstance(a, (str, tuple)) else a.name)
    return out


class St:
    def __init__(self, nc, tag):
        self.nc = nc
        self.P = Prog(nc)
        self.es = contextlib.ExitStack()
        self.tag = tag

    def sb(self, shape, dt=F32):
        _UID[0] += 1
        return self.es.enter_context(self.nc.sbuf_tensor('%s_s%d' % (self.tag, _UID[0]), list(shape), dt))

    def ps(self, shape=(128, 512), dt=F32):
        _UID[0] += 1
        nm = '%s_p%d' % (self.tag, _UID[0])
        if not hasattr(self, 'psnames'):
            self.psnames = set()
        self.psnames.add(nm)
        return self.es.enter_context(self.nc.psum_tensor(nm, list(shape), dt))

    def ring(self, n, shape, dt=F32):
        return Ring([self.sb(shape, dt) for _ in range(n)])

    def psring(self, n, shape=(128, 512), dt=F32):
        return Ring([self.ps(shape, dt) for _ in range(n)])

    def close(self):
        self.P.emit()
        self.es.close()
        self.nc.all_engine_barrier()
        self.nc.clear_and_free_semaphores(self.P.allsems)
        self.nc.all_engine_barrier()

    def begin(self, name):
        if not hasattr(self, 'groups'):
            self.groups = {}
        self.grp = name
        self.groups[name] = []

    def flush(self, names):
        items = []
        for gi, nm in enumerate(names):
            lst = self.groups.pop(nm)
            n = len(lst)
            for k, it in enumerate(lst):
                items.append(((k + 0.5) / n, gi, k, it))
        items.sort(key=lambda x: (x[0], x[1], x[2]))
        self.grp = None
        for _, _, _, (eng, fn, r, w, dma) in items:
            self._add(eng, fn, r, w, dma)

    def op(self, eng, fn, ins, outs, dma=False):
        if getattr(self, 'grp', None) is not None:
            self.groups[self.grp].append((eng, fn, _keys(ins), _keys(outs), dma))
            return
        self._add(eng, fn, _keys(ins), _keys(outs), dma)

    def _add(self, eng, fn, r, w, dma):
        import os
        lim = os.environ.get('OPLIMIT_' + self.tag)
        self.nop_ = getattr(self, 'nop_', 0) + 1
        if lim is not None and self.nop_ > int(lim):
            return
        psn = getattr(self, 'psnames', ())
        w = list(w) + [k for k in r if k in psn and k not in w]
        self.P.add(eng, fn, reads=r, writes=w, dma=dma)

    def mm(self, out, lhsT, rhs, start=True, stop=True):
        self.op('pe', lambda e: e.matmul(out, lhsT, rhs, start=start, stop=stop), [lhsT, rhs], [out])

    def tr(self, out, in_, ident):
        self.op('pe', lambda e: e.transpose(out, in_, ident), [in_, ident], [out])

    def act(self, out, in_, func, bias=None, scale=None, accum=None, eng='act'):
        kw = {}
        if bias is not None:
            kw['bias'] = bias
        if scale is not None:
            kw['scale'] = scale
        if accum is not None:
            kw['accum_out'] = accum
        self.op(eng, lambda e: e.activation(out=out, in_=in_, func=func, **kw),
                [in_, bias, scale], [out, accum])

    def tt(self, out, a, b, op, eng='dve'):
        self.op(eng, lambda e: e.tensor_tensor(out=out, in0=a, in1=b, op=op), [a, b], [out])

    def ts(self, out, a, s1, s2, op0, op1=None, eng='dve'):
        if op1 is None:
            self.op(eng, lambda e: e.tensor_scalar(out=out, in0=a, scalar1=s1, scalar2=None, op0=op0),
                    [a, s1], [out])
        else:
            self.op(eng, lambda e: e.tensor_scalar(out=out, in0=a, scalar1=s1, scalar2=s2, op0=op0, op1=op1),
                    [a, s1, s2], [out])

    def stt(self, out, in0, scalar, in1, op0, op1):
        self.op('dve', lambda e: e.scalar_tensor_tensor(out=out, in0=in0, scalar=scalar, in1=in1, op0=op0, op1=op1),
                [in0, scalar, in1], [out])

    def cp(self, out, in_, eng='dve'):
        if eng == 'act':
            self.op('act', lambda e: e.copy(out=out, in_=in_), [in_], [out])
        else:
            self.op(eng, lambda e: e.tensor_copy(out=out, in_=in_), [in_], [out])

    def recip(self, out, in_):
        self.op('dve', lambda e: e.reciprocal(out=out, in_=in_), [in_], [out])

    def memset(self, out, v, eng='dve'):
        self.op(eng, lambda e: e.memset(out, v), [], [out])

    def reduce(self, out, in_, op, eng='dve'):
        self.op(eng, lambda e: e.tensor_reduce(out=out, in_=in_, axis=AX.X, op=op), [in_], [out])

    def ld(self, out, in_, q='sp', slow=False):
        if slow:
            self.op(q, lambda e: e.dma_start(out=out, in_=in_, allow_slow_non_contiguous=True), [], [out], dma=True)
        else:
            self.op(q, lambda e: e.dma_start(out=out, in_=in_), [], [out], dma=True)

    def stv(self, out, in_, q='pool'):
        self.op(q, lambda e: e.dma_start(out=out, in_=in_), [in_], [], dma=True)

    def rstd(self, ss, scale):
        self.ts(ss, ss, scale, EPS, ALU.mult, ALU.add)
        self.act(ss, ss, AF.Sqrt)
        self.recip(ss, ss)


def make_consts(T):
    c = {}
    L = 64
    mid = 31
    s = np.arange(L)[:, None]
    t = np.arange(L)[None, :]
    tabs = []
    half = lambda a: (a >= 32)
    for dirn in range(2):
        if dirn == 0:
            midt = np.where(t < 32, 15, 47)
            M1 = (s <= t).astype(np.float32) - (s <= midt).astype(np.float32)
            M2 = (s <= t).astype(np.float32)
            M3 = (s > t).astype(np.float32)
            TRI = (s <= t).astype(np.float32)
            NTRI = -(s <= t).astype(np.float32)
            MASK = (s <= t).astype(np.float32)
            MASKD = ((s <= t) & (half(s) == half(t))).astype(np.float32)
            MASKO = ((s <= 31) & (t >= 32)).astype(np.float32)
            M4 = np.where(t >= 32, (s >= 32) & (s <= t), (s >= t + 1) & (s <= 31)).astype(np.float32)
        else:
            midt = np.where(t < 32, 16, 48)
            M1 = -((s < t).astype(np.float32) - (s < midt).astype(np.float32))
            M2 = (s >= t).astype(np.float32)
            M3 = (s < t).astype(np.float32)
            TRI = -(s < t).astype(np.float32)
            NTRI = (s < t).astype(np.float32)
            MASK = (s >= t).astype(np.float32)
            MASKD = ((s >= t) & (half(s) == half(t))).astype(np.float32)
            MASKO = ((s >= 32) & (t <= 31)).astype(np.float32)
            M4 = np.where(t <= 31, (s >= t) & (s <= 31), (s >= 32) & (s <= t - 1)).astype(np.float32)
        NEG = (MASK - 1.0) * 30000.0
        tabs += [M1, M2, M3, TRI, NTRI, NEG, MASKD, MASKO, M4]
    tabs += [np.ones((L, L), np.float32), np.eye(L, dtype=np.float32)]
    c['cst64'] = np.ascontiguousarray(np.concatenate(tabs, axis=1)).astype(np.float32)
    c['identb'] = np.eye(128, dtype=np.float32).astype(ml_dtypes.bfloat16)
    c['identf'] = np.eye(128, dtype=np.float32)
    NBM = 2 * (T + TC) // 128 + NEXP
    tp = np.arange(128)
    mo = np.zeros((128, 128 + 128 + NBM + 1 + 64), np.float32)
    mo[:, 0:128] = 1.0
    mo[:, 128:256] = (tp[:, None] < tp[None, :]).astype(np.float32)
    mo[:, 256:256 + NBM] = 128.0 * np.arange(NBM)[None, :]
    mo[:, 256 + NBM] = tp
    e_ = np.arange(32)
    mo[0:32, 257 + NBM:257 + NBM + 32] = (e_[:, None] < e_[None, :]).astype(np.float32)
    mo[0:32, 289 + NBM:289 + NBM + 32] = (e_[:, None] <= e_[None, :]).astype(np.float32)
    c['moec'] = mo
    ck = np.arange(64)
    ang = 2 * np.pi * np.outer(ck, ck) / 64.0
    cb = np.zeros((128, 128), np.float64)
    sbm = np.zeros((128, 128), np.float64)
    for g in range(2):
        cb[g * 64:(g + 1) * 64, g * 64:(g + 1) * 64] = np.cos(ang)
        sbm[g * 64:(g + 1) * 64, g * 64:(g + 1) * 64] = -np.sin(ang)
    c['chdft'] = np.concatenate([cb, sbm], axis=1).astype(np.float32).astype(ml_dtypes.bfloat16)
    for nm, Ts in (('l', T), ('c', TC)):
        T1 = Ts // 64
        a1 = 2 * np.pi * np.outer(np.arange(T1), np.arange(T1)) / T1
        c['f1_' + nm] = np.concatenate([np.cos(a1), np.sin(a1), -np.sin(a1)], axis=1).astype(np.float32).astype(ml_dtypes.bfloat16)
        tw = 2 * np.pi * np.outer(np.arange(T1), np.arange(64)) / Ts
        c['tw_' + nm] = np.concatenate([np.cos(tw), -np.sin(tw)], axis=1).astype(np.float32)
        sc = 1.0 / np.sqrt(Ts * 64.0)
        c['f2_' + nm] = (np.concatenate([np.cos(ang), np.sin(ang)], axis=1) * sc).astype(np.float32).astype(ml_dtypes.bfloat16)
    return c


def build_program(T, depth=DEPTH, debug=False, max_stages=10 ** 9):
    nc = bass.Bass("TRN2", target_bir_lowering=False)
    A = {}

    def din(name, shape, dt=F32):
        A[name] = nc.dram_tensor(name, list(shape), dt, kind="ExternalInput").ap()

    def dscr(name, shape, dt=F32):
        kind = "ExternalOutput" if debug else "Internal"
        A[name] = nc.dram_tensor(name, list(shape), dt, kind=kind).ap()

    din('x', [T, D])
    din('ctx', [TC, D])
    din('cc', [128, 16])
    din('ada_w', [DEPTH, D, 6 * D])
    din('ada_b', [DEPTH, 6 * D])
    din('norm_mix_w', [DEPTH, D])
    din('norm_ffn_w', [DEPTH, D])
    din('w_in', [DEPTH, D, NIN])
    din('conv_a_w', [DEPTH, 3, 256])
    din('conv_a_b', [DEPTH, 256])
    din('hgrn_lb', [2, DEPTH, 256])
    din('hgrn_norm_w', [DEPTH, 64])
    din('ssm_conv_w', [DEPTH, 3, 512])
    din('ssm_conv_b', [DEPTH, 512])
    din('ssm_A_log', [DEPTH, 2, 4])
    din('ssm_dt_bias', [DEPTH, 2, 4])
    din('ssm_D', [DEPTH, 4])
    din('ssm_norm_w', [DEPTH, 256])
    din('w_branch', [DEPTH, 4, 256, D])
    din('w_out', [DEPTH, D, D])
    din('router_w', [DEPTH, D, 36])
    din('router_b', [DEPTH, 36])
    din('moe_w1', [DEPTH, NEXP, D, DFF])
    din('moe_w3', [DEPTH, NEXP, D, DFF])
    din('moe_w2', [DEPTH, NEXP, DFF, D])
    din('final_norm_w', [D])
    consts = make_consts(T)
    for k, v in consts.items():
        din(k, v.shape, BF16 if v.dtype == ml_dtypes.bfloat16 else F32)
    A['out'] = nc.dram_tensor('out', [T, D], F32, kind="ExternalOutput").ap()

    dscr('MV', [DEPTH, 2, 6 * D])
    NBM = 2 * (T + TC) // 128 + NEXP
    NTM = (T + TC) // 128
    dscr('FN', [T + TC, D], BF16)
    dscr('FB', [NBM * 128, D], BF16)
    dscr('YBUF', [NBM * 128, D], F32)
    dscr('W1S', [NEXP * 128, 8 * DFF], BF16)
    dscr('W3S', [NEXP * 128, 8 * DFF], BF16)
    dscr('W2S', [NEXP * 128, 4 * D], BF16)
    streams = {}
    for nm, Ts in (('c', TC), ('l', T)):
        sd = {'T': Ts, 'nm': nm, 'row': 0 if nm == 'l' else 1}
        for k, shp, dt in (('PJ', [Ts, NPJ], F32), ('GT', [Ts, 4096], BF16), ('U', [Ts + 2, 256], F32),
                           ('XB', [Ts + 2, 512], F32), ('ZF', [Ts, 512], BF16), ('ZB', [Ts // 64, 64, 512], BF16),
                           ('YC', [Ts, 256], F32), ('OF', [Ts, 256], F32), ('YB', [Ts, 256], F32),
                           ('YF', [Ts, 256], F32), ('YD', [Ts, 256], F32),
                           ('X1', [Ts, D], F32), ('X2', [Ts, D], F32)):
            dscr(k + '_' + nm, shp, dt)
            sd[k] = A[k + '_' + nm]
        streams[nm] = sd
    streams['l']['x'] = A['x']
    streams['c']['x'] = A['ctx']

    pst = contextlib.ExitStack()
    with pst:
        states = {}
        for mx in ('h', 'm'):
            for dirn in range(2):
                states[(mx, dirn)] = pst.enter_context(nc.sbuf_tensor('state_%s%d' % (mx, dirn), [128, 2, 64], F32))

        I32 = mybir.dt.int32
        pers = {
            'OHT': pst.enter_context(nc.sbuf_tensor('p_oht', [128, NTM, 64], F32)),
            'WTT': pst.enter_context(nc.sbuf_tensor('p_wtt', [128, NTM, 2], F32)),
            'DEST': pst.enter_context(nc.sbuf_tensor('p_dest', [128, NTM, 2], I32)),
            'IDXW': pst.enter_context(nc.sbuf_tensor('p_idxw', [128, NBM], I32)),
            'PADB': pst.enter_context(nc.sbuf_tensor('p_padb', [128, 64], F32)),
        }
        nst = [0]

        def run(fn, *a):
            nst[0] += 1
            if nst[0] <= max_stages:
                fn(*a)

        for l in range(depth):
            last = (l == DEPTH - 1)
            run(stage_ada, nc, A, l)
            run(stage_proj, nc, A, l, streams, last)
            for nm in (('l',) if last else ('c', 'l')):
                run(stage_fft_a, nc, A, streams[nm])
                run(stage_fft_c, nc, A, streams[nm])
            for dirn in range(2):
                run(stage_scan, nc, A, l, streams, dirn, states)
            for nm in (('l',) if last else ('c', 'l')):
                run(stage_merge, nc, A, l, streams[nm])
            if SPARSE_MOE:
                run(stage_moe_route, nc, A, l, streams, last, pers, NBM)
                run(stage_moe_disp, nc, A, l, streams, last, pers, NBM)
                run(stage_moe_exp, nc, A, l, streams, last, pers, NBM)
                run(stage_moe_comb, nc, A, l, streams, last, pers)
            else:
                run(stage_moe, nc, A, l, streams, last)
            streams['l']['x'] = streams['l']['X2']
            streams['c']['x'] = streams['c']['X2']
    return nc, consts


def stage_ada(nc, A, l):
    S = St(nc, 'ada%d' % l)
    sT = S.sb([128, 16])
    S.ld(sT[:], A['cc'])
    S.act(sT[:], sT[:], AF.Silu)
    sT3 = sT[:].rearrange("p (k j) -> p k j", j=2)
    abt = S.sb([2, 6 * D])
    S.ld(abt[:], A['ada_b'][l].partition_broadcast(2))
    mrow = S.sb([2, 6 * D])
    awr = S.ring(2, [128, 8, 512])
    psr = S.psring(2)
    for j in range(12):
        aw = awr.next()
        S.ld(aw[:], A['ada_w'][l, :, j * 512:(j + 1) * 512].rearrange("(k p) n -> p k n", p=128))
        pp = psr.next()
        for k in range(8):
            S.mm(pp[0:2, :], sT3[:, k, :], aw[:, k, :], start=(k == 0), stop=(k == 7))
        S.tt(mrow[:, j * 512:(j + 1) * 512], pp[0:2, :], abt[:, j * 512:(j + 1) * 512], ALU.add)
    S.stv(A['MV'][l], mrow[:])
    S.close()


PJ_BLOCKS = [(i * 512, (i + 1) * 512) for i in range(6)] + [(3072, 3080)] + \
            [(NPJ + i * 512, NPJ + (i + 1) * 512) for i in range(8)]


def load_mod(S, A, l, row, j, wvec=None):
    t = S.sb([128, D])
    S.ld(t[:], A['MV'][l, row, j * D:(j + 1) * D].partition_broadcast(128))
    if wvec is not None:
        S.stt(t[:], t[:], 1.0, wvec[:], ALU.add, ALU.mult)
    return t


def stage_proj(nc, A, l, streams, last):
    S = St(nc, 'pj%d' % l)
    win = S.sb([128, 8, NIN], BF16)
    for k in range(8):
        S.ld(win[:, k, :], A['w_in'][l, k * 128:(k + 1) * 128, :], q='pool')
    wmix = S.sb([128, D])
    S.ld(wmix[:], A['norm_mix_w'][l].partition_broadcast(128))
    ident = S.sb([128, 128], BF16)
    S.ld(ident[:], A['identb'])
    chd = S.sb([128, 256], BF16)
    S.ld(chd[:], A['chdft'])
    zero = S.sb([2, 512])
    S.memset(zero[:], 0.0)
    xr = S.ring(2, [128, D])
    junk = S.sb([128, D])
    ssr = S.ring(2, [128, 1])
    hn = S.sb([128, D])
    hb = S.sb([128, D], BF16)
    hTr = S.ring(2, [128, D], BF16)
    pT = S.ps([128, D], BF16)
    ppr = S.psring(4)
    pF = S.ps()
    pZ = S.ps()
    stgr = S.ring(4, [128, 512])
    gr = S.ring(3, [128, 512], BF16)
    ur = S.ring(2, [128, 256])
    f4r = S.ring(2, [128, 256], BF16)
    zr = S.ring(2, [128, 512], BF16)
    for nm in ('c', 'l'):
        sd = streams[nm]
        Ts = sd['T']
        need_out = not (last and nm == 'c')
        A1 = load_mod(S, A, l, sd['row'], 1, wmix)
        B1 = load_mod(S, A, l, sd['row'], 0)
        S.stv(sd['U'][0:1, :], zero[0:1, 0:256])
        S.stv(sd['U'][Ts + 1:Ts + 2, :], zero[0:1, 0:256])
        S.stv(sd['XB'][0:1, :], zero[0:1, :])
        S.stv(sd['XB'][Ts + 1:Ts + 2, :], zero[0:1, :])
        for i in range(Ts // 128):
            r0, r1 = i * 128, (i + 1) * 128
            xt = xr.next()
            ss = ssr.next()
            S.ld(xt[:], sd['x'][r0:r1, :])
            S.act(junk[:], xt[:], AF.Square, accum=ss[:])
            S.rstd(ss[:], 1.0 / D)
            S.stt(hn[:], xt[:], ss[:], A1[:], ALU.mult, ALU.mult)
            S.tt(hb[:], hn[:], B1[:], ALU.add)
            for k in range(8):
                S.tr(pT[:, k * 128:(k + 1) * 128], hb[:, k * 128:(k + 1) * 128], ident[:])
            hT = hTr.next()
            S.cp(hT[:], pT[:], eng='act')
            stgs = {}
            for bi, (c0, c1) in enumerate(PJ_BLOCKS):
                if c0 >= NPJ and not need_out:
                    continue
                w = c1 - c0
                pp = ppr.next()
                for k in range(8):
                    S.mm(pp[:, 0:w], hT[:, k * 128:(k + 1) * 128], win[:, k, c0:c1], start=(k == 0), stop=(k == 7))
                if c0 < NPJ:
                    stg = stgr.next()
                    stgs[bi] = stg
                    S.cp(stg[:, 0:w], pp[:, 0:w], eng=('act' if bi % 2 == 0 else 'dve'))
                    S.stv(sd['PJ'][r0:r1, c0:c1], stg[:, 0:w])
                    if bi == 1:
                        ut = ur.next()
                        S.tt(ut[:], stgs[0][:, 256:512], stg[:, 0:256], ALU.mult)
                        S.stv(sd['U'][1 + r0:1 + r1, :], ut[:])
                    if bi == 5:
                        S.stv(sd['XB'][1 + r0:1 + r1, :], stg[:, :])
                else:
                    g = gr.next()
                    S.act(g[:], pp[:], AF.Sigmoid)
                    S.stv(sd['GT'][r0:r1, c0 - NPJ:c1 - NPJ], g[:])
            if need_out:
                for hf in range(2):
                    for k in range(8):
                        S.mm(pF[:, hf * 128:(hf + 1) * 128], win[:, k, 2048 + hf * 128:2048 + (hf + 1) * 128],
                             hT[:, k * 128:(k + 1) * 128], start=(k == 0), stop=(k == 7))
                f4 = f4r.next()
                S.cp(f4[:], pF[:, 0:256])
                for hf in range(2):
                    S.mm(pZ[:, hf * 128:(hf + 1) * 128], f4[:, hf * 128:(hf + 1) * 128], chd[:, 0:128])
                    S.mm(pZ[:, 256 + hf * 128:256 + (hf + 1) * 128], f4[:, hf * 128:(hf + 1) * 128], chd[:, 128:256])
                zt = zr.next()
                S.cp(zt[:], pZ[:], eng='act')
                S.stv(sd['ZF'][r0:r1, :], zt[:])
    S.close()


def stage_fft_a(nc, A, sd):
    nm = sd['nm']
    Ts = sd['T']
    T1 = Ts // 64
    S = St(nc, 'fa' + nm)
    f1 = S.sb([T1, 3 * T1], BF16)
    S.ld(f1[:], A['f1_' + nm])
    tw = S.sb([T1, 128])
    S.ld(tw[:], A['tw_' + nm])
    C1, S1, S1n = f1[:, 0:T1], f1[:, T1:2 * T1], f1[:, 2 * T1:3 * T1]
    ZRt = S.sb([T1, 64 * 256], BF16)
    ZIt = S.sb([T1, 64 * 256], BF16)
    BRt = S.sb([T1, 64 * 256], BF16)
    BIt = S.sb([T1, 64 * 256], BF16)
    ZR = ZRt[:].rearrange("p (b c) -> p b c", c=256)
    ZI = ZIt[:].rearrange("p (b c) -> p b c", c=256)
    BR = BRt[:].rearrange("p (b c) -> p b c", c=256)
    BI = BIt[:].rearrange("p (b c) -> p b c", c=256)
    zv = sd['ZF'].rearrange("(a b) c -> a b c", b=64)
    for q4 in range(4):
        S.ld(ZR[:, q4 * 16:(q4 + 1) * 16, :], zv[:, q4 * 16:(q4 + 1) * 16, 0:256])
        S.ld(ZI[:, q4 * 16:(q4 + 1) * 16, :], zv[:, q4 * 16:(q4 + 1) * 16, 256:512])
    par = S.psring(2)
    pbr = S.psring(2)
    t1r = S.ring(2, [T1, 512])
    t2r = S.ring(2, [T1, 512])
    for j in range(32):
        zr_ = ZRt[:, j * 512:(j + 1) * 512]
        zi_ = ZIt[:, j * 512:(j + 1) * 512]
        pa = par.next()
        pb = pbr.next()
        pa3 = pa[0:T1, :].rearrange("p (a c) -> p a c", a=2)
        pb3 = pb[0:T1, :].rearrange("p (a c) -> p a c", a=2)
        S.mm(pa[0:T1, :], C1, zr_, start=True, stop=False)
        S.mm(pa[0:T1, :], S1, zi_, start=False, stop=True)
        S.mm(pb[0:T1, :], C1, zi_, start=True, stop=False)
        S.mm(pb[0:T1, :], S1n, zr_, start=False, stop=True)
        wr = tw[:, 2 * j:2 * j + 2].unsqueeze(2).to_broadcast([T1, 2, 256])
        wi = tw[:, 64 + 2 * j:64 + 2 * j + 2].unsqueeze(2).to_broadcast([T1, 2, 256])
        t1 = t1r.next()
        t2 = t2r.next()
        t13 = t1[:].rearrange("p (a c) -> p a c", a=2)
        t23 = t2[:].rearrange("p (a c) -> p a c", a=2)
        S.tt(t13, pa3, wr, ALU.mult)
        S.tt(t23, pb3, wi, ALU.mult)
        S.tt(BR[:, 2 * j:2 * j + 2, :], t13, t23, ALU.subtract, eng='pool')
        t1b = t1r.next()
        t2b = t2r.next()
        t1b3 = t1b[:].rearrange("p (a c) -> p a c", a=2)
        t2b3 = t2b[:].rearrange("p (a c) -> p a c", a=2)
        S.tt(t1b3, pa3, wi, ALU.mult)
        S.tt(t2b3, pb3, wr, ALU.mult)
        S.tt(BI[:, 2 * j:2 * j + 2, :], t1b3, t2b3, ALU.add, eng='pool')
    for q4 in range(4):
        S.stv(sd['ZB'][:, q4 * 16:(q4 + 1) * 16, 0:256], BR[:, q4 * 16:(q4 + 1) * 16, :])
        S.stv(sd['ZB'][:, q4 * 16:(q4 + 1) * 16, 256:512], BI[:, q4 * 16:(q4 + 1) * 16, :])
    S.close()


def stage_fft_c(nc, A, sd):
    nm = sd['nm']
    Ts = sd['T']
    T1 = Ts // 64
    S = St(nc, 'fc' + nm)
    f2 = S.sb([64, 128], BF16)
    S.ld(f2[:], A['f2_' + nm])
    CF, SF = f2[:, 0:64], f2[:, 64:128]
    BRt = S.sb([64, T1 * 256], BF16)
    BIt = S.sb([64, T1 * 256], BF16)
    BR = BRt[:].rearrange("p (a c) -> p a c", c=256)
    BI = BIt[:].rearrange("p (a c) -> p a c", c=256)
    zb = sd['ZB'].rearrange("a b c -> b a c")
    nq = 4 if T1 >= 4 else 1
    qs = T1 // nq
    for q4 in range(nq):
        S.ld(BR[:, q4 * qs:(q4 + 1) * qs, :], zb[:, q4 * qs:(q4 + 1) * qs, 0:256])
        S.ld(BI[:, q4 * qs:(q4 + 1) * qs, :], zb[:, q4 * qs:(q4 + 1) * qs, 256:512])
    pr = S.psring(3)
    yr = S.ring(4, [64, 512])
    ycv = sd['YC'].rearrange("(b a) c -> b a c", a=T1)
    for j in range(T1 // 2):
        pp = pr.next()
        pp3 = pp[0:64, :].rearrange("p (a c) -> p a c", a=2)
        S.mm(pp[0:64, :], CF, BRt[:, j * 512:(j + 1) * 512], start=True, stop=False)
        S.mm(pp[0:64, :], SF, BIt[:, j * 512:(j + 1) * 512], start=False, stop=True)
        y = yr.next()
        y3 = y[:].rearrange("p (a c) -> p a c", a=2)
        S.cp(y[:], pp[0:64, :], eng=('act' if j % 2 == 0 else 'dve'))
        S.stv(ycv[:, 2 * j:2 * j + 2, :], y3)
    S.close()


def stage_scan(nc, A, l, streams, dirn, states):
    S = St(nc, 'sc%d%d' % (l, dirn))
    fin = (dirn == 1)
    cst = S.sb([64, 20 * 64])
    S.ld(cst[:], A['cst64'])

    def tab(i):
        return cst[:, i * 64:(i + 1) * 64]
    o = dirn * 9
    M1, M2, M3, TRI, NTRI, NEG, MASKD, MASKO, M4 = [tab(o + i) for i in range(9)]
    ONES, IDF = tab(18), tab(19)
    identb = S.sb([128, 128], BF16)
    S.ld(identb[:], A['identb'])
    idb = identb[0:64, 0:64]
    LB0 = S.sb([64, 256])
    LB1 = S.sb([64, 256])
    if l == 0:
        S.memset(LB0[:], 0.0)
        S.memset(LB1[:], 1.0)
    else:
        a0 = S.sb([64, 256])
        S.ld(a0[:], A['hgrn_lb'][dirn, 0].partition_broadcast(64))
        S.ld(LB0[:], A['hgrn_lb'][dirn, 1].partition_broadcast(64))
        S.tt(LB0[:], LB0[:], a0[:], ALU.subtract)
        S.act(LB0[:], LB0[:], AF.Sigmoid)
        S.ts(LB1[:], LB0[:], -1.0, 1.0, ALU.mult, ALU.add)
    CW = S.sb([64, 3, 512])
    for j in range(3):
        S.ld(CW[:, j, :], A['ssm_conv_w'][l, j].partition_broadcast(64))
    CB = S.sb([64, 512])
    S.ld(CB[:], A['ssm_conv_b'][l].partition_broadcast(64))
    DTB = S.sb([64, 4])
    S.ld(DTB[:], A['ssm_dt_bias'][l, dirn].partition_broadcast(64))
    NEGA = S.sb([64, 4])
    S.ld(NEGA[:], A['ssm_A_log'][l, dirn].partition_broadcast(64))
    S.act(NEGA[:], NEGA[:], AF.Exp)
    S.ts(NEGA[:], NEGA[:], -1.0, None, ALU.mult)
    if fin:
        HNW = S.sb([64, 64])
        S.ld(HNW[:], A['hgrn_norm_w'][l].partition_broadcast(64))
        SNW = S.sb([64, 256])
        S.ld(SNW[:], A['ssm_norm_w'][l].partition_broadcast(64))
        DV = S.sb([64, 4])
        S.ld(DV[:], A['ssm_D'][l].partition_broadcast(64))
    Sh = states[('h', dirn)]
    Sm = states[('m', dirn)]
    S.memset(Sh[:], 0.0)
    S.memset(Sm[:], 0.0)
    Shb = S.sb([128, 2, 64], BF16)
    Smb = S.sb([128, 2, 64], BF16)
    S.memset(Shb[:], 0.0)
    S.memset(Smb[:], 0.0)

    RD = 2
    R = lambda shp, dt=F32, n=RD: S.ring(n, shp, dt)
    qf_r, ff_r, vi_r = R([64, 256]), R([64, 256]), R([64, 256])
    sg_r, sn_r, lf_r, kk_r, qs_r = R([64, 256]), R([64, 256]), R([64, 256]), R([64, 256]), R([64, 256])
    vb_r = R([64, 256], BF16, 3)
    fac_r = R([64, 5, 256])
    el_r = R([128, 4], F32, 3)
    qk_r = R([64, 6, 256], BF16, 3)
    tT_r = R([128, 256], BF16)
    Z_r = [R([128, 384], BF16), R([128, 384], BF16)]
    Zm_r = [R([128, 256], BF16), R([128, 256], BF16)]
    for rr in Z_r + Zm_r:
        for t_ in rr.tiles:
            S.memset(t_[:], 0.0)
    scm_r = R([64, 256], BF16)
    sco_r = R([64, 256], BF16)
    o_r = R([64, 256], F32, 3)
    xb_r = [R([64, 512]) for _ in range(3)]
    dtr_r = R([64, 4])
    cv_r, tmp_r, xc_r = R([64, 512]), R([64, 512]), R([64, 512], F32, 3)
    dt_r, la_r = R([64, 4]), R([64, 4])
    vbm_r = R([64, 256], BF16, 3)
    kkm_r = R([64, 256])
    a1_r, a2_r = R([64, 4, 64]), R([64, 4, 64])
    dm_r = R([64, 256])
    ec_r = R([64, 8])
    elm_r = R([128, 4], F32, 3)
    mk_r = R([64, 4, 256], BF16, 3)
    tTm_r = R([128, 128], BF16)
    scmm_r = R([64, 256], BF16)
    om_r = R([64, 256], F32, 3)
    psE, psE2, psS, psR, psG = S.ps(), S.ps(), S.ps(), S.ps(), S.ps()
    psT = S.ps([128, 1024], BF16)
    psO, psC = S.ps(), S.ps()
    if fin:
        of_r, g_r, z_r, yf_r = R([64, 256]), R([64, 256]), R([64, 256]), R([64, 256])
        sq_r, st_r = R([64, 256]), R([64, 4])
        sq2_r, st2_r = R([64, 256]), R([64, 4])

    order = []
    for nm in ('c', 'l'):
        nch = streams[nm]['T'] // 64
        rng_ = range(nch) if dirn == 0 else range(nch - 1, -1, -1)
        order += [(nm, c) for c in rng_]

    def hA1(nm, c):
        sd = streams[nm]
        r0, r1 = c * 64, (c + 1) * 64
        PJ = sd['PJ']
        qf, ff, vi = qf_r.next(), ff_r.next(), vi_r.next()
        S.ld(qf[:], PJ[r0:r1, 768:1024])
        S.ld(ff[:], PJ[r0:r1, 1024 + 256 * dirn:1280 + 256 * dirn])
        S.ld(vi[:], PJ[r0:r1, 1536:1792])
        sg, sn, lf, kk, qs, vb = sg_r.next(), sn_r.next(), lf_r.next(), kk_r.next(), qs_r.next(), vb_r.next()
        S.act(sg[:], ff[:], AF.Sigmoid)
        S.act(sn[:], ff[:], AF.Sigmoid, scale=-1.0)
        S.tt(sg[:], sg[:], LB1[:], ALU.mult)
        S.tt(sg[:], sg[:], LB0[:], ALU.add)
        S.act(lf[:], sg[:], AF.Ln)
        S.tt(kk[:], sn[:], LB1[:], ALU.mult, eng='pool')
        S.act(qs[:], qf[:], AF.Silu)
        S.cp(vb[:], vi[:], eng='pool')
        S.mm(psE[0:64, 0:256], M1, lf[:])
        S.mm(psE[0:64, 256:512], M2, lf[:])
        S.mm(psE2[0:64, 0:256], M3, lf[:])
        S.mm(psE2[0:64, 256:512], M4, lf[:])
        for p in range(2):
            S.mm(psG[:, 272 + 2 * p:274 + 2 * p], lf[:, p * 128:(p + 1) * 128], ONES[:, 0:2])
        fac = fac_r.next()
        S.act(fac[:, 0, :], psE[0:64, 0:256], AF.Exp)
        S.act(fac[:, 1, :], psE[0:64, 0:256], AF.Exp, scale=-1.0)
        S.act(fac[:, 2, :], psE[0:64, 256:512], AF.Exp)
        S.act(fac[:, 3, :], psE2[0:64, 0:256], AF.Exp)
        S.act(fac[:, 4, :], psE2[0:64, 256:512], AF.Exp)
        el = el_r.next()
        S.act(el[:], psG[:, 272:276], AF.Exp)
        qk = qk_r.next()
        S.tt(qk[:, 0, :], qs[:], fac[:, 0, :], ALU.mult)
        S.tt(qk[:, 1, :], qs[:], fac[:, 4, :], ALU.mult)
        S.tt(qk[:, 2, :], kk[:], fac[:, 1, :], ALU.mult)
        S.tt(qk[:, 3, :], qs[:], fac[:, 2, :], ALU.mult)
        S.tt(qk[:, 4, :], kk[:], fac[:, 4, :], ALU.mult, eng='pool')
        S.tt(qk[:, 5, :], kk[:], fac[:, 3, :], ALU.mult, eng='pool')
        return dict(vb=vb, qk=qk, el=el)

    def hA2(nm, c, t):
        vb, qk, el = t['vb'], t['qk'], t['el']
        for a in range(5):
            for p in range(2):
                S.tr(psT[:, (a * 2 + p) * 64:(a * 2 + p + 1) * 64], qk[:, a, p * 128:(p + 1) * 128], idb)
        tT = tT_r.next()
        Z = [Z_r[0].next(), Z_r[1].next()]
        S.cp(tT[:], psT[:, 0:256])
        S.cp(Z[0][0:64, :], psT[0:64, 256:640])
        S.cp(Z[1][64:128, :], psT[64:128, 256:640])
        for h in range(4):
            p, hf = h // 2, h % 2
            S.mm(psS[0:64, h * 64:(h + 1) * 64], Z[hf][:, p * 64:(p + 1) * 64], tT[:, p * 64:(p + 1) * 64])
        for h in range(4):
            p, hf = h // 2, h % 2
            S.mm(psS[0:64, 256 + h * 64:256 + (h + 1) * 64], Z[hf][:, 256 + p * 64:256 + (p + 1) * 64],
                 tT[:, 128 + p * 64:128 + (p + 1) * 64])
        scm, sco = scm_r.next(), sco_r.next()
        S.tt(scm[:].rearrange("p (h t) -> p h t", h=4), psS[0:64, 0:256].rearrange("p (h t) -> p h t", h=4),
             MASKD.unsqueeze(1).to_broadcast([64, 4, 64]), ALU.mult)
        S.tt(sco[:].rearrange("p (h t) -> p h t", h=4), psS[0:64, 256:512].rearrange("p (h t) -> p h t", h=4),
             MASKO.unsqueeze(1).to_broadcast([64, 4, 64]), ALU.mult)
        S.tt(scm[:], scm[:], sco[:], ALU.add, eng='pool')
        return dict(scm=scm, vb=vb, Z=Z, qk=qk, el=el)

    def hB(nm, c, t):
        sd = streams[nm]
        need_out = not (l == DEPTH - 1 and nm == 'c')
        r0, r1 = c * 64, (c + 1) * 64
        PJ = sd['PJ']
        scm, vb, Z, qk, el = t['scm'], t['vb'], t['Z'], t['qk'], t['el']
        for h in range(4):
            p, hf = h // 2, h % 2
            S.mm(psO[0:64, h * 64:(h + 1) * 64], scm[:, h * 64:(h + 1) * 64], vb[:, h * 64:(h + 1) * 64],
                 start=True, stop=False)
            S.mm(psO[0:64, h * 64:(h + 1) * 64], Z[hf][:, 128 + p * 64:128 + (p + 1) * 64],
                 Shb[:, p, :], start=False, stop=True)
        for p in range(2):
            S.mm(psO[:, 256 + p * 128:256 + (p + 1) * 128], qk[:, 5, p * 128:(p + 1) * 128], vb[:, p * 128:(p + 1) * 128])
        ot = o_r.next()
        S.cp(ot[:], psO[0:64, 0:256], eng='act')
        for h in range(4):
            p, hf = h // 2, h % 2
            sl = slice(hf * 64, (hf + 1) * 64)
            S.stt(Sh[sl, p, :], Sh[sl, p, :], el[sl, 2 * p:2 * p + 1],
                  psO[sl, 256 + p * 128 + hf * 64:256 + p * 128 + (hf + 1) * 64], ALU.mult, ALU.add)
        S.cp(Shb[:], Sh[:], eng='act')
        if not fin:
            S.stv(sd['OF'][r0:r1, :], ot[:])
        elif need_out:
            of, gt = of_r.next(), g_r.next()
            S.ld(of[:], sd['OF'][r0:r1, :])
            S.ld(gt[:], PJ[r0:r1, 1792:2048])
            S.tt(ot[:], ot[:], of[:], ALU.add)
            sq, stt_ = sq_r.next(), st_r.next()
            S.tt(sq[:], ot[:], ot[:], ALU.mult, eng='pool')
            S.reduce(stt_[:], sq[:].rearrange("p (h e) -> p h e", h=4), ALU.add)
            S.rstd(stt_[:], 1.0 / 64)
            S.act(gt[:], gt[:], AF.Silu)
            o3 = ot[:].rearrange("p (h e) -> p h e", h=4)
            S.tt(o3, o3, stt_[:].unsqueeze(2).to_broadcast([64, 4, 64]), ALU.mult)
            S.tt(o3, o3, HNW[:].unsqueeze(1).to_broadcast([64, 4, 64]), ALU.mult, eng='pool')
            S.tt(ot[:], ot[:], gt[:], ALU.mult, eng='pool')
            S.stv(sd['YB'][r0:r1, :], ot[:])

    def mA1(nm, c):
        sd = streams[nm]
        r0, r1 = c * 64, (c + 1) * 64
        PJ = sd['PJ']
        xb = [xb_r[j].next() for j in range(3)]
        for j in range(3):
            S.ld(xb[j][:], sd['XB'][r0 + j:r1 + j, :])
        dtr = dtr_r.next()
        S.ld(dtr[:], PJ[r0:r1, 3072 + 4 * dirn:3076 + 4 * dirn])
        cv, tmp, xc = cv_r.next(), tmp_r.next(), xc_r.next()
        S.tt(cv[:], xb[0][:], CW[:, 0, :], ALU.mult, eng='pool')
        S.tt(tmp[:], xb[1][:], CW[:, 1, :], ALU.mult, eng='pool')
        S.tt(cv[:], cv[:], tmp[:], ALU.add, eng='pool')
        S.tt(tmp[:], xb[2][:], CW[:, 2, :], ALU.mult, eng='pool')
        S.tt(cv[:], cv[:], tmp[:], ALU.add, eng='pool')
        S.tt(cv[:], cv[:], CB[:], ALU.add, eng='pool')
        S.act(xc[:], cv[:], AF.Silu)
        dt, la = dt_r.next(), la_r.next()
        S.tt(dt[:], dtr[:], DTB[:], ALU.add)
        S.act(dt[:], dt[:], AF.Exp)
        S.act(dt[:], dt[:], AF.Ln, bias=1.0)
        S.tt(la[:], dt[:], NEGA[:], ALU.mult)
        vbm, kkm = vbm_r.next(), kkm_r.next()
        S.cp(vbm[:], xc[:, 0:256], eng='pool')
        a1, a2 = a1_r.next(), a2_r.next()
        g4 = lambda ap: ap.rearrange("p (g n) -> p g n", g=2).unsqueeze(2).to_broadcast([64, 2, 2, 64])
        h4 = lambda ap: ap.rearrange("p (g h) -> p g h", g=2).unsqueeze(3).to_broadcast([64, 2, 2, 64])
        o4 = lambda ap: ap.rearrange("p (g h n) -> p g h n", g=2, h=2)
        S.tt(o4(kkm[:]), g4(xc[:, 256:384]), h4(dt[:, 0:4]), ALU.mult)
        lab = la[:, 0:4].unsqueeze(2).to_broadcast([64, 4, 64])
        S.tt(a1[:], ONES.unsqueeze(1).to_broadcast([64, 4, 64]), lab, ALU.mult)
        S.tt(a2[:], NTRI.unsqueeze(1).to_broadcast([64, 4, 64]), lab, ALU.mult)
        for h in range(4):
            S.mm(psR[0:64, h * 64:(h + 1) * 64], a1[:, h, :], TRI, start=True, stop=False)
            S.mm(psR[0:64, h * 64:(h + 1) * 64], a2[:, h, :], ONES, start=False, stop=False)
            S.mm(psR[0:64, h * 64:(h + 1) * 64], IDF, NEG, start=False, stop=True)
        S.mm(psR[0:64, 256:260], M2, la[:])
        S.mm(psR[0:64, 260:264], M3, la[:])
        for p in range(2):
            S.mm(psR[:, 264 + 2 * p:266 + 2 * p], a1[:, 2 * p:2 * p + 2, :].rearrange("p a b -> p (a b)"), ONES[:, 0:2])
        dm, ec, elm = dm_r.next(), ec_r.next(), elm_r.next()
        S.act(dm[:], psR[0:64, 0:256], AF.Exp)
        S.act(ec[:], psR[0:64, 256:264], AF.Exp)
        S.act(elm[:], psR[:, 264:268], AF.Exp)
        mk = mk_r.next()
        S.cp(mk[:, 1, :], kkm[:], eng='pool')
        S.cp(o4(mk[:, 0, :]), g4(xc[:, 384:512]), eng='pool')
        S.tt(o4(mk[:, 2, :]), g4(xc[:, 384:512]), h4(ec[:, 0:4]), ALU.mult)
        S.tt(mk[:, 3, :].rearrange("p (h n) -> p h n", h=4), kkm[:].rearrange("p (h n) -> p h n", h=4),
             ec[:, 4:8].unsqueeze(2).to_broadcast([64, 4, 64]), ALU.mult)
        return dict(vbm=vbm, mk=mk, elm=elm, xc=xc, dm=dm)

    def mA2(nm, c, t):
        vbm, mk, elm, xc, dm = t['vbm'], t['mk'], t['elm'], t['xc'], t['dm']
        for a in range(3):
            for p in range(2):
                S.tr(psT[:, 640 + (a * 2 + p) * 64:640 + (a * 2 + p + 1) * 64], mk[:, a, p * 128:(p + 1) * 128], idb)
        tTm = tTm_r.next()
        Zm = [Zm_r[0].next(), Zm_r[1].next()]
        S.cp(tTm[:], psT[:, 640:768])
        S.cp(Zm[0][0:64, :], psT[0:64, 768:1024])
        S.cp(Zm[1][64:128, :], psT[64:128, 768:1024])
        for h in range(4):
            p, hf = h // 2, h % 2
            S.mm(psG[0:64, h * 64:(h + 1) * 64], Zm[hf][:, p * 64:(p + 1) * 64], tTm[:, p * 64:(p + 1) * 64])
        scmm = scmm_r.next()
        S.tt(scmm[:], psG[0:64, 0:256], dm[:], ALU.mult)
        return dict(scmm=scmm, vbm=vbm, Zm=Zm, mk=mk, elm=elm, xc=xc)

    def mB(nm, c, t):
        sd = streams[nm]
        need_out = not (l == DEPTH - 1 and nm == 'c')
        r0, r1 = c * 64, (c + 1) * 64
        PJ = sd['PJ']
        scmm, vbm, Zm, mk, elm, xc = t['scmm'], t['vbm'], t['Zm'], t['mk'], t['elm'], t['xc']
        for h in range(4):
            p, hf = h // 2, h % 2
            S.mm(psC[0:64, h * 64:(h + 1) * 64], scmm[:, h * 64:(h + 1) * 64], vbm[:, h * 64:(h + 1) * 64],
                 start=True, stop=False)
            S.mm(psC[0:64, h * 64:(h + 1) * 64], Zm[hf][:, 128 + p * 64:128 + (p + 1) * 64],
                 Smb[:, p, :], start=False, stop=True)
        for p in range(2):
            S.mm(psC[:, 256 + p * 128:256 + (p + 1) * 128], mk[:, 3, p * 128:(p + 1) * 128], vbm[:, p * 128:(p + 1) * 128])
        om = om_r.next()
        S.cp(om[:], psC[0:64, 0:256], eng='act')
        for h in range(4):
            p, hf = h // 2, h % 2
            sl = slice(hf * 64, (hf + 1) * 64)
            S.stt(Sm[sl, p, :], Sm[sl, p, :], elm[sl, 2 * p:2 * p + 1],
                  psC[sl, 256 + p * 128 + hf * 64:256 + p * 128 + (hf + 1) * 64], ALU.mult, ALU.add)
        S.cp(Smb[:], Sm[:], eng='act')
        if not fin:
            S.stv(sd['YF'][r0:r1, :], om[:])
        elif need_out:
            yf, zt = yf_r.next(), z_r.next()
            S.ld(yf[:], sd['YF'][r0:r1, :])
            S.ld(zt[:], PJ[r0:r1, 2304:2560])
            S.tt(om[:], om[:], yf[:], ALU.add)
            sq, stt_ = sq2_r.next(), st2_r.next()
            x3 = xc[:, 0:256].rearrange("p (h e) -> p h e", h=4)
            sq3 = sq[:].rearrange("p (h e) -> p h e", h=4)
            S.tt(sq3, x3, DV[:].unsqueeze(2).to_broadcast([64, 4, 64]), ALU.mult, eng='pool')
            S.tt(om[:], om[:], sq[:], ALU.add)
            S.act(zt[:], zt[:], AF.Silu)
            S.tt(om[:], om[:], zt[:], ALU.mult)
            S.tt(sq[:], om[:], om[:], ALU.mult, eng='pool')
            S.reduce(stt_[:, 0:2], sq[:].rearrange("p (g e) -> p g e", g=2), ALU.add)
            S.rstd(stt_[:, 0:2], 1.0 / 128)
            o3 = om[:].rearrange("p (g e) -> p g e", g=2)
            S.tt(o3, o3, stt_[:, 0:2].unsqueeze(2).to_broadcast([64, 2, 128]), ALU.mult)
            S.tt(om[:], om[:], SNW[:], ALU.mult, eng='pool')
            S.stv(sd['YD'][r0:r1, :], om[:])

    wjobs = []
    if dirn == 0 and SPARSE_MOE:
        wr1 = S.ring(3, [128, 8, DFF], BF16)
        wr2 = S.ring(2, [128, 4, D], BF16)
        for e in range(NEXP):
            wjobs.append(('moe_w1', 'W1S', e, wr1))
            wjobs.append(('moe_w3', 'W3S', e, wr1))
            wjobs.append(('moe_w2', 'W2S', e, wr2))
    nsteps = len(order) + 1
    wper = -(-len(wjobs) // max(1, nsteps - 1)) if wjobs else 0

    st1 = None
    st2 = None
    for step in range(len(order) + 2):
        groups = []
        if wjobs:
            S.begin('W')
            for _ in range(wper):
                if not wjobs:
                    break
                src, dst, e, rr = wjobs.pop(0)
                t = rr.next()
                S.ld(t[:], A[src][l, e].rearrange("(k p) n -> p k n", p=128), q='pool')
                S.stv(A[dst][e * 128:(e + 1) * 128, :], t[:].rearrange("p k n -> p (k n)"), q='sp')
            groups.append('W')
        new1 = None
        if step < len(order):
            nm, c = order[step]
            S.begin('A1h')
            th = hA1(nm, c)
            S.begin('A1m')
            tm = mA1(nm, c)
            new1 = (nm, c, th, tm)
            groups += ['A1h', 'A1m']
        new2 = None
        if st1 is not None:
            nm, c, th, tm = st1
            S.begin('A2h')
            th2 = hA2(nm, c, th)
            S.begin('A2m')
            tm2 = mA2(nm, c, tm)
            new2 = (nm, c, th2, tm2)
            groups += ['A2h', 'A2m']
        if st2 is not None:
            nm, c, th, tm = st2
            S.begin('Bh')
            hB(nm, c, th)
            S.begin('Bm')
            mB(nm, c, tm)
            groups += ['Bh', 'Bm']
        S.flush(groups)
        st2 = new2
        st1 = new1
    S.close()


def stage_merge(nc, A, l, sd):
    nm = sd['nm']
    Ts = sd['T']
    S = St(nc, 'mg%d%s' % (l, nm))
    ident = S.sb([128, 128], BF16)
    S.ld(ident[:], A['identb'])
    wbr = S.sb([128, 8, D], BF16)
    S.ld(wbr[:], A['w_branch'][l].rearrange("k (j p) n -> p (k j) n", p=128), q='pool')
    wo = S.sb([128, 8, D], BF16)
    S.ld(wo[:], A['w_out'][l].rearrange("(k p) n -> p k n", p=128), q='pool')
    G1 = load_mod(S, A, l, sd['row'], 2)
    CAW = S.sb([128, 3, 256])
    for j in range(3):
        S.ld(CAW[:, j, :], A['conv_a_w'][l, j].partition_broadcast(128))
    CAB = S.sb([128, 256])
    S.ld(CAB[:], A['conv_a_b'][l].partition_broadcast(128))
    ab_r = S.ring(2, [128, 256])
    u_r = [S.ring(2, [128, 256]) for _ in range(3)]
    yin_r = S.ring(2, [128, 3, 256])
    gt_r = S.ring(2, [128, 4096], BF16)
    x_r = S.ring(2, [128, D])
    cv_r = S.ring(2, [128, 256])
    tmp_r = S.ring(2, [128, 256])
    br_r = S.ring(2, [128, 4, 256], BF16)
    brT_r = S.ring(2, [128, D], BF16)
    mg_r = S.ring(2, [128, D])
    mt_r = S.ring(2, [128, 512])
    mgb_r = S.ring(2, [128, D], BF16)
    mT_r = S.ring(2, [128, D], BF16)
    xo_r = S.ring(2, [128, D])
    pT = S.ps([128, D], BF16)
    pT2 = S.ps([128, D], BF16)
    ppr = S.psring(4)
    for i in range(Ts // 128):
        r0, r1 = i * 128, (i + 1) * 128
        ab = ab_r.next()
        S.ld(ab[:], sd['PJ'][r0:r1, 0:256])
        us = [u_r[j].next() for j in range(3)]
        for j in range(3):
            S.ld(us[j][:], sd['U'][r0 + j:r1 + j, :])
        yin = yin_r.next()
        S.ld(yin[:, 0, :], sd['YB'][r0:r1, :])
        S.ld(yin[:, 1, :], sd['YC'][r0:r1, :])
        S.ld(yin[:, 2, :], sd['YD'][r0:r1, :])
        gt = gt_r.next()
        S.ld(gt[:], sd['GT'][r0:r1, :])
        xt = x_r.next()
        S.ld(xt[:], sd['x'][r0:r1, :])
        cv, tmp = cv_r.next(), tmp_r.next()
        S.tt(cv[:], us[0][:], CAW[:, 0, :], ALU.mult, eng='pool')
        S.tt(tmp[:], us[1][:], CAW[:, 1, :], ALU.mult, eng='pool')
        S.tt(cv[:], cv[:], tmp[:], ALU.add, eng='pool')
        S.tt(tmp[:], us[2][:], CAW[:, 2, :], ALU.mult, eng='pool')
        S.tt(cv[:], cv[:], tmp[:], ALU.add, eng='pool')
        S.tt(cv[:], cv[:], CAB[:], ALU.add, eng='pool')
        br = br_r.next()
        S.tt(br[:, 0, :], cv[:], ab[:], ALU.mult, eng='pool')
        S.cp(br[:, 1:4, :], yin[:], eng='pool')
        for k in range(8):
            S.tr(pT[:, k * 128:(k + 1) * 128], br[:, k // 2, (k % 2) * 128:(k % 2 + 1) * 128], ident[:])
        brT = brT_r.next()
        S.cp(brT[:], pT[:], eng='act')
        mg = mg_r.next()
        for kb in range(4):
            for hf in range(2):
                pp = ppr.next()
                for j in range(2):
                    S.mm(pp[:], brT[:, (2 * kb + j) * 128:(2 * kb + j + 1) * 128], wbr[:, 2 * kb + j, hf * 512:(hf + 1) * 512],
                         start=(j == 0), stop=(j == 1))
                gsl = gt[:, kb * D + hf * 512:kb * D + (hf + 1) * 512]
                if kb == 0:
                    S.tt(mg[:, hf * 512:(hf + 1) * 512], pp[:], gsl, ALU.mult)
                else:
                    mt = mt_r.next()
                    S.tt(mt[:], pp[:], gsl, ALU.mult)
                    S.tt(mg[:, hf * 512:(hf + 1) * 512], mg[:, hf * 512:(hf + 1) * 512], mt[:], ALU.add, eng='pool')
        mgb = mgb_r.next()
        S.cp(mgb[:], mg[:], eng='act')
        for k in range(8):
            S.tr(pT2[:, k * 128:(k + 1) * 128], mgb[:, k * 128:(k + 1) * 128], ident[:])
        mT = mT_r.next()
        S.cp(mT[:], pT2[:], eng='act')
        xo = xo_r.next()
        for hf in range(2):
            pp = ppr.next()
            for k in range(8):
                S.mm(pp[:], mT[:, k * 128:(k + 1) * 128], wo[:, k, hf * 512:(hf + 1) * 512], start=(k == 0), stop=(k == 7))
            mt = mt_r.next()
            S.tt(mt[:], pp[:], G1[:, hf * 512:(hf + 1) * 512], ALU.mult)
            S.tt(xo[:, hf * 512:(hf + 1) * 512], mt[:], xt[:, hf * 512:(hf + 1) * 512], ALU.add, eng='pool')
        S.stv(sd['X1'][r0:r1, :], xo[:])
    S.close()


def stage_moe(nc, A, l, streams, last):
    S = St(nc, 'moe%d' % l)
    identf = S.sb([128, 128])
    S.ld(identf[:], A['identf'])
    wffn = S.sb([128, D])
    S.ld(wffn[:], A['norm_ffn_w'][l].partition_broadcast(128))
    rw = S.sb([128, 8, 36])
    S.ld(rw[:], A['router_w'][l].rearrange("(k p) n -> p k n", p=128))
    rb = S.sb([128, 36])
    S.ld(rb[:], A['router_b'][l].partition_broadcast(128))
    if last:
        fnw = S.sb([128, D])
        S.ld(fnw[:], A['final_norm_w'].partition_broadcast(128))
    mods = {}
    names = ('l',) if last else ('l', 'c')
    for nm in names:
        row = streams[nm]['row']
        mods[nm] = (load_mod(S, A, l, row, 4, wffn), load_mod(S, A, l, row, 3), load_mod(S, A, l, row, 5))
    tiles = []
    for nm in names:
        sd = streams[nm]
        for i in range(sd['T'] // 128):
            tiles.append((nm, i))
    nblk = max(1, -(-len(tiles) // 8))
    per = -(-len(tiles) // nblk)
    blocks = [tiles[i * per:(i + 1) * per] for i in range(nblk)]
    MAXT = per
    fTt = S.sb([128, 8, MAXT * 128], BF16)
    acc = [S.sb([128, D]) for _ in range(MAXT)]
    wg = [S.sb([128, 32]) for _ in range(MAXT)]
    x_r = S.ring(2, [128, D])
    junk = S.sb([128, D])
    ss_r = S.ring(2, [128, 1])
    fn_r = S.ring(1, [128, D])
    fTf_r = S.ring(1, [128, D])
    pTa = S.ps()
    pTb = S.ps()
    pL = S.ps()
    sm_r = S.ring(2, [128, 64])
    w1_r = S.ring(2, [128, 8, DFF], BF16)
    w3_r = S.ring(2, [128, 8, DFF], BF16)
    w2_r = S.ring(2, [128, 4, D], BF16)
    gact_r = S.ring(2, [128, 4, 512], BF16)
    sil_r = S.ring(2, [128, 512])
    ph_r = S.psring(2)
    ph3_r = S.psring(2)
    po_r = S.psring(1)
    xo_r = S.ring(2, [128, D])
    for blk in blocks:
        nt = len(blk)
        for ti, (nm, i) in enumerate(blk):
            sd = streams[nm]
            A2, B2, G2 = mods[nm]
            r0, r1 = i * 128, (i + 1) * 128
            xt, ss, fn = x_r.next(), ss_r.next(), fn_r.next()
            S.ld(xt[:], sd['X1'][r0:r1, :])
            S.act(junk[:], xt[:], AF.Square, accum=ss[:])
            S.rstd(ss[:], 1.0 / D)
            S.stt(fn[:], xt[:], ss[:], A2[:], ALU.mult, ALU.mult)
            S.tt(fn[:], fn[:], B2[:], ALU.add)
            for k in range(8):
                pt = pTa if k < 4 else pTb
                S.tr(pt[:, (k % 4) * 128:(k % 4 + 1) * 128], fn[:, k * 128:(k + 1) * 128], identf[:])
            fTf = fTf_r.next()
            S.cp(fTf[:, 0:512], pTa[:], eng='act')
            S.cp(fTf[:, 512:1024], pTb[:], eng='dve')
            S.cp(fTt[:, :, ti * 128:(ti + 1) * 128], fTf[:].rearrange("p (k t) -> p k t", k=8), eng='pool')
            for k in range(8):
                S.mm(pL[:, 0:36], fTf[:, k * 128:(k + 1) * 128], rw[:, k, :], start=(k == 0), stop=(k == 7))
            sm = sm_r.next()
            lg = sm[:, 0:36]
            S.tt(lg, pL[:, 0:36], rb[:], ALU.add)
            gmax, gsel, gex, gsum = sm[:, 36:37], sm[:, 37:41], sm[:, 41:45], sm[:, 45:46]
            S.reduce(gmax, lg[:, 0:4], ALU.max)
            S.ts(gsel, lg[:, 0:4], gmax, None, ALU.is_equal)
            S.ts(gex, lg[:, 0:4], gmax, None, ALU.subtract)
            S.act(gex, gex, AF.Exp)
            S.reduce(gsum, gex, ALU.add)
            S.recip(gsum, gsum)
            ein, m1, sel1, e2, m2, sel2 = sm[:, 46:54], sm[:, 54:55], sm[:, 56:64], sm[:, 46:54], sm[:, 55:56], sm[:, 46:54]
            S.ts(ein, lg[:, 4:12], gsel[:, 0:1], None, ALU.mult)
            for g in range(1, 4):
                S.stt(ein, lg[:, 4 + 8 * g:12 + 8 * g], gsel[:, g:g + 1], ein, ALU.mult, ALU.add)
            S.reduce(m1, ein, ALU.max)
            S.ts(sel1, ein, m1, None, ALU.is_equal)
            S.stt(e2, sel1, -1e30, ein, ALU.mult, ALU.add)
            S.reduce(m2, e2, ALU.max)
            S.ts(sel2, e2, m2, None, ALU.is_equal)
            wa, wb_ = sm[:, 36:37], sm[:, 41:42]
            S.tt(wa, m2, m1, ALU.subtract)
            S.act(wa, wa, AF.Exp)
            S.ts(wa, wa, 1.0, None, ALU.add)
            S.recip(wa, wa)
            S.ts(wb_, wa, -1.0, 1.0, ALU.mult, ALU.add)
            S.tt(wa, wa, gsum, ALU.mult)
            S.tt(wb_, wb_, gsum, ALU.mult)
            S.ts(sel1, sel1, wa, None, ALU.mult)
            S.stt(sel1, sel2, wb_, sel1, ALU.mult, ALU.add)
            for g in range(4):
                S.ts(wg[ti][:, 8 * g:8 * g + 8], sel1, gsel[:, g:g + 1], None, ALU.mult)
            S.memset(acc[ti][:], 0.0, eng='pool')
        sbs = [list(range(s, min(s + 4, nt))) for s in range(0, nt, 4)]
        for e in range(NEXP):
            w1, w3, w2 = w1_r.next(), w3_r.next(), w2_r.next()
            S.ld(w1[:], A['moe_w1'][l, e].rearrange("(k p) n -> p k n", p=128), q='pool')
            S.ld(w3[:], A['moe_w3'][l, e].rearrange("(k p) n -> p k n", p=128), q='pool')
            S.ld(w2[:], A['moe_w2'][l, e].rearrange("(k p) n -> p k n", p=128), q='pool')
            for si, sb_ in enumerate(sbs):
                n = len(sb_) * 128
                c0 = sb_[0] * 128
                ga = gact_r.next()
                for fc in range(4):
                    ph, ph3 = ph_r.next(), ph3_r.next()
                    for k in range(8):
                        S.mm(ph[:, 0:n], w1[:, k, fc * 128:(fc + 1) * 128], fTt[:, k, c0:c0 + n],
                             start=(k == 0), stop=(k == 7))
                    for k in range(8):
                        S.mm(ph3[:, 0:n], w3[:, k, fc * 128:(fc + 1) * 128], fTt[:, k, c0:c0 + n],
                             start=(k == 0), stop=(k == 7))
                    sil = sil_r.next()
                    S.act(sil[:, 0:n], ph[:, 0:n], AF.Silu)
                    S.tt(ga[:, fc, 0:n], ph3[:, 0:n], sil[:, 0:n], ALU.mult)
                for tj, ti in enumerate(sb_):
                    for hf in range(2):
                        po = po_r.next()
                        for fc in range(4):
                            S.mm(po[:], ga[:, fc, tj * 128:(tj + 1) * 128], w2[:, fc, hf * 512:(hf + 1) * 512],
                                 start=(fc == 0), stop=(fc == 3))
                        S.stt(acc[ti][:, hf * 512:(hf + 1) * 512], po[:], wg[ti][:, e:e + 1], acc[ti][:, hf * 512:(hf + 1) * 512],
                              ALU.mult, ALU.add)
        for ti, (nm, i) in enumerate(blk):
            sd = streams[nm]
            A2, B2, G2 = mods[nm]
            r0, r1 = i * 128, (i + 1) * 128
            xt, xo = x_r.next(), xo_r.next()
            S.ld(xt[:], sd['X1'][r0:r1, :])
            S.tt(acc[ti][:], acc[ti][:], G2[:], ALU.mult, eng='pool')
            S.tt(xo[:], acc[ti][:], xt[:], ALU.add, eng='pool')
            if last:
                ss = ss_r.next()
                S.act(junk[:], xo[:], AF.Square, accum=ss[:])
                S.rstd(ss[:], 1.0 / D)
                S.stt(xo[:], xo[:], ss[:], fnw[:], ALU.mult, ALU.mult)
                S.stv(A['out'][r0:r1, :], xo[:])
            else:
                S.stv(sd['X2'][r0:r1, :], xo[:])
    S.close()


def moe_tiles(streams, last):
    names = ('l',) if last else ('l', 'c')
    tiles = []
    for nm in names:
        for i in range(streams[nm]['T'] // 128):
            tiles.append((nm, i))
    return names, tiles


def stage_moe_prep(nc, A, l):
    S = St(nc, 'mp%d' % l)
    r1 = S.ring(3, [128, 8, DFF], BF16)
    r2 = S.ring(2, [128, 4, D], BF16)
    for e in range(NEXP):
        for src, dst in (('moe_w1', 'W1S'), ('moe_w3', 'W3S')):
            t = r1.next()
            S.ld(t[:], A[src][l, e].rearrange("(k p) n -> p k n", p=128), q='pool')
            S.stv(A[dst][e * 128:(e + 1) * 128, :], t[:].rearrange("p k n -> p (k n)"), q='sp')
        t = r2.next()
        S.ld(t[:], A['moe_w2'][l, e].rearrange("(k p) n -> p k n", p=128), q='pool')
        S.stv(A['W2S'][e * 128:(e + 1) * 128, :], t[:].rearrange("p k n -> p (k n)"), q='sp')
    S.close()


def stage_moe_route(nc, A, l, streams, last, pers, NBM):
    S = St(nc, 'mr%d' % l)
    names, tiles = moe_tiles(streams, last)
    NB = 2 * len(tiles) + NEXP
    identf = S.sb([128, 128])
    S.ld(identf[:], A['identf'])
    moec = S.sb([128, 321 + NBM])
    S.ld(moec[:], A['moec'])
    ONES = moec[:, 0:128]
    TRIX = moec[0:32, 257 + NBM:289 + NBM]
    TRII = moec[0:32, 289 + NBM:321 + NBM]
    wffn = S.sb([128, D])
    S.ld(wffn[:], A['norm_ffn_w'][l].partition_broadcast(128))
    rw = S.sb([128, 8, 36])
    S.ld(rw[:], A['router_w'][l].rearrange("(k p) n -> p k n", p=128))
    rb = S.sb([128, 36])
    S.ld(rb[:], A['router_b'][l].partition_broadcast(128))
    mods = {}
    for nm in names:
        row = streams[nm]['row']
        mods[nm] = (load_mod(S, A, l, row, 4, wffn), load_mod(S, A, l, row, 3))
    OHT, WTT, PADB = pers['OHT'], pers['WTT'], pers['PADB']
    zt = S.sb([128, D], BF16)
    S.memset(zt[:], 0.0)
    for j in range(NB):
        S.stv(A['FB'][j * 128:(j + 1) * 128, :], zt[:], q='sp')
    x_r = S.ring(2, [128, D])
    junk = S.sb([128, D])
    ss_r = S.ring(2, [128, 1])
    fn_r = S.ring(2, [128, D])
    fb_r = S.ring(2, [128, D], BF16)
    fTf_r = S.ring(2, [128, D])
    pTa, pTb, pL = S.ps(), S.ps(), S.ps()
    pCnt = S.ps()
    sm_r = S.ring(2, [128, 80])
    ohs_r = S.ring(2, [128, 32])
    off = 0
    for ti, (nm, i) in enumerate(tiles):
        sd = streams[nm]
        A2, B2 = mods[nm]
        r0, r1 = i * 128, (i + 1) * 128
        xt, ss, fn, fb = x_r.next(), ss_r.next(), fn_r.next(), fb_r.next()
        S.ld(xt[:], sd['X1'][r0:r1, :])
        S.act(junk[:], xt[:], AF.Square, accum=ss[:])
        S.rstd(ss[:], 1.0 / D)
        S.stt(fn[:], xt[:], ss[:], A2[:], ALU.mult, ALU.mult)
        S.tt(fn[:], fn[:], B2[:], ALU.add)
        S.cp(fb[:], fn[:], eng='pool')
        S.stv(A['FN'][ti * 128:(ti + 1) * 128, :], fb[:], q='sp')
        for k in range(8):
            pt = pTa if k < 4 else pTb
            S.tr(pt[:, (k % 4) * 128:(k % 4 + 1) * 128], fn[:, k * 128:(k + 1) * 128], identf[:])
        fTf = fTf_r.next()
        S.cp(fTf[:, 0:512], pTa[:], eng='act')
        S.cp(fTf[:, 512:1024], pTb[:], eng='dve')
        for k in range(8):
            S.mm(pL[:, 0:36], fTf[:, k * 128:(k + 1) * 128], rw[:, k, :], start=(k == 0), stop=(k == 7))
        sm = sm_r.next()
        lg = sm[:, 0:36]
        S.tt(lg, pL[:, 0:36], rb[:], ALU.add)
        gmax, gsel, gex, gsum = sm[:, 36:37], sm[:, 37:41], sm[:, 41:45], sm[:, 45:46]
        S.reduce(gmax, lg[:, 0:4], ALU.max)
        S.ts(gsel, lg[:, 0:4], gmax, None, ALU.is_equal)
        S.ts(gex, lg[:, 0:4], gmax, None, ALU.subtract)
        S.act(gex, gex, AF.Exp)
        S.reduce(gsum, gex, ALU.add)
        S.recip(gsum, gsum)
        ein, m1, m2 = sm[:, 46:54], sm[:, 54:55], sm[:, 55:56]
        sel1, e2, sel2 = sm[:, 56:64], sm[:, 64:72], sm[:, 72:80]
        S.ts(ein, lg[:, 4:12], gsel[:, 0:1], None, ALU.mult)
        for g in range(1, 4):
            S.stt(ein, lg[:, 4 + 8 * g:12 + 8 * g], gsel[:, g:g + 1], ein, ALU.mult, ALU.add)
        S.reduce(m1, ein, ALU.max)
        S.ts(sel1, ein, m1, None, ALU.is_equal)
        S.stt(e2, sel1, -1e30, ein, ALU.mult, ALU.add)
        S.reduce(m2, e2, ALU.max)
        S.ts(sel2, e2, m2, None, ALU.is_equal)
        wa, wb_ = sm[:, 36:37], sm[:, 41:42]
        S.tt(wa, m2, m1, ALU.subtract)
        S.act(wa, wa, AF.Exp)
        S.ts(wa, wa, 1.0, None, ALU.add)
        S.recip(wa, wa)
        S.ts(wb_, wa, -1.0, 1.0, ALU.mult, ALU.add)
        S.tt(WTT[:, ti, 0:1], wa, gsum, ALU.mult)
        S.tt(WTT[:, ti, 1:2], wb_, gsum, ALU.mult)
        for g in range(4):
            S.ts(OHT[:, ti, 8 * g:8 * g + 8], sel1, gsel[:, g:g + 1], None, ALU.mult)
            S.ts(OHT[:, ti, 32 + 8 * g:40 + 8 * g], sel2, gsel[:, g:g + 1], None, ALU.mult)
        ohs = ohs_r.next()
        S.tt(ohs[:], OHT[:, ti, 0:32], OHT[:, ti, 32:64], ALU.add)
        S.mm(pCnt[0:32, 0:2], ohs[:], ONES[:, 0:2], start=(ti == 0), stop=(ti == len(tiles) - 1))
    I32 = mybir.dt.int32
    cf = S.sb([32, 4])
    ci = S.sb([32, 2], I32)
    S.ts(cf[:, 0:1], pCnt[0:32, 0:1], 127.0, None, ALU.add)
    S.cp(ci[:, 0:1], cf[:, 0:1])
    S.op('dve', lambda e: e.tensor_scalar(out=ci[:, 1:2], in0=ci[:, 0:1], scalar1=7, scalar2=7,
                                          op0=ALU.arith_shift_right, op1=ALU.logical_shift_left), [ci[:, 0:1]], [ci[:, 1:2]])
    S.cp(cf[:, 1:2], ci[:, 1:2])
    pbc = S.sb([32, 128])
    S.ts(pbc[:], ONES[0:32, :], cf[:, 1:2], None, ALU.mult)
    pP = S.ps()
    S.mm(pP[:, 0:32], pbc[:], TRIX)
    S.mm(pP[:, 32:64], pbc[:], TRII)
    S.cp(PADB[:], pP[:, 0:64])
    S.close()


def stage_moe_disp(nc, A, l, streams, last, pers, NBM):
    S = St(nc, 'md%d' % l)
    names, tiles = moe_tiles(streams, last)
    NB = 2 * len(tiles) + NEXP
    I32 = mybir.dt.int32
    moec = S.sb([128, 321 + NBM])
    S.ld(moec[:], A['moec'])
    ONES = moec[:, 0:128]
    STRICT = moec[:, 128:256]
    JROW = moec[:, 256:256 + NBM]
    PIDX = moec[:, 256 + NBM:257 + NBM]
    OHT, DEST, IDXW, PADB = pers['OHT'], pers['DEST'], pers['IDXW'], pers['PADB']
    accp = S.sb([128, 32])
    S.memset(accp[:], 0.0)
    ohs_r = S.ring(2, [128, 32])
    tmp_r = S.ring(2, [128, 32])
    t2_r = S.ring(2, [128, 32])
    df_r = S.ring(2, [128, 2])
    fb_r = S.ring(3, [128, D], BF16)
    pr = S.psring(2)
    for ti in range(len(tiles)):
        ohs, tmp, t2, df, fb = ohs_r.next(), tmp_r.next(), t2_r.next(), df_r.next(), fb_r.next()
        pp = pr.next()
        S.tt(ohs[:], OHT[:, ti, 0:32], OHT[:, ti, 32:64], ALU.add)
        S.mm(pp[:, 0:32], ONES, accp[:], start=True, stop=False)
        S.mm(pp[:, 0:32], STRICT, ohs[:], start=False, stop=True)
        S.tt(tmp[:], pp[:, 0:32], PADB[:, 0:32], ALU.add)
        S.tt(accp[:], accp[:], ohs[:], ALU.add, eng='pool')
        for k in range(2):
            S.tt(t2[:], tmp[:], OHT[:, ti, 32 * k:32 * k + 32], ALU.mult)
            S.reduce(df[:, k:k + 1], t2[:], ALU.add)
        S.cp(DEST[:, ti, :], df[:])
        S.ld(fb[:], A['FN'][ti * 128:(ti + 1) * 128, :])
        for k in range(2):
            S.op('pool', lambda e, ti=ti, k=k, fb=fb: e.indirect_dma_start(
                out=A['FB'], out_offset=bass.IndirectOffsetOnAxis(ap=DEST[:, ti, k:k + 1], axis=0),
                in_=fb[:], in_offset=None), [fb[:], DEST[:]], [], dma=True)
    be = S.sb([128, NBM])
    S.memset(be[:], 0.0)
    for e in range(NEXP):
        S.stt(be[:], JROW, PADB[:, 32 + e:33 + e], be[:], ALU.is_ge, ALU.add)
    S.ts(be[:], be[:], 31.0, 128.0, ALU.min, ALU.mult)
    S.ts(be[:], be[:], PIDX, None, ALU.add)
    S.cp(IDXW[:], be[:])
    S.close()


def stage_moe_exp(nc, A, l, streams, last, pers, NBM):
    S = St(nc, 'me%d' % l)
    names, tiles = moe_tiles(streams, last)
    NB = 2 * len(tiles) + NEXP
    IDXW = pers['IDXW']
    ident = S.sb([128, 128], BF16)
    S.ld(ident[:], A['identb'])
    w1_r = S.ring(3, [128, 8 * DFF], BF16)
    w3_r = S.ring(3, [128, 8 * DFF], BF16)
    w2_r = S.ring(3, [128, 4 * D], BF16)
    xb_r = S.ring(3, [128, D], BF16)
    xT_r = S.ring(2, [128, D], BF16)
    sil_r = S.ring(2, [128, DFF])
    g_r = S.ring(2, [128, DFF], BF16)
    gT_r = S.ring(2, [128, DFF], BF16)
    yb_r = S.ring(2, [128, D])
    pT = S.ps([128, D], BF16)
    pT2 = S.ps([128, D], BF16)
    ph_r, ph3_r, po_r = S.psring(2), S.psring(2), S.psring(2)
    for j in range(NB):
        w1, w3, w2 = w1_r.next(), w3_r.next(), w2_r.next()
        for wt, nm in ((w1, 'W1S'), (w3, 'W3S'), (w2, 'W2S')):
            S.op('pool', lambda e, wt=wt, nm=nm, j=j: e.indirect_dma_start(
                out=wt[:], out_offset=None, in_=A[nm],
                in_offset=bass.IndirectOffsetOnAxis(ap=IDXW[:, j:j + 1], axis=0)), [IDXW[:]], [wt[:]], dma=True)
        xb = xb_r.next()
        S.ld(xb[:], A['FB'][j * 128:(j + 1) * 128, :])
        for k in range(8):
            S.tr(pT[:, k * 128:(k + 1) * 128], xb[:, k * 128:(k + 1) * 128], ident[:])
        xT = xT_r.next()
        S.cp(xT[:], pT[:])
        ph, ph3 = ph_r.next(), ph3_r.next()
        for k in range(8):
            S.mm(ph[:], xT[:, k * 128:(k + 1) * 128], w1[:, k * DFF:(k + 1) * DFF], start=(k == 0), stop=(k == 7))
        for k in range(8):
            S.mm(ph3[:], xT[:, k * 128:(k + 1) * 128], w3[:, k * DFF:(k + 1) * DFF], start=(k == 0), stop=(k == 7))
        sil, g = sil_r.next(), g_r.next()
        S.act(sil[:], ph[:], AF.Silu)
        S.tt(g[:], ph3[:], sil[:], ALU.mult)
        for fc in range(4):
            S.tr(pT2[:, fc * 128:(fc + 1) * 128], g[:, fc * 128:(fc + 1) * 128], ident[:])
        gT = gT_r.next()
        S.cp(gT[:], pT2[:, 0:512])
        yb = yb_r.next()
        for hf in range(2):
            po = po_r.next()
            for fc in range(4):
                S.mm(po[:], gT[:, fc * 128:(fc + 1) * 128], w2[:, fc * D + hf * 512:fc * D + (hf + 1) * 512],
                     start=(fc == 0), stop=(fc == 3))
            S.cp(yb[:, hf * 512:(hf + 1) * 512], po[:], eng=('act' if hf == 0 else 'dve'))
        S.stv(A['YBUF'][j * 128:(j + 1) * 128, :], yb[:], q='sp')
    S.close()


def stage_moe_comb(nc, A, l, streams, last, pers):
    S = St(nc, 'mc%d' % l)
    names, tiles = moe_tiles(streams, last)
    DEST, WTT = pers['DEST'], pers['WTT']
    G2 = {nm: load_mod(S, A, l, streams[nm]['row'], 5) for nm in names}
    if last:
        fnw = S.sb([128, D])
        S.ld(fnw[:], A['final_norm_w'].partition_broadcast(128))
    x_r = S.ring(2, [128, D])
    y0_r, y1_r = S.ring(2, [128, D]), S.ring(2, [128, D])
    xo_r = S.ring(2, [128, D])
    junk = S.sb([128, D])
    ss_r = S.ring(2, [128, 1])
    for ti, (nm, i) in enumerate(tiles):
        sd = streams[nm]
        r0, r1 = i * 128, (i + 1) * 128
        xt, y0, y1, xo = x_r.next(), y0_r.next(), y1_r.next(), xo_r.next()
        S.ld(xt[:], sd['X1'][r0:r1, :])
        for k, yt in ((0, y0), (1, y1)):
            S.op('pool', lambda e, ti=ti, k=k, yt=yt: e.indirect_dma_start(
                out=yt[:], out_offset=None, in_=A['YBUF'],
                in_offset=bass.IndirectOffsetOnAxis(ap=DEST[:, ti, k:k + 1], axis=0)), [DEST[:]], [yt[:]], dma=True)
        S.ts(y0[:], y0[:], WTT[:, ti, 0:1], None, ALU.mult)
        S.stt(y0[:], y1[:], WTT[:, ti, 1:2], y0[:], ALU.mult, ALU.add)
        S.tt(y0[:], y0[:], G2[nm][:], ALU.mult, eng='pool')
        S.tt(xo[:], y0[:], xt[:], ALU.add, eng='pool')
        if last:
            ss = ss_r.next()
            S.act(junk[:], xo[:], AF.Square, accum=ss[:])
            S.rstd(ss[:], 1.0 / D)
            S.stt(xo[:], xo[:], ss[:], fnw[:], ALU.mult, ALU.mult)
            S.stv(A['out'][r0:r1, :], xo[:], q='sp')
        else:
            S.stv(sd['X2'][r0:r1, :], xo[:], q='sp')
    S.close()


def prep_inputs(inputs, b, consts):
    f = lambda a: np.ascontiguousarray(np.asarray(a, dtype=np.float32))
    m = {}
    m['x'] = f(inputs['x'][b])
    m['ctx'] = f(inputs['ctx'][b])
    cc = np.stack([f(inputs['c'][b]).reshape(8, 128).T, f(inputs['c_ctx']).reshape(8, 128).T], axis=-1)
    m['cc'] = np.ascontiguousarray(cc.reshape(128, 16))
    for k in ('ada_w', 'ada_b', 'norm_mix_w', 'norm_ffn_w', 'w_in', 'conv_a_w', 'conv_a_b', 'hgrn_lb', 'hgrn_norm_w',
              'ssm_conv_w', 'ssm_conv_b', 'ssm_A_log', 'ssm_dt_bias', 'ssm_D', 'ssm_norm_w', 'w_branch', 'w_out',
              'moe_w1', 'moe_w3', 'moe_w2', 'final_norm_w'):
        m[k] = f(inputs[k])
    m['router_w'] = np.ascontiguousarray(np.concatenate([f(inputs['router_group_w']), f(inputs['router_expert_w'])], axis=-1))
    m['router_b'] = np.ascontiguousarray(np.concatenate([f(inputs['router_group_b']), f(inputs['router_expert_b'])], axis=-1))
    m.update(consts)
    return m


_CACHE = {}


def kernel(**inputs):
    x = np.asarray(inputs['x'])
    B, T, _ = x.shape
    if T not in _CACHE:
        _CACHE[T] = build_program(T)
    nc, consts = _CACHE[T]
    ncores = 8
    in_maps = [prep_inputs(inputs, c % B, consts) for c in range(ncores)]
    res = run_bass_kernel_spmd(nc, in_maps, core_ids=list(range(ncores)))
    out = np.stack([np.asarray(res.results[b]['out']) for b in range(B)], axis=0)
    return out.astype(np.float32)
```

```python
import contextlib
import numpy as np
import ml_dtypes
import concourse.bass as bass
import concourse.mybir as mybir
from concourse.bass_utils import run_bass_kernel_spmd

F32 = mybir.dt.float32
BF16 = mybir.dt.bfloat16
AF = mybir.ActivationFunctionType
ALU = mybir.AluOpType
AX = mybir.AxisListType

COMPUTE = ('pe', 'act', 'dve', 'pool')
ALLENG = ('pe', 'act', 'dve', 'pool', 'sp')
NDMASEM = 12

D = 1024
NIN = 7176
NPJ = 3080
TC = 256
DEPTH = 2
EPS = 1e-6
NEXP = 32
DFF = 512
SPARSE_MOE = True


_UID = [0]


class Op:
    __slots__ = ('eng', 'idx', 'fn', 'waits', 'signal', 'dma', 'dsem', 'dval', 'vc', 'dwait')


class Prog:
    def __init__(self, nc):
        self.nc = nc
        self.ops = {e: [] for e in ALLENG}
        self.lastw = {}
        self.readers = {}
        self.vc = {e: {f: -1 for f in COMPUTE} for e in ALLENG}
        self.selfk = {e: -1 for e in ALLENG}
        self.dma_known = {e: set() for e in ALLENG}
        self.dma_cnt = {e: 0 for e in ALLENG}
        self.dma_ops = {e: [] for e in ALLENG}

    def add(self, eng, fn, reads=(), writes=(), dma=False):
        op = Op()
        op.eng = eng
        op.fn = fn
        op.idx = len(self.ops[eng])
        op.signal = False
        op.dma = dma
        op.waits = []
        op.dwait = None
        op.dsem = None
        op.dval = None
        deps = []
        for r in reads:
            w = self.lastw.get(r)
            if w is not None:
                deps.append((w, True))
        for w_ in writes:
            w = self.lastw.get(w_)
            if w is not None:
                deps.append((w, False))
            for rd in self.readers.get(w_, ()):
                deps.append((rd, False))
        vc = self.vc[eng]
        for d, raw in deps:
            if d is op:
                continue
            if d.dma:
                if id(d) in self.dma_known[eng]:
                    continue
                self.dma_known[eng].add(id(d))
                op.waits.append(d)
            elif d.eng == eng:
                if not dma:
                    if eng == 'pe':
                        continue
                if self.selfk[eng] >= d.idx:
                    continue
                self.selfk[eng] = d.idx
                d.signal = True
                op.waits.append(d)
            else:
                if vc[d.eng] >= d.idx:
                    continue
                d.signal = True
                op.waits.append(d)
                for f in COMPUTE:
                    if d.vc[f] > vc[f]:
                        vc[f] = d.vc[f]
        if dma:
            i = self.dma_cnt[eng]
            self.dma_cnt[eng] += 1
            op.dsem = i % NDMASEM
            op.dval = 16 * (i // NDMASEM + 1)
            if i >= NDMASEM:
                op.dwait = (op.dsem, 16 * (i // NDMASEM))
                prev = self.dma_ops[eng][i - NDMASEM]
                self.dma_known[eng].add(id(prev))
            self.dma_ops[eng].append(op)
            op.vc = None
        else:
            if eng in COMPUTE:
                vc[eng] = op.idx
            op.vc = {f: vc[f] for f in COMPUTE}
        for r in reads:
            self.readers.setdefault(r, []).append(op)
        for w_ in writes:
            self.lastw[w_] = op
            self.readers[w_] = []
        self.ops[eng].append(op)
        return op

    def emit(self):
        nc = self.nc
        with contextlib.ExitStack() as st:
            _UID[0] += 1
            u = _UID[0]
            esem = {e: nc.alloc_semaphore('s%d_%s' % (u, e)) for e in COMPUTE}
            dsem = {e: [nc.alloc_semaphore('d%d_%s_%d' % (u, e, i)) for i in range(NDMASEM)]
                    for e in ALLENG if self.dma_cnt[e] > 0}
            self.allsems = list(esem.values()) + [x for v in dsem.values() for x in v]
            cnts = {}
            for e in COMPUTE:
                c = 0
                for op in self.ops[e]:
                    if op.signal and not op.dma:
                        c += 1
                    cnts[id(op)] = c
            block = st.enter_context(nc.Block())
            final = []
            for e in ALLENG:
                for i in range(min(NDMASEM, self.dma_cnt[e])):
                    n = (self.dma_cnt[e] - 1 - i) // NDMASEM + 1
                    final.append((dsem[e][i], 16 * n))

            def run(e, eng):
                for op in self.ops[e]:
                    if op.dwait is not None:
                        eng.wait_ge(dsem[e][op.dwait[0]], op.dwait[1])
                    for d in op.waits:
                        if d.dma:
                            eng.wait_ge(dsem[d.eng][d.dsem], d.dval)
                        else:
                            eng.wait_ge(esem[d.eng], cnts[id(d)])
                    ins = op.fn(eng)
                    if op.dma:
                        ins.then_inc(dsem[e][op.dsem], 16)
                    elif op.signal:
                        ins.then_inc(esem[e], 1)
                if e == 'sp':
                    for s, v in final:
                        eng.wait_ge(s, v)

            @block.tensor
            def _(eng):
                run('pe', eng)

            @block.scalar
            def _(eng):
                run('act', eng)

            @block.vector
            def _(eng):
                run('dve', eng)

            @block.gpsimd
            def _(eng):
                run('pool', eng)

            @block.sync
            def _(eng):
                run('sp', eng)


class Ring:
    def __init__(self, tiles):
        self.tiles = tiles
        self.i = 0

    def next(self):
        t = self.tiles[self.i % len(self.tiles)]
        self.i += 1
        return t


def _keys(aps):
    out = []
    for a in aps:
        if a is None or isinstance(a, (float, int)):
            continue
        out.append(a if isinstance(a, (str, tuple)) else a.name)
    return out


class St:
    def __init__(self, nc, tag):
        self.nc = nc
        self.P = Prog(nc)
        self.es = contextlib.ExitStack()
        self.tag = tag

    def sb(self, shape, dt=F32):
        _UID[0] += 1
        return self.es.enter_context(self.nc.sbuf_tensor('%s_s%d' % (self.tag, _UID[0]), list(shape), dt))

    def ps(self, shape=(128, 512), dt=F32):
        _UID[0] += 1
        nm = '%s_p%d' % (self.tag, _UID[0])
        if not hasattr(self, 'psnames'):
            self.psnames = set()
        self.psnames.add(nm)
        return self.es.enter_context(self.nc.psum_tensor(nm, list(shape), dt))

    def ring(self, n, shape, dt=F32):
        return Ring([self.sb(shape, dt) for _ in range(n)])

    def psring(self, n, shape=(128, 512), dt=F32):
        return Ring([self.ps(shape, dt) for _ in range(n)])

    def close(self):
        self.P.emit()
        self.es.close()
        self.nc.all_engine_barrier()
        self.nc.clear_and_free_semaphores(self.P.allsems)
        self.nc.all_engine_barrier()

    def begin(self, name):
        if not hasattr(self, 'groups'):
            self.groups = {}
        self.grp = name
        self.groups[name] = []

    def flush(self, names):
        items = []
        for gi, nm in enumerate(names):
            lst = self.groups.pop(nm)
            n = len(lst)
            for k, it in enumerate(lst):
                items.append(((k + 0.5) / n, gi, k, it))
        items.sort(key=lambda x: (x[0], x[1], x[2]))
        self.grp = None
        for _, _, _, (eng, fn, r, w, dma) in items:
            self._add(eng, fn, r, w, dma)

    def op(self, eng, fn, ins, outs, dma=False):
        if getattr(self, 'grp', None) is not None:
            self.groups[self.grp].append((eng, fn, _keys(ins), _keys(outs), dma))
            return
        self._add(eng, fn, _keys(ins), _keys(outs), dma)

    def _add(self, eng, fn, r, w, dma):
        import os
        lim = os.environ.get('OPLIMIT_' + self.tag)
        self.nop_ = getattr(self, 'nop_', 0) + 1
        if lim is not None and self.nop_ > int(lim):
            return
        psn = getattr(self, 'psnames', ())
        w = list(w) + [k for k in r if k in psn and k not in w]
        self.P.add(eng, fn, reads=r, writes=w, dma=dma)

    def mm(self, out, lhsT, rhs, start=True, stop=True):
        self.op('pe', lambda e: e.matmul(out, lhsT, rhs, start=start, stop=stop), [lhsT, rhs], [out])

    def tr(self, out, in_, ident):
        self.op('pe', lambda e: e.transpose(out, in_, ident), [in_, ident], [out])

    def act(self, out, in_, func, bias=None, scale=None, accum=None, eng='act'):
        kw = {}
        if bias is not None:
            kw['bias'] = bias
        if scale is not None:
            kw['scale'] = scale
        if accum is not None:
            kw['accum_out'] = accum
        self.op(eng, lambda e: e.activation(out=out, in_=in_, func=func, **kw),
                [in_, bias, scale], [out, accum])

    def tt(self, out, a, b, op, eng='dve'):
        self.op(eng, lambda e: e.tensor_tensor(out=out, in0=a, in1=b, op=op), [a, b], [out])

    def ts(self, out, a, s1, s2, op0, op1=None, eng='dve'):
        if op1 is None:
            self.op(eng, lambda e: e.tensor_scalar(out=out, in0=a, scalar1=s1, scalar2=None, op0=op0),
                    [a, s1], [out])
        else:
            self.op(eng, lambda e: e.tensor_scalar(out=out, in0=a, scalar1=s1, scalar2=s2, op0=op0, op1=op1),
                    [a, s1, s2], [out])

    def stt(self, out, in0, scalar, in1, op0, op1):
        self.op('dve', lambda e: e.scalar_tensor_tensor(out=out, in0=in0, scalar=scalar, in1=in1, op0=op0, op1=op1),
                [in0, scalar, in1], [out])

    def cp(self, out, in_, eng='dve'):
        if eng == 'act':
            self.op('act', lambda e: e.copy(out=out, in_=in_), [in_], [out])
        else:
            self.op(eng, lambda e: e.tensor_copy(out=out, in_=in_), [in_], [out])

    def recip(self, out, in_):
        self.op('dve', lambda e: e.reciprocal(out=out, in_=in_), [in_], [out])

    def memset(self, out, v, eng='dve'):
        self.op(eng, lambda e: e.memset(out, v), [], [out])

    def reduce(self, out, in_, op, eng='dve'):
        self.op(eng, lambda e: e.tensor_reduce(out=out, in_=in_, axis=AX.X, op=op), [in_], [out])

    def ld(self, out, in_, q='sp', slow=False):
        if slow:
            self.op(q, lambda e: e.dma_start(out=out, in_=in_, allow_slow_non_contiguous=True), [], [out], dma=True)
        else:
            self.op(q, lambda e: e.dma_start(out=out, in_=in_), [], [out], dma=True)

    def stv(self, out, in_, q='pool'):
        self.op(q, lambda e: e.dma_start(out=out, in_=in_), [in_], [], dma=True)

    def rstd(self, ss, scale):
        self.ts(ss, ss, scale, EPS, ALU.mult, ALU.add)
        self.act(ss, ss, AF.Sqrt)
        self.recip(ss, ss)


def make_consts(T):
    c = {}
    L = 64
    mid = 31
    s = np.arange(L)[:, None]
    t = np.arange(L)[None, :]
    tabs = []
    half = lambda a: (a >= 32)
    for dirn in range(2):
        if dirn == 0:
            midt = np.where(t < 32, 15, 47)
            M1 = (s <= t).astype(np.float32) - (s <= midt).astype(np.float32)
            M2 = (s <= t).astype(np.float32)
            M3 = (s > t).astype(np.float32)
            TRI = (s <= t).astype(np.float32)
            NTRI = -(s <= t).astype(np.float32)
            MASK = (s <= t).astype(np.float32)
            MASKD = ((s <= t) & (half(s) == half(t))).astype(np.float32)
            MASKO = ((s <= 31) & (t >= 32)).astype(np.float32)
            M4 = np.where(t >= 32, (s >= 32) & (s <= t), (s >= t + 1) & (s <= 31)).astype(np.float32)
        else:
            midt = np.where(t < 32, 16, 48)
            M1 = -((s < t).astype(np.float32) - (s < midt).astype(np.float32))
            M2 = (s >= t).astype(np.float32)
            M3 = (s < t).astype(np.float32)
            TRI = -(s < t).astype(np.float32)
            NTRI = (s < t).astype(np.float32)
            MASK = (s >= t).astype(np.float32)
            MASKD = ((s >= t) & (half(s) == half(t))).astype(np.float32)
            MASKO = ((s >= 32) & (t <= 31)).astype(np.float32)
            M4 = np.where(t <= 31, (s >= t) & (s <= 31), (s >= 32) & (s <= t - 1)).astype(np.float32)
        NEG = (MASK - 1.0) * 30000.0
        tabs += [M1, M2, M3, TRI, NTRI, NEG, MASKD, MASKO, M4]
    tabs += [np.ones((L, L), np.float32), np.eye(L, dtype=np.float32)]
    c['cst64'] = np.ascontiguousarray(np.concatenate(tabs, axis=1)).astype(np.float32)
    c['identb'] = np.eye(128, dtype=np.float32).astype(ml_dtypes.bfloat16)
    c['identf'] = np.eye(128, dtype=np.float32)
    NBM = 2 * (T + TC) // 128 + NEXP
    tp = np.arange(128)
    mo = np.zeros((128, 128 + 128 + NBM + 1 + 64), np.float32)
    mo[:, 0:128] = 1.0
    mo[:, 128:256] = (tp[:, None] < tp[None, :]).astype(np.float32)
    mo[:, 256:256 + NBM] = 128.0 * np.arange(NBM)[None, :]
    mo[:, 256 + NBM] = tp
    e_ = np.arange(32)
    mo[0:32, 257 + NBM:257 + NBM + 32] = (e_[:, None] < e_[None, :]).astype(np.float32)
    mo[0:32, 289 + NBM:289 + NBM + 32] = (e_[:, None] <= e_[None, :]).astype(np.float32)
    c['moec'] = mo
    ck = np.arange(64)
    ang = 2 * np.pi * np.outer(ck, ck) / 64.0
    cb = np.zeros((128, 128), np.float64)
    sbm = np.zeros((128, 128), np.float64)
    for g in range(2):
        cb[g * 64:(g + 1) * 64, g * 64:(g + 1) * 64] = np.cos(ang)
        sbm[g * 64:(g + 1) * 64, g * 64:(g + 1) * 64] = -np.sin(ang)
    c['chdft'] = np.concatenate([cb, sbm], axis=1).astype(np.float32).astype(ml_dtypes.bfloat16)
    for nm, Ts in (('l', T), ('c', TC)):
        T1 = Ts // 64
        a1 = 2 * np.pi * np.outer(np.arange(T1), np.arange(T1)) / T1
        c['f1_' + nm] = np.concatenate([np.cos(a1), np.sin(a1), -np.sin(a1)], axis=1).astype(np.float32).astype(ml_dtypes.bfloat16)
        tw = 2 * np.pi * np.outer(np.arange(T1), np.arange(64)) / Ts
        c['tw_' + nm] = np.concatenate([np.cos(tw), -np.sin(tw)], axis=1).astype(np.float32)
        sc = 1.0 / np.sqrt(Ts * 64.0)
        c['f2_' + nm] = (np.concatenate([np.cos(ang), np.sin(ang)], axis=1) * sc).astype(np.float32).astype(ml_dtypes.bfloat16)
    return c


def build_program(T, depth=DEPTH, debug=False, max_stages=10 ** 9):
    nc = bass.Bass("TRN2", target_bir_lowering=False)
    A = {}

    def din(name, shape, dt=F32):
        A[name] = nc.dram_tensor(name, list(shape), dt, kind="ExternalInput").ap()

    def dscr(name, shape, dt=F32):
        kind = "ExternalOutput" if debug else "Internal"
        A[name] = nc.dram_tensor(name, list(shape), dt, kind=kind).ap()

    din('x', [T, D])
    din('ctx', [TC, D])
    din('cc', [128, 16])
    din('ada_w', [DEPTH, D, 6 * D])
    din('ada_b', [DEPTH, 6 * D])
    din('norm_mix_w', [DEPTH, D])
    din('norm_ffn_w', [DEPTH, D])
    din('w_in', [DEPTH, D, NIN])
    din('conv_a_w', [DEPTH, 3, 256])
    din('conv_a_b', [DEPTH, 256])
    din('hgrn_lb', [2, DEPTH, 256])
    din('hgrn_norm_w', [DEPTH, 64])
    din('ssm_conv_w', [DEPTH, 3, 512])
    din('ssm_conv_b', [DEPTH, 512])
    din('ssm_A_log', [DEPTH, 2, 4])
    din('ssm_dt_bias', [DEPTH, 2, 4])
    din('ssm_D', [DEPTH, 4])
    din('ssm_norm_w', [DEPTH, 256])
    din('w_branch', [DEPTH, 4, 256, D])
    din('w_out', [DEPTH, D, D])
    din('router_w', [DEPTH, D, 36])
    din('router_b', [DEPTH, 36])
    din('moe_w1', [DEPTH, NEXP, D, DFF])
    din('moe_w3', [DEPTH, NEXP, D, DFF])
    din('moe_w2', [DEPTH, NEXP, DFF, D])
    din('final_norm_w', [D])
    consts = make_consts(T)
    for k, v in consts.items():
        din(k, v.shape, BF16 if v.dtype == ml_dtypes.bfloat16 else F32)
    A['out'] = nc.dram_tensor('out', [T, D], F32, kind="ExternalOutput").ap()

    dscr('MV', [DEPTH, 2, 6 * D])
    NBM = 2 * (T + TC) // 128 + NEXP
    NTM = (T + TC) // 128
    dscr('FN', [T + TC, D], BF16)
    dscr('FB', [NBM * 128, D], BF16)
    dscr('YBUF', [NBM * 128, D], F32)
    dscr('W1S', [NEXP * 128, 8 * DFF], BF16)
    dscr('W3S', [NEXP * 128, 8 * DFF], BF16)
    dscr('W2S', [NEXP * 128, 4 * D], BF16)
    streams = {}
    for nm, Ts in (('c', TC), ('l', T)):
        sd = {'T': Ts, 'nm': nm, 'row': 0 if nm == 'l' else 1}
        for k, shp, dt in (('PJ', [Ts, NPJ], F32), ('GT', [Ts, 4096], BF16), ('U', [Ts + 2, 256], F32),
                           ('XB', [Ts + 2, 512], F32), ('ZF', [Ts, 512], BF16), ('ZB', [Ts // 64, 64, 512], BF16),
                           ('YC', [Ts, 256], F32), ('OF', [Ts, 256], F32), ('YB', [Ts, 256], F32),
                           ('YF', [Ts, 256], F32), ('YD', [Ts, 256], F32),
                           ('X1', [Ts, D], F32), ('X2', [Ts, D], F32)):
            dscr(k + '_' + nm, shp, dt)
            sd[k] = A[k + '_' + nm]
        streams[nm] = sd
    streams['l']['x'] = A['x']
    streams['c']['x'] = A['ctx']

    pst = contextlib.ExitStack()
    with pst:
        states = {}
        for mx in ('h', 'm'):
            for dirn in range(2):
                states[(mx, dirn)] = pst.enter_context(nc.sbuf_tensor('state_%s%d' % (mx, dirn), [128, 2, 64], F32))

        I32 = mybir.dt.int32
        pers = {
            'OHT': pst.enter_context(nc.sbuf_tensor('p_oht', [128, NTM, 64], F32)),
            'WTT': pst.enter_context(nc.sbuf_tensor('p_wtt', [128, NTM, 2], F32)),
            'DEST': pst.enter_context(nc.sbuf_tensor('p_dest', [128, NTM, 2], I32)),
            'IDXW': pst.enter_context(nc.sbuf_tensor('p_idxw', [128, NBM], I32)),
            'PADB': pst.enter_context(nc.sbuf_tensor('p_padb', [128, 64], F32)),
        }
        nst = [0]

        def run(fn, *a):
            nst[0] += 1
            if nst[0] <= max_stages:
                fn(*a)

        for l in range(depth):
            last = (l == DEPTH - 1)
            run(stage_ada, nc, A, l)
            run(stage_proj, nc, A, l, streams, last)
            for nm in (('l',) if last else ('c', 'l')):
                run(stage_fft_a, nc, A, streams[nm])
                run(stage_fft_c, nc, A, streams[nm])
            for dirn in range(2):
                run(stage_scan, nc, A, l, streams, dirn, states)
            for nm in (('l',) if last else ('c', 'l')):
                run(stage_merge, nc, A, l, streams[nm])
            if SPARSE_MOE:
                run(stage_moe_route, nc, A, l, streams, last, pers, NBM)
                run(stage_moe_disp, nc, A, l, streams, last, pers, NBM)
                run(stage_moe_exp, nc, A, l, streams, last, pers, NBM)
                run(stage_moe_comb, nc, A, l, streams, last, pers)
            else:
                run(stage_moe, nc, A, l, streams, last)
            streams['l']['x'] = streams['l']['X2']
            streams['c']['x'] = streams['c']['X2']
    return nc, consts


def stage_ada(nc, A, l):
    S = St(nc, 'ada%d' % l)
    sT = S.sb([128, 16])
    S.ld(sT[:], A['cc'])
    S.act(sT[:], sT[:], AF.Silu)
    sT3 = sT[:].rearrange("p (k j) -> p k j", j=2)
    abt = S.sb([2, 6 * D])
    S.ld(abt[:], A['ada_b'][l].partition_broadcast(2))
    mrow = S.sb([2, 6 * D])
    awr = S.ring(2, [128, 8, 512])
    psr = S.psring(2)
    for j in range(12):
        aw = awr.next()
        S.ld(aw[:], A['ada_w'][l, :, j * 512:(j + 1) * 512].rearrange("(k p) n -> p k n", p=128))
        pp = psr.next()
        for k in range(8):
            S.mm(pp[0:2, :], sT3[:, k, :], aw[:, k, :], start=(k == 0), stop=(k == 7))
        S.tt(mrow[:, j * 512:(j + 1) * 512], pp[0:2, :], abt[:, j * 512:(j + 1) * 512], ALU.add)
    S.stv(A['MV'][l], mrow[:])
    S.close()


def pipelined(S, items, phases):
    n, K = len(items), len(phases)
    ctxs = {}
    for step in range(n + K - 1):
        groups = []
        for k in range(K):
            idx = step - k
            if 0 <= idx < n:
                S.begin('g%d' % k)
                ctxs[idx] = phases[k](items[idx], ctxs.get(idx))
                groups.append('g%d' % k)
        S.flush(groups)


PJ_BLOCKS = [(i * 512, (i + 1) * 512) for i in range(6)] + [(3072, 3080)] + \
            [(NPJ + i * 512, NPJ + (i + 1) * 512) for i in range(8)]


def load_mod(S, A, l, row, j, wvec=None):
    t = S.sb([128, D])
    S.ld(t[:], A['MV'][l, row, j * D:(j + 1) * D].partition_broadcast(128))
    if wvec is not None:
        S.stt(t[:], t[:], 1.0, wvec[:], ALU.add, ALU.mult)
    return t


def stage_proj(nc, A, l, streams, last):
    S = St(nc, 'pj%d' % l)
    win = S.sb([128, 8, NIN], BF16)
    for k in range(8):
        S.ld(win[:, k, :], A['w_in'][l, k * 128:(k + 1) * 128, :], q='pool')
    wmix = S.sb([128, D])
    S.ld(wmix[:], A['norm_mix_w'][l].partition_broadcast(128))
    ident = S.sb([128, 128], BF16)
    S.ld(ident[:], A['identb'])
    chd = S.sb([128, 256], BF16)
    S.ld(chd[:], A['chdft'])
    zero = S.sb([2, 512])
    S.memset(zero[:], 0.0)
    xr = S.ring(2, [128, D])
    junk = S.sb([128, D])
    ssr = S.ring(2, [128, 1])
    hn = S.sb([128, D])
    hb = S.sb([128, D], BF16)
    hTr = S.ring(2, [128, D], BF16)
    pT = S.ps([128, D], BF16)
    ppr = S.psring(4)
    pF = S.ps()
    pZ = S.ps()
    stgr = S.ring(4, [128, 512])
    gr = S.ring(3, [128, 512], BF16)
    ur = S.ring(2, [128, 256])
    f4r = S.ring(2, [128, 256], BF16)
    zr = S.ring(2, [128, 512], BF16)
    for nm in ('c', 'l'):
        sd = streams[nm]
        Ts = sd['T']
        need_out = not (last and nm == 'c')
        A1 = load_mod(S, A, l, sd['row'], 1, wmix)
        B1 = load_mod(S, A, l, sd['row'], 0)
        S.stv(sd['U'][0:1, :], zero[0:1, 0:256])
        S.stv(sd['U'][Ts + 1:Ts + 2, :], zero[0:1, 0:256])
        S.stv(sd['XB'][0:1, :], zero[0:1, :])
        S.stv(sd['XB'][Ts + 1:Ts + 2, :], zero[0:1, :])
        def pj1(i, _c, sd=sd, A1=A1, B1=B1):
            r0, r1 = i * 128, (i + 1) * 128
            xt = xr.next()
            ss = ssr.next()
            S.ld(xt[:], sd['x'][r0:r1, :])
            S.act(junk[:], xt[:], AF.Square, accum=ss[:])
            S.rstd(ss[:], 1.0 / D)
            S.stt(hn[:], xt[:], ss[:], A1[:], ALU.mult, ALU.mult)
            S.tt(hb[:], hn[:], B1[:], ALU.add)
            for k in range(8):
                S.tr(pT[:, k * 128:(k + 1) * 128], hb[:, k * 128:(k + 1) * 128], ident[:])
            hT = hTr.next()
            S.cp(hT[:], pT[:], eng='act')
            return hT

        def pj2(i, hT, sd=sd, need_out=need_out):
            r0, r1 = i * 128, (i + 1) * 128
            stgs = {}
            for bi, (c0, c1) in enumerate(PJ_BLOCKS):
                if c0 >= NPJ and not need_out:
                    continue
                w = c1 - c0
                pp = ppr.next()
                for k in range(8):
                    S.mm(pp[:, 0:w], hT[:, k * 128:(k + 1) * 128], win[:, k, c0:c1], start=(k == 0), stop=(k == 7))
                if c0 < NPJ:
                    stg = stgr.next()
                    stgs[bi] = stg
                    S.cp(stg[:, 0:w], pp[:, 0:w], eng=('act' if bi % 2 == 0 else 'dve'))
                    S.stv(sd['PJ'][r0:r1, c0:c1], stg[:, 0:w])
                    if bi == 1:
                        ut = ur.next()
                        S.tt(ut[:], stgs[0][:, 256:512], stg[:, 0:256], ALU.mult)
                        S.stv(sd['U'][1 + r0:1 + r1, :], ut[:])
                    if bi == 5:
                        S.stv(sd['XB'][1 + r0:1 + r1, :], stg[:, :])
                else:
                    g = gr.next()
                    S.act(g[:], pp[:], AF.Sigmoid)
                    S.stv(sd['GT'][r0:r1, c0 - NPJ:c1 - NPJ], g[:])
            if need_out:
                for hf in range(2):
                    for k in range(8):
                        S.mm(pF[:, hf * 128:(hf + 1) * 128], win[:, k, 2048 + hf * 128:2048 + (hf + 1) * 128],
                             hT[:, k * 128:(k + 1) * 128], start=(k == 0), stop=(k == 7))
                f4 = f4r.next()
                S.cp(f4[:], pF[:, 0:256])
                for hf in range(2):
                    S.mm(pZ[:, hf * 128:(hf + 1) * 128], f4[:, hf * 128:(hf + 1) * 128], chd[:, 0:128])
                    S.mm(pZ[:, 256 + hf * 128:256 + (hf + 1) * 128], f4[:, hf * 128:(hf + 1) * 128], chd[:, 128:256])
                zt = zr.next()
                S.cp(zt[:], pZ[:], eng='act')
                S.stv(sd['ZF'][r0:r1, :], zt[:])
        pipelined(S, list(range(Ts // 128)), [pj1, pj2])
    S.close()


def stage_fft_a(nc, A, sd):
    nm = sd['nm']
    Ts = sd['T']
    T1 = Ts // 64
    S = St(nc, 'fa' + nm)
    f1 = S.sb([T1, 3 * T1], BF16)
    S.ld(f1[:], A['f1_' + nm])
    tw = S.sb([T1, 128])
    S.ld(tw[:], A['tw_' + nm])
    C1, S1, S1n = f1[:, 0:T1], f1[:, T1:2 * T1], f1[:, 2 * T1:3 * T1]
    ZRt = S.sb([T1, 64 * 256], BF16)
    ZIt = S.sb([T1, 64 * 256], BF16)
    BRt = S.sb([T1, 64 * 256], BF16)
    BIt = S.sb([T1, 64 * 256], BF16)
    ZR = ZRt[:].rearrange("p (b c) -> p b c", c=256)
    ZI = ZIt[:].rearrange("p (b c) -> p b c", c=256)
    BR = BRt[:].rearrange("p (b c) -> p b c", c=256)
    BI = BIt[:].rearrange("p (b c) -> p b c", c=256)
    zv = sd['ZF'].rearrange("(a b) c -> a b c", b=64)
    for q4 in range(4):
        S.ld(ZR[:, q4 * 16:(q4 + 1) * 16, :], zv[:, q4 * 16:(q4 + 1) * 16, 0:256])
        S.ld(ZI[:, q4 * 16:(q4 + 1) * 16, :], zv[:, q4 * 16:(q4 + 1) * 16, 256:512])
    par = S.psring(2)
    pbr = S.psring(2)
    t1r = S.ring(2, [T1, 512])
    t2r = S.ring(2, [T1, 512])
    for j in range(32):
        zr_ = ZRt[:, j * 512:(j + 1) * 512]
        zi_ = ZIt[:, j * 512:(j + 1) * 512]
        pa = par.next()
        pb = pbr.next()
        pa3 = pa[0:T1, :].rearrange("p (a c) -> p a c", a=2)
        pb3 = pb[0:T1, :].rearrange("p (a c) -> p a c", a=2)
        S.mm(pa[0:T1, :], C1, zr_, start=True, stop=False)
        S.mm(pa[0:T1, :], S1, zi_, start=False, stop=True)
        S.mm(pb[0:T1, :], C1, zi_, start=True, stop=False)
        S.mm(pb[0:T1, :], S1n, zr_, start=False, stop=True)
        wr = tw[:, 2 * j:2 * j + 2].unsqueeze(2).to_broadcast([T1, 2, 256])
        wi = tw[:, 64 + 2 * j:64 + 2 * j + 2].unsqueeze(2).to_broadcast([T1, 2, 256])
        t1 = t1r.next()
        t2 = t2r.next()
        t13 = t1[:].rearrange("p (a c) -> p a c", a=2)
        t23 = t2[:].rearrange("p (a c) -> p a c", a=2)
        S.tt(t13, pa3, wr, ALU.mult)
        S.tt(t23, pb3, wi, ALU.mult)
        S.tt(BR[:, 2 * j:2 * j + 2, :], t13, t23, ALU.subtract, eng='pool')
        t1b = t1r.next()
        t2b = t2r.next()
        t1b3 = t1b[:].rearrange("p (a c) -> p a c", a=2)
        t2b3 = t2b[:].rearrange("p (a c) -> p a c", a=2)
        S.tt(t1b3, pa3, wi, ALU.mult)
        S.tt(t2b3, pb3, wr, ALU.mult)
        S.tt(BI[:, 2 * j:2 * j + 2, :], t1b3, t2b3, ALU.add, eng='pool')
    for q4 in range(4):
        S.stv(sd['ZB'][:, q4 * 16:(q4 + 1) * 16, 0:256], BR[:, q4 * 16:(q4 + 1) * 16, :])
        S.stv(sd['ZB'][:, q4 * 16:(q4 + 1) * 16, 256:512], BI[:, q4 * 16:(q4 + 1) * 16, :])
    S.close()


def stage_fft_c(nc, A, sd):
    nm = sd['nm']
    Ts = sd['T']
    T1 = Ts // 64
    S = St(nc, 'fc' + nm)
    f2 = S.sb([64, 128], BF16)
    S.ld(f2[:], A['f2_' + nm])
    CF, SF = f2[:, 0:64], f2[:, 64:128]
    BRt = S.sb([64, T1 * 256], BF16)
    BIt = S.sb([64, T1 * 256], BF16)
    BR = BRt[:].rearrange("p (a c) -> p a c", c=256)
    BI = BIt[:].rearrange("p (a c) -> p a c", c=256)
    zb = sd['ZB'].rearrange("a b c -> b a c")
    nq = 4 if T1 >= 4 else 1
    qs = T1 // nq
    for q4 in range(nq):
        S.ld(BR[:, q4 * qs:(q4 + 1) * qs, :], zb[:, q4 * qs:(q4 + 1) * qs, 0:256])
        S.ld(BI[:, q4 * qs:(q4 + 1) * qs, :], zb[:, q4 * qs:(q4 + 1) * qs, 256:512])
    pr = S.psring(3)
    yr = S.ring(4, [64, 512])
    ycv = sd['YC'].rearrange("(b a) c -> b a c", a=T1)
    for j in range(T1 // 2):
        pp = pr.next()
        pp3 = pp[0:64, :].rearrange("p (a c) -> p a c", a=2)
        S.mm(pp[0:64, :], CF, BRt[:, j * 512:(j + 1) * 512], start=True, stop=False)
        S.mm(pp[0:64, :], SF, BIt[:, j * 512:(j + 1) * 512], start=False, stop=True)
        y = yr.next()
        y3 = y[:].rearrange("p (a c) -> p a c", a=2)
        S.cp(y[:], pp[0:64, :], eng=('act' if j % 2 == 0 else 'dve'))
        S.stv(ycv[:, 2 * j:2 * j + 2, :], y3)
    S.close()


def stage_scan(nc, A, l, streams, dirn, states):
    S = St(nc, 'sc%d%d' % (l, dirn))
    fin = (dirn == 1)
    cst = S.sb([64, 20 * 64])
    S.ld(cst[:], A['cst64'])

    def tab(i):
        return cst[:, i * 64:(i + 1) * 64]
    o = dirn * 9
    M1, M2, M3, TRI, NTRI, NEG, MASKD, MASKO, M4 = [tab(o + i) for i in range(9)]
    ONES, IDF = tab(18), tab(19)
    identb = S.sb([128, 128], BF16)
    S.ld(identb[:], A['identb'])
    idb = identb[0:64, 0:64]
    LB0 = S.sb([64, 256])
    LB1 = S.sb([64, 256])
    if l == 0:
        S.memset(LB0[:], 0.0)
        S.memset(LB1[:], 1.0)
    else:
        a0 = S.sb([64, 256])
        S.ld(a0[:], A['hgrn_lb'][dirn, 0].partition_broadcast(64))
        S.ld(LB0[:], A['hgrn_lb'][dirn, 1].partition_broadcast(64))
        S.tt(LB0[:], LB0[:], a0[:], ALU.subtract)
        S.act(LB0[:], LB0[:], AF.Sigmoid)
        S.ts(LB1[:], LB0[:], -1.0, 1.0, ALU.mult, ALU.add)
    CW = S.sb([64, 3, 512])
    for j in range(3):
        S.ld(CW[:, j, :], A['ssm_conv_w'][l, j].partition_broadcast(64))
    CB = S.sb([64, 512])
    S.ld(CB[:], A['ssm_conv_b'][l].partition_broadcast(64))
    DTB = S.sb([64, 4])
    S.ld(DTB[:], A['ssm_dt_bias'][l, dirn].partition_broadcast(64))
    NEGA = S.sb([64, 4])
    S.ld(NEGA[:], A['ssm_A_log'][l, dirn].partition_broadcast(64))
    S.act(NEGA[:], NEGA[:], AF.Exp)
    S.ts(NEGA[:], NEGA[:], -1.0, None, ALU.mult)
    if fin:
        HNW = S.sb([64, 64])
        S.ld(HNW[:], A['hgrn_norm_w'][l].partition_broadcast(64))
        SNW = S.sb([64, 256])
        S.ld(SNW[:], A['ssm_norm_w'][l].partition_broadcast(64))
        DV = S.sb([64, 4])
        S.ld(DV[:], A['ssm_D'][l].partition_broadcast(64))
    Sh = states[('h', dirn)]
    Sm = states[('m', dirn)]
    S.memset(Sh[:], 0.0)
    S.memset(Sm[:], 0.0)
    Shb = S.sb([128, 2, 64], BF16)
    Smb = S.sb([128, 2, 64], BF16)
    S.memset(Shb[:], 0.0)
    S.memset(Smb[:], 0.0)

    RD = 2
    R = lambda shp, dt=F32, n=RD: S.ring(n, shp, dt)
    qf_r, ff_r, vi_r = R([64, 256]), R([64, 256]), R([64, 256])
    sg_r, sn_r, lf_r, kk_r, qs_r = R([64, 256]), R([64, 256]), R([64, 256]), R([64, 256]), R([64, 256])
    vb_r = R([64, 256], BF16)
    fac_r = R([64, 5, 256])
    el_r = R([128, 4])
    qk_r = R([64, 6, 256], BF16)
    tT_r = R([128, 256], BF16)
    Z_r = [R([128, 384], BF16), R([128, 384], BF16)]
    Zm_r = [R([128, 256], BF16), R([128, 256], BF16)]
    for rr in Z_r + Zm_r:
        for t_ in rr.tiles:
            S.memset(t_[:], 0.0)
    scm_r = R([64, 256], BF16)
    sco_r = R([64, 256], BF16)
    o_r = R([64, 256], F32, 3)
    xb_r = [R([64, 512]) for _ in range(3)]
    dtr_r = R([64, 4])
    cv_r, tmp_r, xc_r = R([64, 512]), R([64, 512]), R([64, 512])
    dt_r, la_r = R([64, 4]), R([64, 4])
    vbm_r = R([64, 256], BF16)
    kkm_r = R([64, 256])
    a1_r, a2_r = R([64, 4, 64]), R([64, 4, 64])
    dm_r = R([64, 256])
    ec_r = R([64, 8])
    elm_r = R([128, 4])
    mk_r = R([64, 4, 256], BF16)
    tTm_r = R([128, 128], BF16)
    scmm_r = R([64, 256], BF16)
    om_r = R([64, 256], F32, 3)
    psE, psE2, psS, psR, psG = S.ps(), S.ps(), S.ps(), S.ps(), S.ps()
    psT = S.ps([128, 1024], BF16)
    psO, psC = S.ps(), S.ps()
    if fin:
        of_r, g_r, z_r, yf_r = R([64, 256]), R([64, 256]), R([64, 256]), R([64, 256])
        sq_r, st_r = R([64, 256]), R([64, 4])
        sq2_r, st2_r = R([64, 256]), R([64, 4])

    order = []
    for nm in ('c', 'l'):
        nch = streams[nm]['T'] // 64
        rng_ = range(nch) if dirn == 0 else range(nch - 1, -1, -1)
        order += [(nm, c) for c in rng_]

    def hA(nm, c):
        sd = streams[nm]
        r0, r1 = c * 64, (c + 1) * 64
        PJ = sd['PJ']
        qf, ff, vi = qf_r.next(), ff_r.next(), vi_r.next()
        S.ld(qf[:], PJ[r0:r1, 768:1024])
        S.ld(ff[:], PJ[r0:r1, 1024 + 256 * dirn:1280 + 256 * dirn])
        S.ld(vi[:], PJ[r0:r1, 1536:1792])
        sg, sn, lf, kk, qs, vb = sg_r.next(), sn_r.next(), lf_r.next(), kk_r.next(), qs_r.next(), vb_r.next()
        S.act(sg[:], ff[:], AF.Sigmoid)
        S.act(sn[:], ff[:], AF.Sigmoid, scale=-1.0)
        S.tt(sg[:], sg[:], LB1[:], ALU.mult)
        S.tt(sg[:], sg[:], LB0[:], ALU.add)
        S.act(lf[:], sg[:], AF.Ln)
        S.tt(kk[:], sn[:], LB1[:], ALU.mult, eng='pool')
        S.act(qs[:], qf[:], AF.Silu)
        S.cp(vb[:], vi[:], eng='pool')
        S.mm(psE[0:64, 0:256], M1, lf[:])
        S.mm(psE[0:64, 256:512], M2, lf[:])
        S.mm(psE2[0:64, 0:256], M3, lf[:])
        S.mm(psE2[0:64, 256:512], M4, lf[:])
        for p in range(2):
            S.mm(psG[:, 272 + 2 * p:274 + 2 * p], lf[:, p * 128:(p + 1) * 128], ONES[:, 0:2])
        fac = fac_r.next()
        S.act(fac[:, 0, :], psE[0:64, 0:256], AF.Exp)
        S.act(fac[:, 1, :], psE[0:64, 0:256], AF.Exp, scale=-1.0)
        S.act(fac[:, 2, :], psE[0:64, 256:512], AF.Exp)
        S.act(fac[:, 3, :], psE2[0:64, 0:256], AF.Exp)
        S.act(fac[:, 4, :], psE2[0:64, 256:512], AF.Exp)
        el = el_r.next()
        S.act(el[:], psG[:, 272:276], AF.Exp)
        qk = qk_r.next()
        S.tt(qk[:, 0, :], qs[:], fac[:, 0, :], ALU.mult)
        S.tt(qk[:, 1, :], qs[:], fac[:, 4, :], ALU.mult)
        S.tt(qk[:, 2, :], kk[:], fac[:, 1, :], ALU.mult)
        S.tt(qk[:, 3, :], qs[:], fac[:, 2, :], ALU.mult)
        S.tt(qk[:, 4, :], kk[:], fac[:, 4, :], ALU.mult, eng='pool')
        S.tt(qk[:, 5, :], kk[:], fac[:, 3, :], ALU.mult, eng='pool')
        for a in range(5):
            for p in range(2):
                S.tr(psT[:, (a * 2 + p) * 64:(a * 2 + p + 1) * 64], qk[:, a, p * 128:(p + 1) * 128], idb)
        tT = tT_r.next()
        Z = [Z_r[0].next(), Z_r[1].next()]
        S.cp(tT[:], psT[:, 0:256])
        S.cp(Z[0][0:64, :], psT[0:64, 256:640])
        S.cp(Z[1][64:128, :], psT[64:128, 256:640])
        for h in range(4):
            p, hf = h // 2, h % 2
            S.mm(psS[0:64, h * 64:(h + 1) * 64], Z[hf][:, p * 64:(p + 1) * 64], tT[:, p * 64:(p + 1) * 64])
        for h in range(4):
            p, hf = h // 2, h % 2
            S.mm(psS[0:64, 256 + h * 64:256 + (h + 1) * 64], Z[hf][:, 256 + p * 64:256 + (p + 1) * 64],
                 tT[:, 128 + p * 64:128 + (p + 1) * 64])
        scm, sco = scm_r.next(), sco_r.next()
        S.tt(scm[:].rearrange("p (h t) -> p h t", h=4), psS[0:64, 0:256].rearrange("p (h t) -> p h t", h=4),
             MASKD.unsqueeze(1).to_broadcast([64, 4, 64]), ALU.mult)
        S.tt(sco[:].rearrange("p (h t) -> p h t", h=4), psS[0:64, 256:512].rearrange("p (h t) -> p h t", h=4),
             MASKO.unsqueeze(1).to_broadcast([64, 4, 64]), ALU.mult)
        S.tt(scm[:], scm[:], sco[:], ALU.add, eng='pool')
        return dict(scm=scm, vb=vb, Z=Z, qk=qk, el=el)

    def hB(nm, c, t):
        sd = streams[nm]
        need_out = not (l == DEPTH - 1 and nm == 'c')
        r0, r1 = c * 64, (c + 1) * 64
        PJ = sd['PJ']
        scm, vb, Z, qk, el = t['scm'], t['vb'], t['Z'], t['qk'], t['el']
        for h in range(4):
            p, hf = h // 2, h % 2
            S.mm(psO[0:64, h * 64:(h + 1) * 64], scm[:, h * 64:(h + 1) * 64], vb[:, h * 64:(h + 1) * 64],
                 start=True, stop=False)
            S.mm(psO[0:64, h * 64:(h + 1) * 64], Z[hf][:, 128 + p * 64:128 + (p + 1) * 64],
                 Shb[:, p, :], start=False, stop=True)
        for p in range(2):
            S.mm(psO[:, 256 + p * 128:256 + (p + 1) * 128], qk[:, 5, p * 128:(p + 1) * 128], vb[:, p * 128:(p + 1) * 128])
        ot = o_r.next()
        S.cp(ot[:], psO[0:64, 0:256], eng='act')
        for h in range(4):
            p, hf = h // 2, h % 2
            sl = slice(hf * 64, (hf + 1) * 64)
            S.stt(Sh[sl, p, :], Sh[sl, p, :], el[sl, 2 * p:2 * p + 1],
                  psO[sl, 256 + p * 128 + hf * 64:256 + p * 128 + (hf + 1) * 64], ALU.mult, ALU.add)
        S.cp(Shb[:], Sh[:], eng='act')
        if not fin:
            S.stv(sd['OF'][r0:r1, :], ot[:])
        elif need_out:
            of, gt = of_r.next(), g_r.next()
            S.ld(of[:], sd['OF'][r0:r1, :])
            S.ld(gt[:], PJ[r0:r1, 1792:2048])
            S.tt(ot[:], ot[:], of[:], ALU.add)
            sq, stt_ = sq_r.next(), st_r.next()
            S.tt(sq[:], ot[:], ot[:], ALU.mult, eng='pool')
            S.reduce(stt_[:], sq[:].rearrange("p (h e) -> p h e", h=4), ALU.add)
            S.rstd(stt_[:], 1.0 / 64)
            S.act(gt[:], gt[:], AF.Silu)
            o3 = ot[:].rearrange("p (h e) -> p h e", h=4)
            S.tt(o3, o3, stt_[:].unsqueeze(2).to_broadcast([64, 4, 64]), ALU.mult)
            S.tt(o3, o3, HNW[:].unsqueeze(1).to_broadcast([64, 4, 64]), ALU.mult, eng='pool')
            S.tt(ot[:], ot[:], gt[:], ALU.mult, eng='pool')
            S.stv(sd['YB'][r0:r1, :], ot[:])

    def mA(nm, c):
        sd = streams[nm]
        r0, r1 = c * 64, (c + 1) * 64
        PJ = sd['PJ']
        xb = [xb_r[j].next() for j in range(3)]
        for j in range(3):
            S.ld(xb[j][:], sd['XB'][r0 + j:r1 + j, :])
        dtr = dtr_r.next()
        S.ld(dtr[:], PJ[r0:r1, 3072 + 4 * dirn:3076 + 4 * dirn])
        cv, tmp, xc = cv_r.next(), tmp_r.next(), xc_r.next()
        S.tt(cv[:], xb[0][:], CW[:, 0, :], ALU.mult, eng='pool')
        S.tt(tmp[:], xb[1][:], CW[:, 1, :], ALU.mult, eng='pool')
        S.tt(cv[:], cv[:], tmp[:], ALU.add, eng='pool')
        S.tt(tmp[:], xb[2][:], CW[:, 2, :], ALU.mult, eng='pool')
        S.tt(cv[:], cv[:], tmp[:], ALU.add, eng='pool')
        S.tt(cv[:], cv[:], CB[:], ALU.add, eng='pool')
        S.act(xc[:], cv[:], AF.Silu)
        dt, la = dt_r.next(), la_r.next()
        S.tt(dt[:], dtr[:], DTB[:], ALU.add)
        S.act(dt[:], dt[:], AF.Exp)
        S.act(dt[:], dt[:], AF.Ln, bias=1.0)
        S.tt(la[:], dt[:], NEGA[:], ALU.mult)
        vbm, kkm = vbm_r.next(), kkm_r.next()
        S.cp(vbm[:], xc[:, 0:256], eng='pool')
        a1, a2 = a1_r.next(), a2_r.next()
        g4 = lambda ap: ap.rearrange("p (g n) -> p g n", g=2).unsqueeze(2).to_broadcast([64, 2, 2, 64])
        h4 = lambda ap: ap.rearrange("p (g h) -> p g h", g=2).unsqueeze(3).to_broadcast([64, 2, 2, 64])
        o4 = lambda ap: ap.rearrange("p (g h n) -> p g h n", g=2, h=2)
        S.tt(o4(kkm[:]), g4(xc[:, 256:384]), h4(dt[:, 0:4]), ALU.mult)
        lab = la[:, 0:4].unsqueeze(2).to_broadcast([64, 4, 64])
        S.tt(a1[:], ONES.unsqueeze(1).to_broadcast([64, 4, 64]), lab, ALU.mult)
        S.tt(a2[:], NTRI.unsqueeze(1).to_broadcast([64, 4, 64]), lab, ALU.mult)
        for h in range(4):
            S.mm(psR[0:64, h * 64:(h + 1) * 64], a1[:, h, :], TRI, start=True, stop=False)
            S.mm(psR[0:64, h * 64:(h + 1) * 64], a2[:, h, :], ONES, start=False, stop=False)
            S.mm(psR[0:64, h * 64:(h + 1) * 64], IDF, NEG, start=False, stop=True)
        S.mm(psR[0:64, 256:260], M2, la[:])
        S.mm(psR[0:64, 260:264], M3, la[:])
        for p in range(2):
            S.mm(psR[:, 264 + 2 * p:266 + 2 * p], a1[:, 2 * p:2 * p + 2, :].rearrange("p a b -> p (a b)"), ONES[:, 0:2])
        dm, ec, elm = dm_r.next(), ec_r.next(), elm_r.next()
        S.act(dm[:], psR[0:64, 0:256], AF.Exp)
        S.act(ec[:], psR[0:64, 256:264], AF.Exp)
        S.act(elm[:], psR[:, 264:268], AF.Exp)
        mk = mk_r.next()
        S.cp(mk[:, 1, :], kkm[:], eng='pool')
        S.cp(o4(mk[:, 0, :]), g4(xc[:, 384:512]), eng='pool')
        S.tt(o4(mk[:, 2, :]), g4(xc[:, 384:512]), h4(ec[:, 0:4]), ALU.mult)
        S.tt(mk[:, 3, :].rearrange("p (h n) -> p h n", h=4), kkm[:].rearrange("p (h n) -> p h n", h=4),
             ec[:, 4:8].unsqueeze(2).to_broadcast([64, 4, 64]), ALU.mult)
        for a in range(3):
            for p in range(2):
                S.tr(psT[:, 640 + (a * 2 + p) * 64:640 + (a * 2 + p + 1) * 64], mk[:, a, p * 128:(p + 1) * 128], idb)
        tTm = tTm_r.next()
        Zm = [Zm_r[0].next(), Zm_r[1].next()]
        S.cp(tTm[:], psT[:, 640:768])
        S.cp(Zm[0][0:64, :], psT[0:64, 768:1024])
        S.cp(Zm[1][64:128, :], psT[64:128, 768:1024])
        for h in range(4):
            p, hf = h // 2, h % 2
            S.mm(psG[0:64, h * 64:(h + 1) * 64], Zm[hf][:, p * 64:(p + 1) * 64], tTm[:, p * 64:(p + 1) * 64])
        scmm = scmm_r.next()
        S.tt(scmm[:], psG[0:64, 0:256], dm[:], ALU.mult)
        return dict(scmm=scmm, vbm=vbm, Zm=Zm, mk=mk, elm=elm, xc=xc)

    def mB(nm, c, t):
        sd = streams[nm]
        need_out = not (l == DEPTH - 1 and nm == 'c')
        r0, r1 = c * 64, (c + 1) * 64
        PJ = sd['PJ']
        scmm, vbm, Zm, mk, elm, xc = t['scmm'], t['vbm'], t['Zm'], t['mk'], t['elm'], t['xc']
        for h in range(4):
            p, hf = h // 2, h % 2
            S.mm(psC[0:64, h * 64:(h + 1) * 64], scmm[:, h * 64:(h + 1) * 64], vbm[:, h * 64:(h + 1) * 64],
                 start=True, stop=False)
            S.mm(psC[0:64, h * 64:(h + 1) * 64], Zm[hf][:, 128 + p * 64:128 + (p + 1) * 64],
                 Smb[:, p, :], start=False, stop=True)
        for p in range(2):
            S.mm(psC[:, 256 + p * 128:256 + (p + 1) * 128], mk[:, 3, p * 128:(p + 1) * 128], vbm[:, p * 128:(p + 1) * 128])
        om = om_r.next()
        S.cp(om[:], psC[0:64, 0:256], eng='act')
        for h in range(4):
            p, hf = h // 2, h % 2
            sl = slice(hf * 64, (hf + 1) * 64)
            S.stt(Sm[sl, p, :], Sm[sl, p, :], elm[sl, 2 * p:2 * p + 1],
                  psC[sl, 256 + p * 128 + hf * 64:256 + p * 128 + (hf + 1) * 64], ALU.mult, ALU.add)
        S.cp(Smb[:], Sm[:], eng='act')
        if not fin:
            S.stv(sd['YF'][r0:r1, :], om[:])
        elif need_out:
            yf, zt = yf_r.next(), z_r.next()
            S.ld(yf[:], sd['YF'][r0:r1, :])
            S.ld(zt[:], PJ[r0:r1, 2304:2560])
            S.tt(om[:], om[:], yf[:], ALU.add)
            sq, stt_ = sq2_r.next(), st2_r.next()
            x3 = xc[:, 0:256].rearrange("p (h e) -> p h e", h=4)
            sq3 = sq[:].rearrange("p (h e) -> p h e", h=4)
            S.tt(sq3, x3, DV[:].unsqueeze(2).to_broadcast([64, 4, 64]), ALU.mult, eng='pool')
            S.tt(om[:], om[:], sq[:], ALU.add)
            S.act(zt[:], zt[:], AF.Silu)
            S.tt(om[:], om[:], zt[:], ALU.mult)
            S.tt(sq[:], om[:], om[:], ALU.mult, eng='pool')
            S.reduce(stt_[:, 0:2], sq[:].rearrange("p (g e) -> p g e", g=2), ALU.add)
            S.rstd(stt_[:, 0:2], 1.0 / 128)
            o3 = om[:].rearrange("p (g e) -> p g e", g=2)
            S.tt(o3, o3, stt_[:, 0:2].unsqueeze(2).to_broadcast([64, 2, 128]), ALU.mult)
            S.tt(om[:], om[:], SNW[:], ALU.mult, eng='pool')
            S.stv(sd['YD'][r0:r1, :], om[:])

    wjobs = []
    if dirn == 0 and SPARSE_MOE:
        wr1 = S.ring(3, [128, 8, DFF], BF16)
        wr2 = S.ring(2, [128, 4, D], BF16)
        for e in range(NEXP):
            wjobs.append(('moe_w1', 'W1S', e, wr1))
            wjobs.append(('moe_w3', 'W3S', e, wr1))
            wjobs.append(('moe_w2', 'W2S', e, wr2))
    nsteps = len(order) + 1
    wper = -(-len(wjobs) // max(1, nsteps - 1)) if wjobs else 0

    prev = None
    for step in range(len(order) + 1):
        groups = []
        cur = None
        if wjobs:
            S.begin('W')
            for _ in range(wper):
                if not wjobs:
                    break
                src, dst, e, rr = wjobs.pop(0)
                t = rr.next()
                S.ld(t[:], A[src][l, e].rearrange("(k p) n -> p k n", p=128), q='pool')
                S.stv(A[dst][e * 128:(e + 1) * 128, :], t[:].rearrange("p k n -> p (k n)"), q='sp')
            groups.append('W')
        if step < len(order):
            nm, c = order[step]
            S.begin('Ah')
            th = hA(nm, c)
            S.begin('Am')
            tm = mA(nm, c)
            cur = (nm, c, th, tm)
            groups += ['Ah', 'Am']
        if prev is not None:
            nm, c, th, tm = prev
            S.begin('Bh')
            hB(nm, c, th)
            S.begin('Bm')
            mB(nm, c, tm)
            groups += ['Bh', 'Bm']
        S.flush(groups)
        prev = cur
    S.close()


def stage_merge(nc, A, l, sd):
    nm = sd['nm']
    Ts = sd['T']
    S = St(nc, 'mg%d%s' % (l, nm))
    ident = S.sb([128, 128], BF16)
    S.ld(ident[:], A['identb'])
    wbr = S.sb([128, 8, D], BF16)
    S.ld(wbr[:], A['w_branch'][l].rearrange("k (j p) n -> p (k j) n", p=128), q='pool')
    wo = S.sb([128, 8, D], BF16)
    S.ld(wo[:], A['w_out'][l].rearrange("(k p) n -> p k n", p=128), q='pool')
    G1 = load_mod(S, A, l, sd['row'], 2)
    CAW = S.sb([128, 3, 256])
    for j in range(3):
        S.ld(CAW[:, j, :], A['conv_a_w'][l, j].partition_broadcast(128))
    CAB = S.sb([128, 256])
    S.ld(CAB[:], A['conv_a_b'][l].partition_broadcast(128))
    ab_r = S.ring(2, [128, 256])
    u_r = [S.ring(2, [128, 256]) for _ in range(3)]
    yin_r = S.ring(2, [128, 3, 256])
    gt_r = S.ring(2, [128, 4096], BF16)
    x_r = S.ring(2, [128, D])
    cv_r = S.ring(2, [128, 256])
    tmp_r = S.ring(2, [128, 256])
    br_r = S.ring(2, [128, 4, 256], BF16)
    brT_r = S.ring(2, [128, D], BF16)
    mg_r = S.ring(2, [128, D])
    mt_r = S.ring(2, [128, 512])
    mgb_r = S.ring(2, [128, D], BF16)
    mT_r = S.ring(2, [128, D], BF16)
    xo_r = S.ring(2, [128, D])
    pT = S.ps([128, D], BF16)
    pT2 = S.ps([128, D], BF16)
    ppr = S.psring(4)
    def mg1(i, _c):
        r0, r1 = i * 128, (i + 1) * 128
        ab = ab_r.next()
        S.ld(ab[:], sd['PJ'][r0:r1, 0:256])
        us = [u_r[j].next() for j in range(3)]
        for j in range(3):
            S.ld(us[j][:], sd['U'][r0 + j:r1 + j, :])
        yin = yin_r.next()
        S.ld(yin[:, 0, :], sd['YB'][r0:r1, :])
        S.ld(yin[:, 1, :], sd['YC'][r0:r1, :])
        S.ld(yin[:, 2, :], sd['YD'][r0:r1, :])
        gt = gt_r.next()
        S.ld(gt[:], sd['GT'][r0:r1, :])
        xt = x_r.next()
        S.ld(xt[:], sd['x'][r0:r1, :])
        cv, tmp = cv_r.next(), tmp_r.next()
        S.tt(cv[:], us[0][:], CAW[:, 0, :], ALU.mult, eng='pool')
        S.tt(tmp[:], us[1][:], CAW[:, 1, :], ALU.mult, eng='pool')
        S.tt(cv[:], cv[:], tmp[:], ALU.add, eng='pool')
        S.tt(tmp[:], us[2][:], CAW[:, 2, :], ALU.mult, eng='pool')
        S.tt(cv[:], cv[:], tmp[:], ALU.add, eng='pool')
        S.tt(cv[:], cv[:], CAB[:], ALU.add, eng='pool')
        br = br_r.next()
        S.tt(br[:, 0, :], cv[:], ab[:], ALU.mult, eng='pool')
        S.cp(br[:, 1:4, :], yin[:], eng='pool')
        for k in range(8):
            S.tr(pT[:, k * 128:(k + 1) * 128], br[:, k // 2, (k % 2) * 128:(k % 2 + 1) * 128], ident[:])
        brT = brT_r.next()
        S.cp(brT[:], pT[:], eng='act')
        return (brT, gt, xt)

    def mg2(i, c_):
        brT, gt, xt = c_
        r0, r1 = i * 128, (i + 1) * 128
        mg = mg_r.next()
        for kb in range(4):
            for hf in range(2):
                pp = ppr.next()
                for j in range(2):
                    S.mm(pp[:], brT[:, (2 * kb + j) * 128:(2 * kb + j + 1) * 128], wbr[:, 2 * kb + j, hf * 512:(hf + 1) * 512],
                         start=(j == 0), stop=(j == 1))
                gsl = gt[:, kb * D + hf * 512:kb * D + (hf + 1) * 512]
                if kb == 0:
                    S.tt(mg[:, hf * 512:(hf + 1) * 512], pp[:], gsl, ALU.mult)
                else:
                    mt = mt_r.next()
                    S.tt(mt[:], pp[:], gsl, ALU.mult)
                    S.tt(mg[:, hf * 512:(hf + 1) * 512], mg[:, hf * 512:(hf + 1) * 512], mt[:], ALU.add, eng='pool')
        mgb = mgb_r.next()
        S.cp(mgb[:], mg[:], eng='act')
        for k in range(8):
            S.tr(pT2[:, k * 128:(k + 1) * 128], mgb[:, k * 128:(k + 1) * 128], ident[:])
        mT = mT_r.next()
        S.cp(mT[:], pT2[:], eng='act')
        xo = xo_r.next()
        for hf in range(2):
            pp = ppr.next()
            for k in range(8):
                S.mm(pp[:], mT[:, k * 128:(k + 1) * 128], wo[:, k, hf * 512:(hf + 1) * 512], start=(k == 0), stop=(k == 7))
            mt = mt_r.next()
            S.tt(mt[:], pp[:], G1[:, hf * 512:(hf + 1) * 512], ALU.mult)
            S.tt(xo[:, hf * 512:(hf + 1) * 512], mt[:], xt[:, hf * 512:(hf + 1) * 512], ALU.add, eng='pool')
        S.stv(sd['X1'][r0:r1, :], xo[:])
    pipelined(S, list(range(Ts // 128)), [mg1, mg2])
    S.close()


def stage_moe(nc, A, l, streams, last):
    S = St(nc, 'moe%d' % l)
    identf = S.sb([128, 128])
    S.ld(identf[:], A['identf'])
    wffn = S.sb([128, D])
    S.ld(wffn[:], A['norm_ffn_w'][l].partition_broadcast(128))
    rw = S.sb([128, 8, 36])
    S.ld(rw[:], A['router_w'][l].rearrange("(k p) n -> p k n", p=128))
    rb = S.sb([128, 36])
    S.ld(rb[:], A['router_b'][l].partition_broadcast(128))
    if last:
        fnw = S.sb([128, D])
        S.ld(fnw[:], A['final_norm_w'].partition_broadcast(128))
    mods = {}
    names = ('l',) if last else ('l', 'c')
    for nm in names:
        row = streams[nm]['row']
        mods[nm] = (load_mod(S, A, l, row, 4, wffn), load_mod(S, A, l, row, 3), load_mod(S, A, l, row, 5))
    tiles = []
    for nm in names:
        sd = streams[nm]
        for i in range(sd['T'] // 128):
            tiles.append((nm, i))
    nblk = max(1, -(-len(tiles) // 8))
    per = -(-len(tiles) // nblk)
    blocks = [tiles[i * per:(i + 1) * per] for i in range(nblk)]
    MAXT = per
    fTt = S.sb([128, 8, MAXT * 128], BF16)
    acc = [S.sb([128, D]) for _ in range(MAXT)]
    wg = [S.sb([128, 32]) for _ in range(MAXT)]
    x_r = S.ring(2, [128, D])
    junk = S.sb([128, D])
    ss_r = S.ring(2, [128, 1])
    fn_r = S.ring(1, [128, D])
    fTf_r = S.ring(1, [128, D])
    pTa = S.ps()
    pTb = S.ps()
    pL = S.ps()
    sm_r = S.ring(2, [128, 64])
    w1_r = S.ring(2, [128, 8, DFF], BF16)
    w3_r = S.ring(2, [128, 8, DFF], BF16)
    w2_r = S.ring(2, [128, 4, D], BF16)
    gact_r = S.ring(2, [128, 4, 512], BF16)
    sil_r = S.ring(2, [128, 512])
    ph_r = S.psring(2)
    ph3_r = S.psring(2)
    po_r = S.psring(1)
    xo_r = S.ring(2, [128, D])
    for blk in blocks:
        nt = len(blk)
        for ti, (nm, i) in enumerate(blk):
            sd = streams[nm]
            A2, B2, G2 = mods[nm]
            r0, r1 = i * 128, (i + 1) * 128
            xt, ss, fn = x_r.next(), ss_r.next(), fn_r.next()
            S.ld(xt[:], sd['X1'][r0:r1, :])
            S.act(junk[:], xt[:], AF.Square, accum=ss[:])
            S.rstd(ss[:], 1.0 / D)
            S.stt(fn[:], xt[:], ss[:], A2[:], ALU.mult, ALU.mult)
            S.tt(fn[:], fn[:], B2[:], ALU.add)
            for k in range(8):
                pt = pTa if k < 4 else pTb
                S.tr(pt[:, (k % 4) * 128:(k % 4 + 1) * 128], fn[:, k * 128:(k + 1) * 128], identf[:])
            fTf = fTf_r.next()
            S.cp(fTf[:, 0:512], pTa[:], eng='act')
            S.cp(fTf[:, 512:1024], pTb[:], eng='dve')
            S.cp(fTt[:, :, ti * 128:(ti + 1) * 128], fTf[:].rearrange("p (k t) -> p k t", k=8), eng='pool')
            for k in range(8):
                S.mm(pL[:, 0:36], fTf[:, k * 128:(k + 1) * 128], rw[:, k, :], start=(k == 0), stop=(k == 7))
            sm = sm_r.next()
            lg = sm[:, 0:36]
            S.tt(lg, pL[:, 0:36], rb[:], ALU.add)
            gmax, gsel, gex, gsum = sm[:, 36:37], sm[:, 37:41], sm[:, 41:45], sm[:, 45:46]
            S.reduce(gmax, lg[:, 0:4], ALU.max)
            S.ts(gsel, lg[:, 0:4], gmax, None, ALU.is_equal)
            S.ts(gex, lg[:, 0:4], gmax, None, ALU.subtract)
            S.act(gex, gex, AF.Exp)
            S.reduce(gsum, gex, ALU.add)
            S.recip(gsum, gsum)
            ein, m1, sel1, e2, m2, sel2 = sm[:, 46:54], sm[:, 54:55], sm[:, 56:64], sm[:, 46:54], sm[:, 55:56], sm[:, 46:54]
            S.ts(ein, lg[:, 4:12], gsel[:, 0:1], None, ALU.mult)
            for g in range(1, 4):
                S.stt(ein, lg[:, 4 + 8 * g:12 + 8 * g], gsel[:, g:g + 1], ein, ALU.mult, ALU.add)
            S.reduce(m1, ein, ALU.max)
            S.ts(sel1, ein, m1, None, ALU.is_equal)
            S.stt(e2, sel1, -1e30, ein, ALU.mult, ALU.add)
            S.reduce(m2, e2, ALU.max)
            S.ts(sel2, e2, m2, None, ALU.is_equal)
            wa, wb_ = sm[:, 36:37], sm[:, 41:42]
            S.tt(wa, m2, m1, ALU.subtract)
            S.act(wa, wa, AF.Exp)
            S.ts(wa, wa, 1.0, None, ALU.add)
            S.recip(wa, wa)
            S.ts(wb_, wa, -1.0, 1.0, ALU.mult, ALU.add)
            S.tt(wa, wa, gsum, ALU.mult)
            S.tt(wb_, wb_, gsum, ALU.mult)
            S.ts(sel1, sel1, wa, None, ALU.mult)
            S.stt(sel1, sel2, wb_, sel1, ALU.mult, ALU.add)
            for g in range(4):
                S.ts(wg[ti][:, 8 * g:8 * g + 8], sel1, gsel[:, g:g + 1], None, ALU.mult)
            S.memset(acc[ti][:], 0.0, eng='pool')
        sbs = [list(range(s, min(s + 4, nt))) for s in range(0, nt, 4)]
        for e in range(NEXP):
            w1, w3, w2 = w1_r.next(), w3_r.next(), w2_r.next()
            S.ld(w1[:], A['moe_w1'][l, e].rearrange("(k p) n -> p k n", p=128), q='pool')
            S.ld(w3[:], A['moe_w3'][l, e].rearrange("(k p) n -> p k n", p=128), q='pool')
            S.ld(w2[:], A['moe_w2'][l, e].rearrange("(k p) n -> p k n", p=128), q='pool')
            for si, sb_ in enumerate(sbs):
                n = len(sb_) * 128
                c0 = sb_[0] * 128
                ga = gact_r.next()
                for fc in range(4):
                    ph, ph3 = ph_r.next(), ph3_r.next()
                    for k in range(8):
                        S.mm(ph[:, 0:n], w1[:, k, fc * 128:(fc + 1) * 128], fTt[:, k, c0:c0 + n],
                             start=(k == 0), stop=(k == 7))
                    for k in range(8):
                        S.mm(ph3[:, 0:n], w3[:, k, fc * 128:(fc + 1) * 128], fTt[:, k, c0:c0 + n],
                             start=(k == 0), stop=(k == 7))
                    sil = sil_r.next()
                    S.act(sil[:, 0:n], ph[:, 0:n], AF.Silu)
                    S.tt(ga[:, fc, 0:n], ph3[:, 0:n], sil[:, 0:n], ALU.mult)
                for tj, ti in enumerate(sb_):
                    for hf in range(2):
                        po = po_r.next()
                        for fc in range(4):
                            S.mm(po[:], ga[:, fc, tj * 128:(tj + 1) * 128], w2[:, fc, hf * 512:(hf + 1) * 512],
                                 start=(fc == 0), stop=(fc == 3))
                        S.stt(acc[ti][:, hf * 512:(hf + 1) * 512], po[:], wg[ti][:, e:e + 1], acc[ti][:, hf * 512:(hf + 1) * 512],
                              ALU.mult, ALU.add)
        for ti, (nm, i) in enumerate(blk):
            sd = streams[nm]
            A2, B2, G2 = mods[nm]
            r0, r1 = i * 128, (i + 1) * 128
            xt, xo = x_r.next(), xo_r.next()
            S.ld(xt[:], sd['X1'][r0:r1, :])
            S.tt(acc[ti][:], acc[ti][:], G2[:], ALU.mult, eng='pool')
            S.tt(xo[:], acc[ti][:], xt[:], ALU.add, eng='pool')
            if last:
                ss = ss_r.next()
                S.act(junk[:], xo[:], AF.Square, accum=ss[:])
                S.rstd(ss[:], 1.0 / D)
                S.stt(xo[:], xo[:], ss[:], fnw[:], ALU.mult, ALU.mult)
                S.stv(A['out'][r0:r1, :], xo[:])
            else:
                S.stv(sd['X2'][r0:r1, :], xo[:])
    S.close()


def moe_tiles(streams, last):
    names = ('l',) if last else ('l', 'c')
    tiles = []
    for nm in names:
        for i in range(streams[nm]['T'] // 128):
            tiles.append((nm, i))
    return names, tiles


def stage_moe_prep(nc, A, l):
    S = St(nc, 'mp%d' % l)
    r1 = S.ring(3, [128, 8, DFF], BF16)
    r2 = S.ring(2, [128, 4, D], BF16)
    for e in range(NEXP):
        for src, dst in (('moe_w1', 'W1S'), ('moe_w3', 'W3S')):
            t = r1.next()
            S.ld(t[:], A[src][l, e].rearrange("(k p) n -> p k n", p=128), q='pool')
            S.stv(A[dst][e * 128:(e + 1) * 128, :], t[:].rearrange("p k n -> p (k n)"), q='sp')
        t = r2.next()
        S.ld(t[:], A['moe_w2'][l, e].rearrange("(k p) n -> p k n", p=128), q='pool')
        S.stv(A['W2S'][e * 128:(e + 1) * 128, :], t[:].rearrange("p k n -> p (k n)"), q='sp')
    S.close()


def stage_moe_route(nc, A, l, streams, last, pers, NBM):
    S = St(nc, 'mr%d' % l)
    names, tiles = moe_tiles(streams, last)
    NB = 2 * len(tiles) + NEXP
    identf = S.sb([128, 128])
    S.ld(identf[:], A['identf'])
    moec = S.sb([128, 321 + NBM])
    S.ld(moec[:], A['moec'])
    ONES = moec[:, 0:128]
    TRIX = moec[0:32, 257 + NBM:289 + NBM]
    TRII = moec[0:32, 289 + NBM:321 + NBM]
    wffn = S.sb([128, D])
    S.ld(wffn[:], A['norm_ffn_w'][l].partition_broadcast(128))
    rw = S.sb([128, 8, 36])
    S.ld(rw[:], A['router_w'][l].rearrange("(k p) n -> p k n", p=128))
    rb = S.sb([128, 36])
    S.ld(rb[:], A['router_b'][l].partition_broadcast(128))
    mods = {}
    for nm in names:
        row = streams[nm]['row']
        mods[nm] = (load_mod(S, A, l, row, 4, wffn), load_mod(S, A, l, row, 3))
    OHT, WTT, PADB = pers['OHT'], pers['WTT'], pers['PADB']
    zt = S.sb([128, D], BF16)
    S.memset(zt[:], 0.0)
    for j in range(NB):
        S.stv(A['FB'][j * 128:(j + 1) * 128, :], zt[:], q='sp')
    x_r = S.ring(2, [128, D])
    junk = S.sb([128, D])
    ss_r = S.ring(2, [128, 1])
    fn_r = S.ring(2, [128, D])
    fb_r = S.ring(2, [128, D], BF16)
    fTf_r = S.ring(2, [128, D])
    pTa, pTb, pL = S.ps(), S.ps(), S.ps()
    pCnt = S.ps()
    sm_r = S.ring(2, [128, 80])
    ohs_r = S.ring(2, [128, 32])
    off = 0
    for ti, (nm, i) in enumerate(tiles):
        sd = streams[nm]
        A2, B2 = mods[nm]
        r0, r1 = i * 128, (i + 1) * 128
        xt, ss, fn, fb = x_r.next(), ss_r.next(), fn_r.next(), fb_r.next()
        S.ld(xt[:], sd['X1'][r0:r1, :])
        S.act(junk[:], xt[:], AF.Square, accum=ss[:])
        S.rstd(ss[:], 1.0 / D)
        S.stt(fn[:], xt[:], ss[:], A2[:], ALU.mult, ALU.mult)
        S.tt(fn[:], fn[:], B2[:], ALU.add)
        S.cp(fb[:], fn[:], eng='pool')
        S.stv(A['FN'][ti * 128:(ti + 1) * 128, :], fb[:], q='sp')
        for k in range(8):
            pt = pTa if k < 4 else pTb
            S.tr(pt[:, (k % 4) * 128:(k % 4 + 1) * 128], fn[:, k * 128:(k + 1) * 128], identf[:])
        fTf = fTf_r.next()
        S.cp(fTf[:, 0:512], pTa[:], eng='act')
        S.cp(fTf[:, 512:1024], pTb[:], eng='dve')
        for k in range(8):
            S.mm(pL[:, 0:36], fTf[:, k * 128:(k + 1) * 128], rw[:, k, :], start=(k == 0), stop=(k == 7))
        sm = sm_r.next()
        lg = sm[:, 0:36]
        S.tt(lg, pL[:, 0:36], rb[:], ALU.add)
        gmax, gsel, gex, gsum = sm[:, 36:37], sm[:, 37:41], sm[:, 41:45], sm[:, 45:46]
        S.reduce(gmax, lg[:, 0:4], ALU.max)
        S.ts(gsel, lg[:, 0:4], gmax, None, ALU.is_equal)
        S.ts(gex, lg[:, 0:4], gmax, None, ALU.subtract)
        S.act(gex, gex, AF.Exp)
        S.reduce(gsum, gex, ALU.add)
        S.recip(gsum, gsum)
        ein, m1, m2 = sm[:, 46:54], sm[:, 54:55], sm[:, 55:56]
        sel1, e2, sel2 = sm[:, 56:64], sm[:, 64:72], sm[:, 72:80]
        S.ts(ein, lg[:, 4:12], gsel[:, 0:1], None, ALU.mult)
        for g in range(1, 4):
            S.stt(ein, lg[:, 4 + 8 * g:12 + 8 * g], gsel[:, g:g + 1], ein, ALU.mult, ALU.add)
        S.reduce(m1, ein, ALU.max)
        S.ts(sel1, ein, m1, None, ALU.is_equal)
        S.stt(e2, sel1, -1e30, ein, ALU.mult, ALU.add)
        S.reduce(m2, e2, ALU.max)
        S.ts(sel2, e2, m2, None, ALU.is_equal)
        wa, wb_ = sm[:, 36:37], sm[:, 41:42]
        S.tt(wa, m2, m1, ALU.subtract)
        S.act(wa, wa, AF.Exp)
        S.ts(wa, wa, 1.0, None, ALU.add)
        S.recip(wa, wa)
        S.ts(wb_, wa, -1.0, 1.0, ALU.mult, ALU.add)
        S.tt(WTT[:, ti, 0:1], wa, gsum, ALU.mult)
        S.tt(WTT[:, ti, 1:2], wb_, gsum, ALU.mult)
        for g in range(4):
            S.ts(OHT[:, ti, 8 * g:8 * g + 8], sel1, gsel[:, g:g + 1], None, ALU.mult)
            S.ts(OHT[:, ti, 32 + 8 * g:40 + 8 * g], sel2, gsel[:, g:g + 1], None, ALU.mult)
        ohs = ohs_r.next()
        S.tt(ohs[:], OHT[:, ti, 0:32], OHT[:, ti, 32:64], ALU.add)
        S.mm(pCnt[0:32, 0:2], ohs[:], ONES[:, 0:2], start=(ti == 0), stop=(ti == len(tiles) - 1))
    I32 = mybir.dt.int32
    cf = S.sb([32, 4])
    ci = S.sb([32, 2], I32)
    S.ts(cf[:, 0:1], pCnt[0:32, 0:1], 127.0, None, ALU.add)
    S.cp(ci[:, 0:1], cf[:, 0:1])
    S.op('dve', lambda e: e.tensor_scalar(out=ci[:, 1:2], in0=ci[:, 0:1], scalar1=7, scalar2=7,
                                          op0=ALU.arith_shift_right, op1=ALU.logical_shift_left), [ci[:, 0:1]], [ci[:, 1:2]])
    S.cp(cf[:, 1:2], ci[:, 1:2])
    pbc = S.sb([32, 128])
    S.ts(pbc[:], ONES[0:32, :], cf[:, 1:2], None, ALU.mult)
    pP = S.ps()
    S.mm(pP[:, 0:32], pbc[:], TRIX)
    S.mm(pP[:, 32:64], pbc[:], TRII)
    S.cp(PADB[:], pP[:, 0:64])
    S.close()


def stage_moe_disp(nc, A, l, streams, last, pers, NBM):
    S = St(nc, 'md%d' % l)
    names, tiles = moe_tiles(streams, last)
    NB = 2 * len(tiles) + NEXP
    I32 = mybir.dt.int32
    moec = S.sb([128, 321 + NBM])
    S.ld(moec[:], A['moec'])
    ONES = moec[:, 0:128]
    STRICT = moec[:, 128:256]
    JROW = moec[:, 256:256 + NBM]
    PIDX = moec[:, 256 + NBM:257 + NBM]
    OHT, DEST, IDXW, PADB = pers['OHT'], pers['DEST'], pers['IDXW'], pers['PADB']
    accp = S.sb([128, 32])
    S.memset(accp[:], 0.0)
    ohs_r = S.ring(2, [128, 32])
    tmp_r = S.ring(2, [128, 32])
    t2_r = S.ring(2, [128, 32])
    df_r = S.ring(2, [128, 2])
    fb_r = S.ring(3, [128, D], BF16)
    pr = S.psring(2)
    for ti in range(len(tiles)):
        ohs, tmp, t2, df, fb = ohs_r.next(), tmp_r.next(), t2_r.next(), df_r.next(), fb_r.next()
        pp = pr.next()
        S.tt(ohs[:], OHT[:, ti, 0:32], OHT[:, ti, 32:64], ALU.add)
        S.mm(pp[:, 0:32], ONES, accp[:], start=True, stop=False)
        S.mm(pp[:, 0:32], STRICT, ohs[:], start=False, stop=True)
        S.tt(tmp[:], pp[:, 0:32], PADB[:, 0:32], ALU.add)
        S.tt(accp[:], accp[:], ohs[:], ALU.add, eng='pool')
        for k in range(2):
            S.tt(t2[:], tmp[:], OHT[:, ti, 32 * k:32 * k + 32], ALU.mult)
            S.reduce(df[:, k:k + 1], t2[:], ALU.add)
        S.cp(DEST[:, ti, :], df[:])
        S.ld(fb[:], A['FN'][ti * 128:(ti + 1) * 128, :])
        for k in range(2):
            S.op('pool', lambda e, ti=ti, k=k, fb=fb: e.indirect_dma_start(
                out=A['FB'], out_offset=bass.IndirectOffsetOnAxis(ap=DEST[:, ti, k:k + 1], axis=0),
                in_=fb[:], in_offset=None), [fb[:], DEST[:]], [], dma=True)
    be = S.sb([128, NBM])
    S.memset(be[:], 0.0)
    for e in range(NEXP):
        S.stt(be[:], JROW, PADB[:, 32 + e:33 + e], be[:], ALU.is_ge, ALU.add)
    S.ts(be[:], be[:], 31.0, 128.0, ALU.min, ALU.mult)
    S.ts(be[:], be[:], PIDX, None, ALU.add)
    S.cp(IDXW[:], be[:])
    S.close()


def stage_moe_exp(nc, A, l, streams, last, pers, NBM):
    S = St(nc, 'me%d' % l)
    names, tiles = moe_tiles(streams, last)
    NB = 2 * len(tiles) + NEXP
    IDXW = pers['IDXW']
    ident = S.sb([128, 128], BF16)
    S.ld(ident[:], A['identb'])
    w1_r = S.ring(3, [128, 8 * DFF], BF16)
    w3_r = S.ring(3, [128, 8 * DFF], BF16)
    w2_r = S.ring(3, [128, 4 * D], BF16)
    xb_r = S.ring(3, [128, D], BF16)
    xT_r = S.ring(2, [128, D], BF16)
    sil_r = S.ring(2, [128, DFF])
    g_r = S.ring(2, [128, DFF], BF16)
    gT_r = S.ring(2, [128, DFF], BF16)
    yb_r = S.ring(2, [128, D])
    pT = S.ps([128, D], BF16)
    pT2 = S.ps([128, D], BF16)
    ph_r, ph3_r, po_r = S.psring(2), S.psring(2), S.psring(2)
    def ex1(j, _c):
        w1, w3, w2 = w1_r.next(), w3_r.next(), w2_r.next()
        for wt, nm in ((w1, 'W1S'), (w3, 'W3S'), (w2, 'W2S')):
            S.op('pool', lambda e, wt=wt, nm=nm, j=j: e.indirect_dma_start(
                out=wt[:], out_offset=None, in_=A[nm],
                in_offset=bass.IndirectOffsetOnAxis(ap=IDXW[:, j:j + 1], axis=0)), [IDXW[:]], [wt[:]], dma=True)
        xb = xb_r.next()
        S.ld(xb[:], A['FB'][j * 128:(j + 1) * 128, :])
        for k in range(8):
            S.tr(pT[:, k * 128:(k + 1) * 128], xb[:, k * 128:(k + 1) * 128], ident[:])
        xT = xT_r.next()
        S.cp(xT[:], pT[:])
        ph, ph3 = ph_r.next(), ph3_r.next()
        for k in range(8):
            S.mm(ph[:], xT[:, k * 128:(k + 1) * 128], w1[:, k * DFF:(k + 1) * DFF], start=(k == 0), stop=(k == 7))
        for k in range(8):
            S.mm(ph3[:], xT[:, k * 128:(k + 1) * 128], w3[:, k * DFF:(k + 1) * DFF], start=(k == 0), stop=(k == 7))
        return (ph, ph3, w2)

    def ex2(j, c_):
        ph, ph3, w2 = c_
        sil, g = sil_r.next(), g_r.next()
        S.act(sil[:], ph[:], AF.Silu)
        S.tt(g[:], ph3[:], sil[:], ALU.mult)
        for fc in range(4):
            S.tr(pT2[:, fc * 128:(fc + 1) * 128], g[:, fc * 128:(fc + 1) * 128], ident[:])
        gT = gT_r.next()
        S.cp(gT[:], pT2[:, 0:512])
        yb = yb_r.next()
        for hf in range(2):
            po = po_r.next()
            for fc in range(4):
                S.mm(po[:], gT[:, fc * 128:(fc + 1) * 128], w2[:, fc * D + hf * 512:fc * D + (hf + 1) * 512],
                     start=(fc == 0), stop=(fc == 3))
            S.cp(yb[:, hf * 512:(hf + 1) * 512], po[:], eng=('act' if hf == 0 else 'dve'))
        S.stv(A['YBUF'][j * 128:(j + 1) * 128, :], yb[:], q='sp')
    pipelined(S, list(range(NB)), [ex1, ex2])
    S.close()


def stage_moe_comb(nc, A, l, streams, last, pers):
    S = St(nc, 'mc%d' % l)
    names, tiles = moe_tiles(streams, last)
    DEST, WTT = pers['DEST'], pers['WTT']
    G2 = {nm: load_mod(S, A, l, streams[nm]['row'], 5) for nm in names}
    if last:
        fnw = S.sb([128, D])
        S.ld(fnw[:], A['final_norm_w'].partition_broadcast(128))
    x_r = S.ring(2, [128, D])
    y0_r, y1_r = S.ring(2, [128, D]), S.ring(2, [128, D])
    xo_r = S.ring(2, [128, D])
    junk = S.sb([128, D])
    ss_r = S.ring(2, [128, 1])
    for ti, (nm, i) in enumerate(tiles):
        sd = streams[nm]
        r0, r1 = i * 128, (i + 1) * 128
        xt, y0, y1, xo = x_r.next(), y0_r.next(), y1_r.next(), xo_r.next()
        S.ld(xt[:], sd['X1'][r0:r1, :])
        for k, yt in ((0, y0), (1, y1)):
            S.op('pool', lambda e, ti=ti, k=k, yt=yt: e.indirect_dma_start(
                out=yt[:], out_offset=None, in_=A['YBUF'],
                in_offset=bass.IndirectOffsetOnAxis(ap=DEST[:, ti, k:k + 1], axis=0)), [DEST[:]], [yt[:]], dma=True)
        S.ts(y0[:], y0[:], WTT[:, ti, 0:1], None, ALU.mult)
        S.stt(y0[:], y1[:], WTT[:, ti, 1:2], y0[:], ALU.mult, ALU.add)
        S.tt(y0[:], y0[:], G2[nm][:], ALU.mult, eng='pool')
        S.tt(xo[:], y0[:], xt[:], ALU.add, eng='pool')
        if last:
            ss = ss_r.next()
            S.act(junk[:], xo[:], AF.Square, accum=ss[:])
            S.rstd(ss[:], 1.0 / D)
            S.stt(xo[:], xo[:], ss[:], fnw[:], ALU.mult, ALU.mult)
            S.stv(A['out'][r0:r1, :], xo[:], q='sp')
        else:
            S.stv(sd['X2'][r0:r1, :], xo[:], q='sp')
    S.close()


def prep_inputs(inputs, b, consts):
    f = lambda a: np.ascontiguousarray(np.asarray(a, dtype=np.float32))
    m = {}
    m['x'] = f(inputs['x'][b])
    m['ctx'] = f(inputs['ctx'][b])
    cc = np.stack([f(inputs['c'][b]).reshape(8, 128).T, f(inputs['c_ctx']).reshape(8, 128).T], axis=-1)
    m['cc'] = np.ascontiguousarray(cc.reshape(128, 16))
    for k in ('ada_w', 'ada_b', 'norm_mix_w', 'norm_ffn_w', 'w_in', 'conv_a_w', 'conv_a_b', 'hgrn_lb', 'hgrn_norm_w',
              'ssm_conv_w', 'ssm_conv_b', 'ssm_A_log', 'ssm_dt_bias', 'ssm_D', 'ssm_norm_w', 'w_branch', 'w_out',
              'moe_w1', 'moe_w3', 'moe_w2', 'final_norm_w'):
        m[k] = f(inputs[k])
    m['router_w'] = np.ascontiguousarray(np.concatenate([f(inputs['router_group_w']), f(inputs['router_expert_w'])], axis=-1))
    m['router_b'] = np.ascontiguousarray(np.concatenate([f(inputs['router_group_b']), f(inputs['router_expert_b'])], axis=-1))
    m.update(consts)
    return m


_CACHE = {}


def kernel(**inputs):
    x = np.asarray(inputs['x'])
    B, T, _ = x.shape
    if T not in _CACHE:
        _CACHE[T] = build_program(T)
    nc, consts = _CACHE[T]
    ncores = 8
    in_maps = [prep_inputs(inputs, c % B, consts) for c in range(ncores)]
    res = run_bass_kernel_spmd(nc, in_maps, core_ids=list(range(ncores)))
    out = np.stack([np.asarray(res.results[b]['out']) for b in range(B)], axis=0)
    return out.astype(np.float32)
```

```python
import contextlib
import numpy as np
import ml_dtypes
import concourse.bass as bass
import concourse.mybir as mybir
from concourse.bass_utils import run_bass_kernel_spmd

F32 = mybir.dt.float32
BF16 = mybir.dt.bfloat16
AF = mybir.ActivationFunctionType
ALU = mybir.AluOpType
AX = mybir.AxisListType

COMPUTE = ('pe', 'act', 'dve', 'pool')
ALLENG = ('pe', 'act', 'dve', 'pool', 'sp')
NDMASEM = 12

D = 1024
NIN = 7176
NPJ = 3080
TC = 256
DEPTH = 2
EPS = 1e-6
NEXP = 32
DFF = 512
SPARSE_MOE = True


_UID = [0]


class Op:
    __slots__ = ('eng', 'idx', 'fn', 'waits', 'signal', 'dma', 'dsem', 'dval', 'vc', 'dwait')


class Prog:
    def __init__(self, nc):
        self.nc = nc
        self.ops = {e: [] for e in ALLENG}
        self.lastw = {}
        self.readers = {}
        self.vc = {e: {f: -1 for f in COMPUTE} for e in ALLENG}
        self.selfk = {e: -1 for e in ALLENG}
        self.dma_known = {e: set() for e in ALLENG}
        self.dma_cnt = {e: 0 for e in ALLENG}
        self.dma_ops = {e: [] for e in ALLENG}

    def add(self, eng, fn, reads=(), writes=(), dma=False):
        op = Op()
        op.eng = eng
        op.fn = fn
        op.idx = len(self.ops[eng])
        op.signal = False
        op.dma = dma
        op.waits = []
        op.dwait = None
        op.dsem = None
        op.dval = None
        deps = []
        for r in reads:
            w = self.lastw.get(r)
            if w is not None:
                deps.append((w, True))
        for w_ in writes:
            w = self.lastw.get(w_)
            if w is not None:
                deps.append((w, False))
            for rd in self.readers.get(w_, ()):
                deps.append((rd, False))
        vc = self.vc[eng]
        for d, raw in deps:
            if d is op:
                continue
            if d.dma:
                if id(d) in self.dma_known[eng]:
                    continue
                self.dma_known[eng].add(id(d))
                op.waits.append(d)
            elif d.eng == eng:
                if not dma:
                    if eng == 'pe':
                        continue
                if self.selfk[eng] >= d.idx:
                    continue
                self.selfk[eng] = d.idx
                d.signal = True
                op.waits.append(d)
            else:
                if vc[d.eng] >= d.idx:
                    continue
                d.signal = True
                op.waits.append(d)
                for f in COMPUTE:
                    if d.vc[f] > vc[f]:
                        vc[f] = d.vc[f]
        if dma:
            i = self.dma_cnt[eng]
            self.dma_cnt[eng] += 1
            op.dsem = i % NDMASEM
            op.dval = 16 * (i // NDMASEM + 1)
            if i >= NDMASEM:
                op.dwait = (op.dsem, 16 * (i // NDMASEM))
                prev = self.dma_ops[eng][i - NDMASEM]
                self.dma_known[eng].add(id(prev))
            self.dma_ops[eng].append(op)
            op.vc = None
        else:
            if eng in COMPUTE:
                vc[eng] = op.idx
            op.vc = {f: vc[f] for f in COMPUTE}
        for r in reads:
            self.readers.setdefault(r, []).append(op)
        for w_ in writes:
            self.lastw[w_] = op
            self.readers[w_] = []
        self.ops[eng].append(op)
        return op

    def emit(self):
        nc = self.nc
        with contextlib.ExitStack() as st:
            _UID[0] += 1
            u = _UID[0]
            esem = {e: nc.alloc_semaphore('s%d_%s' % (u, e)) for e in COMPUTE}
            dsem = {e: [nc.alloc_semaphore('d%d_%s_%d' % (u, e, i)) for i in range(NDMASEM)]
                    for e in ALLENG if self.dma_cnt[e] > 0}
            self.allsems = list(esem.values()) + [x for v in dsem.values() for x in v]
            cnts = {}
            for e in COMPUTE:
                c = 0
                for op in self.ops[e]:
                    if op.signal and not op.dma:
                        c += 1
                    cnts[id(op)] = c
            block = st.enter_context(nc.Block())
            final = []
            for e in ALLENG:
                for i in range(min(NDMASEM, self.dma_cnt[e])):
                    n = (self.dma_cnt[e] - 1 - i) // NDMASEM + 1
                    final.append((dsem[e][i], 16 * n))

            def run(e, eng):
                for op in self.ops[e]:
                    if op.dwait is not None:
                        eng.wait_ge(dsem[e][op.dwait[0]], op.dwait[1])
                    for d in op.waits:
                        if d.dma:
                            eng.wait_ge(dsem[d.eng][d.dsem], d.dval)
                        else:
                            eng.wait_ge(esem[d.eng], cnts[id(d)])
                    ins = op.fn(eng)
                    if op.dma:
                        ins.then_inc(dsem[e][op.dsem], 16)
                    elif op.signal:
                        ins.then_inc(esem[e], 1)
                if e == 'sp':
                    for s, v in final:
                        eng.wait_ge(s, v)

            @block.tensor
            def _(eng):
                run('pe', eng)

            @block.scalar
            def _(eng):
                run('act', eng)

            @block.vector
            def _(eng):
                run('dve', eng)

            @block.gpsimd
            def _(eng):
                run('pool', eng)

            @block.sync
            def _(eng):
                run('sp', eng)


class Ring:
    def __init__(self, tiles):
        self.tiles = tiles
        self.i = 0

    def next(self):
        t = self.tiles[self.i % len(self.tiles)]
        self.i += 1
        return t


def _keys(aps):
    out = []
    for a in aps:
        if a is None or isinstance(a, (float, int)):
            continue
        out.append(a if isinstance(a, (str, tuple)) else a.name)
    return out


class St:
    def __init__(self, nc, tag):
        self.nc = nc
        self.P = Prog(nc)
        self.es = contextlib.ExitStack()
        self.tag = tag

    def sb(self, shape, dt=F32):
        _UID[0] += 1
        return self.es.enter_context(self.nc.sbuf_tensor('%s_s%d' % (self.tag, _UID[0]), list(shape), dt))

    def ps(self, shape=(128, 512), dt=F32):
        _UID[0] += 1
        nm = '%s_p%d' % (self.tag, _UID[0])
        if not hasattr(self, 'psnames'):
            self.psnames = set()
        self.psnames.add(nm)
        return self.es.enter_context(self.nc.psum_tensor(nm, list(shape), dt))

    def ring(self, n, shape, dt=F32):
        return Ring([self.sb(shape, dt) for _ in range(n)])

    def psring(self, n, shape=(128, 512), dt=F32):
        return Ring([self.ps(shape, dt) for _ in range(n)])

    def close(self):
        self.P.emit()
        self.es.close()
        self.nc.all_engine_barrier()
        self.nc.clear_and_free_semaphores(self.P.allsems)
        self.nc.all_engine_barrier()

    def begin(self, name):
        if not hasattr(self, 'groups'):
            self.groups = {}
        self.grp = name
        self.groups[name] = []

    def flush(self, names):
        items = []
        for gi, nm in enumerate(names):
            lst = self.groups.pop(nm)
            n = len(lst)
            for k, it in enumerate(lst):
                items.append(((k + 0.5) / n, gi, k, it))
        items.sort(key=lambda x: (x[0], x[1], x[2]))
        self.grp = None
        for _, _, _, (eng, fn, r, w, dma) in items:
            self._add(eng, fn, r, w, dma)

    def op(self, eng, fn, ins, outs, dma=False):
        if getattr(self, 'grp', None) is not None:
            self.groups[self.grp].append((eng, fn, _keys(ins), _keys(outs), dma))
            return
        self._add(eng, fn, _keys(ins), _keys(outs), dma)

    def _add(self, eng, fn, r, w, dma):
        import os
        lim = os.environ.get('OPLIMIT_' + self.tag)
        self.nop_ = getattr(self, 'nop_', 0) + 1
        if lim is not None and self.nop_ > int(lim):
            return
        psn = getattr(self, 'psnames', ())
        w = list(w) + [k for k in r if k in psn and k not in w]
        self.P.add(eng, fn, reads=r, writes=w, dma=dma)

    def mm(self, out, lhsT, rhs, start=True, stop=True):
        self.op('pe', lambda e: e.matmul(out, lhsT, rhs, start=start, stop=stop), [lhsT, rhs], [out])

    def tr(self, out, in_, ident):
        self.op('pe', lambda e: e.transpose(out, in_, ident), [in_, ident], [out])

    def act(self, out, in_, func, bias=None, scale=None, accum=None, eng='act'):
        kw = {}
        if bias is not None:
            kw['bias'] = bias
        if scale is not None:
            kw['scale'] = scale
        if accum is not None:
            kw['accum_out'] = accum
        self.op(eng, lambda e: e.activation(out=out, in_=in_, func=func, **kw),
                [in_, bias, scale], [out, accum])

    def tt(self, out, a, b, op, eng='dve'):
        self.op(eng, lambda e: e.tensor_tensor(out=out, in0=a, in1=b, op=op), [a, b], [out])

    def ts(self, out, a, s1, s2, op0, op1=None, eng='dve'):
        if op1 is None:
            self.op(eng, lambda e: e.tensor_scalar(out=out, in0=a, scalar1=s1, scalar2=None, op0=op0),
                    [a, s1], [out])
        else:
            self.op(eng, lambda e: e.tensor_scalar(out=out, in0=a, scalar1=s1, scalar2=s2, op0=op0, op1=op1),
                    [a, s1, s2], [out])

    def stt(self, out, in0, scalar, in1, op0, op1):
        self.op('dve', lambda e: e.scalar_tensor_tensor(out=out, in0=in0, scalar=scalar, in1=in1, op0=op0, op1=op1),
                [in0, scalar, in1], [out])

    def cp(self, out, in_, eng='dve'):
        if eng == 'act':
            self.op('act', lambda e: e.copy(out=out, in_=in_), [in_], [out])
        else:
            self.op(eng, lambda e: e.tensor_copy(out=out, in_=in_), [in_], [out])

    def recip(self, out, in_):
        self.op('dve', lambda e: e.reciprocal(out=out, in_=in_), [in_], [out])

    def memset(self, out, v, eng='dve'):
        self.op(eng, lambda e: e.memset(out, v), [], [out])

    def reduce(self, out, in_, op, eng='dve'):
        self.op(eng, lambda e: e.tensor_reduce(out=out, in_=in_, axis=AX.X, op=op), [in_], [out])

    def ld(self, out, in_, q='sp', slow=False):
        if slow:
            self.op(q, lambda e: e.dma_start(out=out, in_=in_, allow_slow_non_contiguous=True), [], [out], dma=True)
        else:
            self.op(q, lambda e: e.dma_start(out=out, in_=in_), [], [out], dma=True)

    def stv(self, out, in_, q='pool'):
        self.op(q, lambda e: e.dma_start(out=out, in_=in_), [in_], [], dma=True)

    def rstd(self, ss, scale):
        self.ts(ss, ss, scale, EPS, ALU.mult, ALU.add)
        self.act(ss, ss, AF.Sqrt)
        self.recip(ss, ss)


def make_consts(T):
    c = {}
    L = 64
    mid = 31
    s = np.arange(L)[:, None]
    t = np.arange(L)[None, :]
    tabs = []
    half = lambda a: (a >= 32)
    for dirn in range(2):
        if dirn == 0:
            midt = np.where(t < 32, 15, 47)
            M1 = (s <= t).astype(np.float32) - (s <= midt).astype(np.float32)
            M2 = (s <= t).astype(np.float32)
            M3 = (s > t).astype(np.float32)
            TRI = (s <= t).astype(np.float32)
            NTRI = -(s <= t).astype(np.float32)
            MASK = (s <= t).astype(np.float32)
            MASKD = ((s <= t) & (half(s) == half(t))).astype(np.float32)
            MASKO = ((s <= 31) & (t >= 32)).astype(np.float32)
            M4 = np.where(t >= 32, (s >= 32) & (s <= t), (s >= t + 1) & (s <= 31)).astype(np.float32)
        else:
            midt = np.where(t < 32, 16, 48)
            M1 = -((s < t).astype(np.float32) - (s < midt).astype(np.float32))
            M2 = (s >= t).astype(np.float32)
            M3 = (s < t).astype(np.float32)
            TRI = -(s < t).astype(np.float32)
            NTRI = (s < t).astype(np.float32)
            MASK = (s >= t).astype(np.float32)
            MASKD = ((s >= t) & (half(s) == half(t))).astype(np.float32)
            MASKO = ((s >= 32) & (t <= 31)).astype(np.float32)
            M4 = np.where(t <= 31, (s >= t) & (s <= 31), (s >= 32) & (s <= t - 1)).astype(np.float32)
        NEG = (MASK - 1.0) * 30000.0
        tabs += [M1, M2, M3, TRI, NTRI, NEG, MASKD, MASKO, M4]
    tabs += [np.ones((L, L), np.float32), np.eye(L, dtype=np.float32)]
    c['cst64'] = np.ascontiguousarray(np.concatenate(tabs, axis=1)).astype(np.float32)
    c['identb'] = np.eye(128, dtype=np.float32).astype(ml_dtypes.bfloat16)
    c['identf'] = np.eye(128, dtype=np.float32)
    NBM = 2 * (T + TC) // 128 + NEXP
    tp = np.arange(128)
    mo = np.zeros((128, 128 + 128 + NBM + 1 + 64), np.float32)
    mo[:, 0:128] = 1.0
    mo[:, 128:256] = (tp[:, None] < tp[None, :]).astype(np.float32)
    mo[:, 256:256 + NBM] = 128.0 * np.arange(NBM)[None, :]
    mo[:, 256 + NBM] = tp
    e_ = np.arange(32)
    mo[0:32, 257 + NBM:257 + NBM + 32] = (e_[:, None] < e_[None, :]).astype(np.float32)
    mo[0:32, 289 + NBM:289 + NBM + 32] = (e_[:, None] <= e_[None, :]).astype(np.float32)
    c['moec'] = mo
    ck = np.arange(64)
    ang = 2 * np.pi * np.outer(ck, ck) / 64.0
    cb = np.zeros((128, 128), np.float64)
    sbm = np.zeros((128, 128), np.float64)
    for g in range(2):
        cb[g * 64:(g + 1) * 64, g * 64:(g + 1) * 64] = np.cos(ang)
        sbm[g * 64:(g + 1) * 64, g * 64:(g + 1) * 64] = -np.sin(ang)
    c['chdft'] = np.concatenate([cb, sbm], axis=1).astype(np.float32).astype(ml_dtypes.bfloat16)
    for nm, Ts in (('l', T), ('c', TC)):
        T1 = Ts // 64
        a1 = 2 * np.pi * np.outer(np.arange(T1), np.arange(T1)) / T1
        c['f1_' + nm] = np.concatenate([np.cos(a1), np.sin(a1), -np.sin(a1)], axis=1).astype(np.float32).astype(ml_dtypes.bfloat16)
        tw = 2 * np.pi * np.outer(np.arange(T1), np.arange(64)) / Ts
        c['tw_' + nm] = np.concatenate([np.cos(tw), -np.sin(tw)], axis=1).astype(np.float32)
        sc = 1.0 / np.sqrt(Ts * 64.0)
        c['f2_' + nm] = (np.concatenate([np.cos(ang), np.sin(ang)], axis=1) * sc).astype(np.float32).astype(ml_dtypes.bfloat16)
    return c


def build_program(T, depth=DEPTH, debug=False, max_stages=10 ** 9):
    nc = bass.Bass("TRN2", target_bir_lowering=False)
    A = {}

    def din(name, shape, dt=F32):
        A[name] = nc.dram_tensor(name, list(shape), dt, kind="ExternalInput").ap()

    def dscr(name, shape, dt=F32):
        kind = "ExternalOutput" if debug else "Internal"
        A[name] = nc.dram_tensor(name, list(shape), dt, kind=kind).ap()

    din('x', [T, D])
    din('ctx', [TC, D])
    din('cc', [128, 16])
    din('ada_w', [DEPTH, D, 6 * D])
    din('ada_b', [DEPTH, 6 * D])
    din('norm_mix_w', [DEPTH, D])
    din('norm_ffn_w', [DEPTH, D])
    din('w_in', [DEPTH, D, NIN])
    din('conv_a_w', [DEPTH, 3, 256])
    din('conv_a_b', [DEPTH, 256])
    din('hgrn_lb', [2, DEPTH, 256])
    din('hgrn_norm_w', [DEPTH, 64])
    din('ssm_conv_w', [DEPTH, 3, 512])
    din('ssm_conv_b', [DEPTH, 512])
    din('ssm_A_log', [DEPTH, 2, 4])
    din('ssm_dt_bias', [DEPTH, 2, 4])
    din('ssm_D', [DEPTH, 4])
    din('ssm_norm_w', [DEPTH, 256])
    din('w_branch', [DEPTH, 4, 256, D])
    din('w_out', [DEPTH, D, D])
    din('router_w', [DEPTH, D, 36])
    din('router_b', [DEPTH, 36])
    din('moe_w1', [DEPTH, NEXP, D, DFF])
    din('moe_w3', [DEPTH, NEXP, D, DFF])
    din('moe_w2', [DEPTH, NEXP, DFF, D])
    din('final_norm_w', [D])
    consts = make_consts(T)
    for k, v in consts.items():
        din(k, v.shape, BF16 if v.dtype == ml_dtypes.bfloat16 else F32)
    A['out'] = nc.dram_tensor('out', [T, D], F32, kind="ExternalOutput").ap()

    dscr('MV', [DEPTH, 2, 6 * D])
    NBM = 2 * (T + TC) // 128 + NEXP
    NTM = (T + TC) // 128
    dscr('FN', [T + TC, D], BF16)
    dscr('FB', [NBM * 128, D], BF16)
    dscr('YBUF', [NBM * 128, D], F32)
    dscr('W1S', [NEXP * 128, 8 * DFF], BF16)
    dscr('W3S', [NEXP * 128, 8 * DFF], BF16)
    dscr('W2S', [NEXP * 128, 4 * D], BF16)
    streams = {}
    for nm, Ts in (('c', TC), ('l', T)):
        sd = {'T': Ts, 'nm': nm, 'row': 0 if nm == 'l' else 1}
        for k, shp, dt in (('PJ', [Ts, NPJ], F32), ('GT', [Ts, 4096], BF16), ('U', [Ts + 2, 256], F32),
                           ('XB', [Ts + 2, 512], F32), ('ZF', [Ts, 512], BF16), ('ZB', [Ts // 64, 64, 512], BF16),
                           ('YC', [Ts, 256], F32), ('OF', [Ts, 256], F32), ('YB', [Ts, 256], F32),
                           ('YF', [Ts, 256], F32), ('YD', [Ts, 256], F32),
                           ('X1', [Ts, D], F32), ('X2', [Ts, D], F32)):
            dscr(k + '_' + nm, shp, dt)
            sd[k] = A[k + '_' + nm]
        streams[nm] = sd
    streams['l']['x'] = A['x']
    streams['c']['x'] = A['ctx']

    pst = contextlib.ExitStack()
    with pst:
        states = {}
        for mx in ('h', 'm'):
            for dirn in range(2):
                states[(mx, dirn)] = pst.enter_context(nc.sbuf_tensor('state_%s%d' % (mx, dirn), [128, 2, 64], F32))

        I32 = mybir.dt.int32
        pers = {
            'OHT': pst.enter_context(nc.sbuf_tensor('p_oht', [128, NTM, 64], F32)),
            'WTT': pst.enter_context(nc.sbuf_tensor('p_wtt', [128, NTM, 2], F32)),
            'DEST': pst.enter_context(nc.sbuf_tensor('p_dest', [128, NTM, 2], I32)),
            'IDXW': pst.enter_context(nc.sbuf_tensor('p_idxw', [128, NBM], I32)),
            'PADB': pst.enter_context(nc.sbuf_tensor('p_padb', [128, 64], F32)),
        }
        nst = [0]

        def run(fn, *a):
            nst[0] += 1
            if nst[0] <= max_stages:
                fn(*a)

        for l in range(depth):
            last = (l == DEPTH - 1)
            run(stage_ada, nc, A, l)
            run(stage_proj, nc, A, l, streams, last)
            for nm in (('l',) if last else ('c', 'l')):
                run(stage_fft_a, nc, A, streams[nm])
                run(stage_fft_c, nc, A, streams[nm])
            for dirn in range(2):
                run(stage_scan, nc, A, l, streams, dirn, states)
            for nm in (('l',) if last else ('c', 'l')):
                run(stage_merge, nc, A, l, streams[nm])
            if SPARSE_MOE:
                run(stage_moe_route, nc, A, l, streams, last, pers, NBM)
                run(stage_moe_disp, nc, A, l, streams, last, pers, NBM)
                run(stage_moe_exp, nc, A, l, streams, last, pers, NBM)
                run(stage_moe_comb, nc, A, l, streams, last, pers)
            else:
                run(stage_moe, nc, A, l, streams, last)
            streams['l']['x'] = streams['l']['X2']
            streams['c']['x'] = streams['c']['X2']
    return nc, consts


def stage_ada(nc, A, l):
    S = St(nc, 'ada%d' % l)
    sT = S.sb([128, 16])
    S.ld(sT[:], A['cc'])
    S.act(sT[:], sT[:], AF.Silu)
    sT3 = sT[:].rearrange("p (k j) -> p k j", j=2)
    abt = S.sb([2, 6 * D])
    S.ld(abt[:], A['ada_b'][l].partition_broadcast(2))
    mrow = S.sb([2, 6 * D])
    awr = S.ring(2, [128, 8, 512])
    psr = S.psring(2)
    for j in range(12):
        aw = awr.next()
        S.ld(aw[:], A['ada_w'][l, :, j * 512:(j + 1) * 512].rearrange("(k p) n -> p k n", p=128))
        pp = psr.next()
        for k in range(8):
            S.mm(pp[0:2, :], sT3[:, k, :], aw[:, k, :], start=(k == 0), stop=(k == 7))
        S.tt(mrow[:, j * 512:(j + 1) * 512], pp[0:2, :], abt[:, j * 512:(j + 1) * 512], ALU.add)
    S.stv(A['MV'][l], mrow[:])
    S.close()


def pipelined(S, items, phases):
    n, K = len(items), len(phases)
    ctxs = {}
    for step in range(n + K - 1):
        groups = []
        for k in range(K):
            idx = step - k
            if 0 <= idx < n:
                S.begin('g%d' % k)
                ctxs[idx] = phases[k](items[idx], ctxs.get(idx))
                groups.append('g%d' % k)
        S.flush(groups)


PJ_BLOCKS = [(i * 512, (i + 1) * 512) for i in range(6)] + [(3072, 3080)] + \
            [(NPJ + i * 512, NPJ + (i + 1) * 512) for i in range(8)]


def load_mod(S, A, l, row, j, wvec=None):
    t = S.sb([128, D])
    S.ld(t[:], A['MV'][l, row, j * D:(j + 1) * D].partition_broadcast(128))
    if wvec is not None:
        S.stt(t[:], t[:], 1.0, wvec[:], ALU.add, ALU.mult)
    return t


def stage_proj(nc, A, l, streams, last):
    S = St(nc, 'pj%d' % l)
    win = S.sb([128, 8, NIN], BF16)
    for k in range(8):
        S.ld(win[:, k, :], A['w_in'][l, k * 128:(k + 1) * 128, :], q='pool')
    wmix = S.sb([128, D])
    S.ld(wmix[:], A['norm_mix_w'][l].partition_broadcast(128))
    ident = S.sb([128, 128], BF16)
    S.ld(ident[:], A['identb'])
    chd = S.sb([128, 256], BF16)
    S.ld(chd[:], A['chdft'])
    zero = S.sb([2, 512])
    S.memset(zero[:], 0.0)
    xr = S.ring(2, [128, D])
    junk = S.sb([128, D])
    ssr = S.ring(2, [128, 1])
    hn = S.sb([128, D])
    hb = S.sb([128, D], BF16)
    hTr = S.ring(2, [128, D], BF16)
    pT = S.ps([128, D], BF16)
    ppr = S.psring(4)
    pF = S.ps()
    pZ = S.ps()
    stgr = S.ring(4, [128, 512])
    gr = S.ring(3, [128, 512], BF16)
    ur = S.ring(2, [128, 256])
    f4r = S.ring(2, [128, 256], BF16)
    zr = S.ring(2, [128, 512], BF16)
    for nm in ('c', 'l'):
        sd = streams[nm]
        Ts = sd['T']
        need_out = not (last and nm == 'c')
        A1 = load_mod(S, A, l, sd['row'], 1, wmix)
        B1 = load_mod(S, A, l, sd['row'], 0)
        S.stv(sd['U'][0:1, :], zero[0:1, 0:256])
        S.stv(sd['U'][Ts + 1:Ts + 2, :], zero[0:1, 0:256])
        S.stv(sd['XB'][0:1, :], zero[0:1, :])
        S.stv(sd['XB'][Ts + 1:Ts + 2, :], zero[0:1, :])
        def pj1(i, _c, sd=sd, A1=A1, B1=B1):
            r0, r1 = i * 128, (i + 1) * 128
            xt = xr.next()
            ss = ssr.next()
            S.ld(xt[:], sd['x'][r0:r1, :])
            S.act(junk[:], xt[:], AF.Square, accum=ss[:])
            S.rstd(ss[:], 1.0 / D)
            S.stt(hn[:], xt[:], ss[:], A1[:], ALU.mult, ALU.mult)
            S.tt(hb[:], hn[:], B1[:], ALU.add)
            for k in range(8):
                S.tr(pT[:, k * 128:(k + 1) * 128], hb[:, k * 128:(k + 1) * 128], ident[:])
            hT = hTr.next()
            S.cp(hT[:], pT[:], eng='act')
            return hT

        def pj2(i, hT, sd=sd, need_out=need_out):
            r0, r1 = i * 128, (i + 1) * 128
            stgs = {}
            for bi, (c0, c1) in enumerate(PJ_BLOCKS):
                if c0 >= NPJ and not need_out:
                    continue
                w = c1 - c0
                pp = ppr.next()
                for k in range(8):
                    S.mm(pp[:, 0:w], hT[:, k * 128:(k + 1) * 128], win[:, k, c0:c1], start=(k == 0), stop=(k == 7))
                if c0 < NPJ:
                    stg = stgr.next()
                    stgs[bi] = stg
                    S.cp(stg[:, 0:w], pp[:, 0:w], eng=('act' if bi % 2 == 0 else 'dve'))
                    S.stv(sd['PJ'][r0:r1, c0:c1], stg[:, 0:w])
                    if bi == 1:
                        ut = ur.next()
                        S.tt(ut[:], stgs[0][:, 256:512], stg[:, 0:256], ALU.mult)
                        S.stv(sd['U'][1 + r0:1 + r1, :], ut[:])
                    if bi == 5:
                        S.stv(sd['XB'][1 + r0:1 + r1, :], stg[:, :])
                else:
                    g = gr.next()
                    S.act(g[:], pp[:], AF.Sigmoid)
                    S.stv(sd['GT'][r0:r1, c0 - NPJ:c1 - NPJ], g[:])
            if need_out:
                for hf in range(2):
                    for k in range(8):
                        S.mm(pF[:, hf * 128:(hf + 1) * 128], win[:, k, 2048 + hf * 128:2048 + (hf + 1) * 128],
                             hT[:, k * 128:(k + 1) * 128], start=(k == 0), stop=(k == 7))
                f4 = f4r.next()
                S.cp(f4[:], pF[:, 0:256])
                for hf in range(2):
                    S.mm(pZ[:, hf * 128:(hf + 1) * 128], f4[:, hf * 128:(hf + 1) * 128], chd[:, 0:128])
                    S.mm(pZ[:, 256 + hf * 128:256 + (hf + 1) * 128], f4[:, hf * 128:(hf + 1) * 128], chd[:, 128:256])
                zt = zr.next()
                S.cp(zt[:], pZ[:], eng='act')
                S.stv(sd['ZF'][r0:r1, :], zt[:])
        pipelined(S, list(range(Ts // 128)), [pj1, pj2])
    S.close()


def stage_fft_a(nc, A, sd):
    nm = sd['nm']
    Ts = sd['T']
    T1 = Ts // 64
    S = St(nc, 'fa' + nm)
    f1 = S.sb([T1, 3 * T1], BF16)
    S.ld(f1[:], A['f1_' + nm])
    tw = S.sb([T1, 128])
    S.ld(tw[:], A['tw_' + nm])
    C1, S1, S1n = f1[:, 0:T1], f1[:, T1:2 * T1], f1[:, 2 * T1:3 * T1]
    ZRt = S.sb([T1, 64 * 256], BF16)
    ZIt = S.sb([T1, 64 * 256], BF16)
    BRt = S.sb([T1, 64 * 256], BF16)
    BIt = S.sb([T1, 64 * 256], BF16)
    ZR = ZRt[:].rearrange("p (b c) -> p b c", c=256)
    ZI = ZIt[:].rearrange("p (b c) -> p b c", c=256)
    BR = BRt[:].rearrange("p (b c) -> p b c", c=256)
    BI = BIt[:].rearrange("p (b c) -> p b c", c=256)
    zv = sd['ZF'].rearrange("(a b) c -> a b c", b=64)
    for q4 in range(4):
        S.ld(ZR[:, q4 * 16:(q4 + 1) * 16, :], zv[:, q4 * 16:(q4 + 1) * 16, 0:256])
        S.ld(ZI[:, q4 * 16:(q4 + 1) * 16, :], zv[:, q4 * 16:(q4 + 1) * 16, 256:512])
    par = S.psring(2)
    pbr = S.psring(2)
    t1r = S.ring(2, [T1, 512])
    t2r = S.ring(2, [T1, 512])
    for j in range(32):
        zr_ = ZRt[:, j * 512:(j + 1) * 512]
        zi_ = ZIt[:, j * 512:(j + 1) * 512]
        pa = par.next()
        pb = pbr.next()
        pa3 = pa[0:T1, :].rearrange("p (a c) -> p a c", a=2)
        pb3 = pb[0:T1, :].rearrange("p (a c) -> p a c", a=2)
        S.mm(pa[0:T1, :], C1, zr_, start=True, stop=False)
        S.mm(pa[0:T1, :], S1, zi_, start=False, stop=True)
        S.mm(pb[0:T1, :], C1, zi_, start=True, stop=False)
        S.mm(pb[0:T1, :], S1n, zr_, start=False, stop=True)
        wr = tw[:, 2 * j:2 * j + 2].unsqueeze(2).to_broadcast([T1, 2, 256])
        wi = tw[:, 64 + 2 * j:64 + 2 * j + 2].unsqueeze(2).to_broadcast([T1, 2, 256])
        t1 = t1r.next()
        t2 = t2r.next()
        t13 = t1[:].rearrange("p (a c) -> p a c", a=2)
        t23 = t2[:].rearrange("p (a c) -> p a c", a=2)
        S.tt(t13, pa3, wr, ALU.mult)
        S.tt(t23, pb3, wi, ALU.mult)
        S.tt(BR[:, 2 * j:2 * j + 2, :], t13, t23, ALU.subtract, eng='pool')
        t1b = t1r.next()
        t2b = t2r.next()
        t1b3 = t1b[:].rearrange("p (a c) -> p a c", a=2)
        t2b3 = t2b[:].rearrange("p (a c) -> p a c", a=2)
        S.tt(t1b3, pa3, wi, ALU.mult)
        S.tt(t2b3, pb3, wr, ALU.mult)
        S.tt(BI[:, 2 * j:2 * j + 2, :], t1b3, t2b3, ALU.add, eng='pool')
    for q4 in range(4):
        S.stv(sd['ZB'][:, q4 * 16:(q4 + 1) * 16, 0:256], BR[:, q4 * 16:(q4 + 1) * 16, :])
        S.stv(sd['ZB'][:, q4 * 16:(q4 + 1) * 16, 256:512], BI[:, q4 * 16:(q4 + 1) * 16, :])
    S.close()


def stage_fft_c(nc, A, sd):
    nm = sd['nm']
    Ts = sd['T']
    T1 = Ts // 64
    S = St(nc, 'fc' + nm)
    f2 = S.sb([64, 128], BF16)
    S.ld(f2[:], A['f2_' + nm])
    CF, SF = f2[:, 0:64], f2[:, 64:128]
    BRt = S.sb([64, T1 * 256], BF16)
    BIt = S.sb([64, T1 * 256], BF16)
    BR = BRt[:].rearrange("p (a c) -> p a c", c=256)
    BI = BIt[:].rearrange("p (a c) -> p a c", c=256)
    zb = sd['ZB'].rearrange("a b c -> b a c")
    nq = 4 if T1 >= 4 else 1
    qs = T1 // nq
    for q4 in range(nq):
        S.ld(BR[:, q4 * qs:(q4 + 1) * qs, :], zb[:, q4 * qs:(q4 + 1) * qs, 0:256])
        S.ld(BI[:, q4 * qs:(q4 + 1) * qs, :], zb[:, q4 * qs:(q4 + 1) * qs, 256:512])
    pr = S.psring(3)
    yr = S.ring(4, [64, 512])
    ycv = sd['YC'].rearrange("(b a) c -> b a c", a=T1)
    for j in range(T1 // 2):
        pp = pr.next()
        pp3 = pp[0:64, :].rearrange("p (a c) -> p a c", a=2)
        S.mm(pp[0:64, :], CF, BRt[:, j * 512:(j + 1) * 512], start=True, stop=False)
        S.mm(pp[0:64, :], SF, BIt[:, j * 512:(j + 1) * 512], start=False, stop=True)
        y = yr.next()
        y3 = y[:].rearrange("p (a c) -> p a c", a=2)
        S.cp(y[:], pp[0:64, :], eng=('act' if j % 2 == 0 else 'dve'))
        S.stv(ycv[:, 2 * j:2 * j + 2, :], y3)
    S.close()


def stage_scan(nc, A, l, streams, dirn, states):
    S = St(nc, 'sc%d%d' % (l, dirn))
    fin = (dirn == 1)
    cst = S.sb([64, 20 * 64])
    S.ld(cst[:], A['cst64'])

    def tab(i):
        return cst[:, i * 64:(i + 1) * 64]
    o = dirn * 9
    M1, M2, M3, TRI, NTRI, NEG, MASKD, MASKO, M4 = [tab(o + i) for i in range(9)]
    ONES, IDF = tab(18), tab(19)
    identb = S.sb([128, 128], BF16)
    S.ld(identb[:], A['identb'])
    idb = identb[0:64, 0:64]
    LB0 = S.sb([64, 256])
    LB1 = S.sb([64, 256])
    if l == 0:
        S.memset(LB0[:], 0.0)
        S.memset(LB1[:], 1.0)
    else:
        a0 = S.sb([64, 256])
        S.ld(a0[:], A['hgrn_lb'][dirn, 0].partition_broadcast(64))
        S.ld(LB0[:], A['hgrn_lb'][dirn, 1].partition_broadcast(64))
        S.tt(LB0[:], LB0[:], a0[:], ALU.subtract)
        S.act(LB0[:], LB0[:], AF.Sigmoid)
        S.ts(LB1[:], LB0[:], -1.0, 1.0, ALU.mult, ALU.add)
    CW = S.sb([64, 3, 512])
    for j in range(3):
        S.ld(CW[:, j, :], A['ssm_conv_w'][l, j].partition_broadcast(64))
    CB = S.sb([64, 512])
    S.ld(CB[:], A['ssm_conv_b'][l].partition_broadcast(64))
    DTB = S.sb([64, 4])
    S.ld(DTB[:], A['ssm_dt_bias'][l, dirn].partition_broadcast(64))
    NEGA = S.sb([64, 4])
    S.ld(NEGA[:], A['ssm_A_log'][l, dirn].partition_broadcast(64))
    S.act(NEGA[:], NEGA[:], AF.Exp)
    S.ts(NEGA[:], NEGA[:], -1.0, None, ALU.mult)
    if fin:
        HNW = S.sb([64, 64])
        S.ld(HNW[:], A['hgrn_norm_w'][l].partition_broadcast(64))
        SNW = S.sb([64, 256])
        S.ld(SNW[:], A['ssm_norm_w'][l].partition_broadcast(64))
        DV = S.sb([64, 4])
        S.ld(DV[:], A['ssm_D'][l].partition_broadcast(64))
    Sh = states[('h', dirn)]
    Sm = states[('m', dirn)]
    S.memset(Sh[:], 0.0)
    S.memset(Sm[:], 0.0)
    Shb = S.sb([128, 2, 64], BF16)
    Smb = S.sb([128, 2, 64], BF16)
    S.memset(Shb[:], 0.0)
    S.memset(Smb[:], 0.0)

    RD = 2
    R = lambda shp, dt=F32, n=RD: S.ring(n, shp, dt)
    qf_r, ff_r, vi_r = R([64, 256]), R([64, 256]), R([64, 256])
    sg_r, sn_r, lf_r, kk_r, qs_r = R([64, 256]), R([64, 256]), R([64, 256]), R([64, 256]), R([64, 256])
    vb_r = R([64, 256], BF16)
    fac_r = R([64, 5, 256])
    el_r = R([128, 4])
    qk_r = R([64, 6, 256], BF16)
    tT_r = R([128, 256], BF16)
    Z_r = [R([128, 384], BF16), R([128, 384], BF16)]
    Zm_r = [R([128, 256], BF16), R([128, 256], BF16)]
    for rr in Z_r + Zm_r:
        for t_ in rr.tiles:
            S.memset(t_[:], 0.0)
    scm_r = R([64, 256], BF16)
    sco_r = R([64, 256], BF16)
    o_r = R([64, 256], F32, 3)
    xb_r = [R([64, 512]) for _ in range(3)]
    dtr_r = R([64, 4])
    cv_r, tmp_r, xc_r = R([64, 512]), R([64, 512]), R([64, 512])
    dt_r, la_r = R([64, 4]), R([64, 4])
    vbm_r = R([64, 256], BF16)
    kkm_r = R([64, 256])
    a1_r, a2_r = R([64, 4, 64]), R([64, 4, 64])
    dm_r = R([64, 256])
    ec_r = R([64, 8])
    elm_r = R([128, 4])
    mk_r = R([64, 4, 256], BF16)
    tTm_r = R([128, 128], BF16)
    scmm_r = R([64, 256], BF16)
    om_r = R([64, 256], F32, 3)
    psE, psE2, psS, psR, psG = S.ps(), S.ps(), S.ps(), S.ps(), S.ps()
    psT = S.ps([128, 1024], BF16)
    psO, psC = S.ps(), S.ps()
    if fin:
        of_r, g_r, z_r, yf_r = R([64, 256]), R([64, 256]), R([64, 256]), R([64, 256])
        sq_r, st_r = R([64, 256]), R([64, 4])
        sq2_r, st2_r = R([64, 256]), R([64, 4])

    order = []
    for nm in ('c', 'l'):
        nch = streams[nm]['T'] // 64
        rng_ = range(nch) if dirn == 0 else range(nch - 1, -1, -1)
        order += [(nm, c) for c in rng_]

    def hA(nm, c):
        sd = streams[nm]
        r0, r1 = c * 64, (c + 1) * 64
        PJ = sd['PJ']
        qf, ff, vi = qf_r.next(), ff_r.next(), vi_r.next()
        S.ld(qf[:], PJ[r0:r1, 768:1024])
        S.ld(ff[:], PJ[r0:r1, 1024 + 256 * dirn:1280 + 256 * dirn])
        S.ld(vi[:], PJ[r0:r1, 1536:1792])
        sg, sn, lf, kk, qs, vb = sg_r.next(), sn_r.next(), lf_r.next(), kk_r.next(), qs_r.next(), vb_r.next()
        S.act(sg[:], ff[:], AF.Sigmoid)
        S.act(sn[:], ff[:], AF.Sigmoid, scale=-1.0)
        S.tt(sg[:], sg[:], LB1[:], ALU.mult)
        S.tt(sg[:], sg[:], LB0[:], ALU.add)
        S.act(lf[:], sg[:], AF.Ln)
        S.tt(kk[:], sn[:], LB1[:], ALU.mult, eng='pool')
        S.act(qs[:], qf[:], AF.Silu)
        S.cp(vb[:], vi[:], eng='pool')
        S.mm(psE[0:64, 0:256], M1, lf[:])
        S.mm(psE[0:64, 256:512], M2, lf[:])
        S.mm(psE2[0:64, 0:256], M3, lf[:])
        S.mm(psE2[0:64, 256:512], M4, lf[:])
        for p in range(2):
            S.mm(psG[:, 272 + 2 * p:274 + 2 * p], lf[:, p * 128:(p + 1) * 128], ONES[:, 0:2])
        fac = fac_r.next()
        S.act(fac[:, 0, :], psE[0:64, 0:256], AF.Exp)
        S.act(fac[:, 1, :], psE[0:64, 0:256], AF.Exp, scale=-1.0)
        S.act(fac[:, 2, :], psE[0:64, 256:512], AF.Exp)
        S.act(fac[:, 3, :], psE2[0:64, 0:256], AF.Exp)
        S.act(fac[:, 4, :], psE2[0:64, 256:512], AF.Exp)
        el = el_r.next()
        S.act(el[:], psG[:, 272:276], AF.Exp)
        qk = qk_r.next()
        S.tt(qk[:, 0, :], qs[:], fac[:, 0, :], ALU.mult)
        S.tt(qk[:, 1, :], qs[:], fac[:, 4, :], ALU.mult)
        S.tt(qk[:, 2, :], kk[:], fac[:, 1, :], ALU.mult)
        S.tt(qk[:, 3, :], qs[:], fac[:, 2, :], ALU.mult)
        S.tt(qk[:, 4, :], kk[:], fac[:, 4, :], ALU.mult, eng='pool')
        S.tt(qk[:, 5, :], kk[:], fac[:, 3, :], ALU.mult, eng='pool')
        for a in range(5):
            for p in range(2):
                S.tr(psT[:, (a * 2 + p) * 64:(a * 2 + p + 1) * 64], qk[:, a, p * 128:(p + 1) * 128], idb)
        tT = tT_r.next()
        Z = [Z_r[0].next(), Z_r[1].next()]
        S.cp(tT[:], psT[:, 0:256])
        S.cp(Z[0][0:64, :], psT[0:64, 256:640])
        S.cp(Z[1][64:128, :], psT[64:128, 256:640])
        for h in range(4):
            p, hf = h // 2, h % 2
            S.mm(psS[0:64, h * 64:(h + 1) * 64], Z[hf][:, p * 64:(p + 1) * 64], tT[:, p * 64:(p + 1) * 64])
        for h in range(4):
            p, hf = h // 2, h % 2
            S.mm(psS[0:64, 256 + h * 64:256 + (h + 1) * 64], Z[hf][:, 256 + p * 64:256 + (p + 1) * 64],
                 tT[:, 128 + p * 64:128 + (p + 1) * 64])
        scm, sco = scm_r.next(), sco_r.next()
        S.tt(scm[:].rearrange("p (h t) -> p h t", h=4), psS[0:64, 0:256].rearrange("p (h t) -> p h t", h=4),
             MASKD.unsqueeze(1).to_broadcast([64, 4, 64]), ALU.mult)
        S.tt(sco[:].rearrange("p (h t) -> p h t", h=4), psS[0:64, 256:512].rearrange("p (h t) -> p h t", h=4),
             MASKO.unsqueeze(1).to_broadcast([64, 4, 64]), ALU.mult)
        S.tt(scm[:], scm[:], sco[:], ALU.add, eng='pool')
        return dict(scm=scm, vb=vb, Z=Z, qk=qk, el=el)

    def hB(nm, c, t):
        sd = streams[nm]
        need_out = not (l == DEPTH - 1 and nm == 'c')
        r0, r1 = c * 64, (c + 1) * 64
        PJ = sd['PJ']
        scm, vb, Z, qk, el = t['scm'], t['vb'], t['Z'], t['qk'], t['el']
        for h in range(4):
            p, hf = h // 2, h % 2
            S.mm(psO[0:64, h * 64:(h + 1) * 64], scm[:, h * 64:(h + 1) * 64], vb[:, h * 64:(h + 1) * 64],
                 start=True, stop=False)
            S.mm(psO[0:64, h * 64:(h + 1) * 64], Z[hf][:, 128 + p * 64:128 + (p + 1) * 64],
                 Shb[:, p, :], start=False, stop=True)
        for p in range(2):
            S.mm(psO[:, 256 + p * 128:256 + (p + 1) * 128], qk[:, 5, p * 128:(p + 1) * 128], vb[:, p * 128:(p + 1) * 128])
        ot = o_r.next()
        S.cp(ot[:], psO[0:64, 0:256], eng='act')
        for h in range(4):
            p, hf = h // 2, h % 2
            sl = slice(hf * 64, (hf + 1) * 64)
            S.stt(Sh[sl, p, :], Sh[sl, p, :], el[sl, 2 * p:2 * p + 1],
                  psO[sl, 256 + p * 128 + hf * 64:256 + p * 128 + (hf + 1) * 64], ALU.mult, ALU.add)
        S.cp(Shb[:], Sh[:], eng='act')
        if not fin:
            S.stv(sd['OF'][r0:r1, :], ot[:])
        elif need_out:
            of, gt = of_r.next(), g_r.next()
            S.ld(of[:], sd['OF'][r0:r1, :])
            S.ld(gt[:], PJ[r0:r1, 1792:2048])
            S.tt(ot[:], ot[:], of[:], ALU.add)
            sq, stt_ = sq_r.next(), st_r.next()
            S.tt(sq[:], ot[:], ot[:], ALU.mult, eng='pool')
            S.reduce(stt_[:], sq[:].rearrange("p (h e) -> p h e", h=4), ALU.add)
            S.rstd(stt_[:], 1.0 / 64)
            S.act(gt[:], gt[:], AF.Silu)
            o3 = ot[:].rearrange("p (h e) -> p h e", h=4)
            S.tt(o3, o3, stt_[:].unsqueeze(2).to_broadcast([64, 4, 64]), ALU.mult)
            S.tt(o3, o3, HNW[:].unsqueeze(1).to_broadcast([64, 4, 64]), ALU.mult, eng='pool')
            S.tt(ot[:], ot[:], gt[:], ALU.mult, eng='pool')
            S.stv(sd['YB'][r0:r1, :], ot[:])

    def mA(nm, c):
        sd = streams[nm]
        r0, r1 = c * 64, (c + 1) * 64
        PJ = sd['PJ']
        xb = [xb_r[j].next() for j in range(3)]
        for j in range(3):
            S.ld(xb[j][:], sd['XB'][r0 + j:r1 + j, :])
        dtr = dtr_r.next()
        S.ld(dtr[:], PJ[r0:r1, 3072 + 4 * dirn:3076 + 4 * dirn])
        cv, tmp, xc = cv_r.next(), tmp_r.next(), xc_r.next()
        S.tt(cv[:], xb[0][:], CW[:, 0, :], ALU.mult, eng='pool')
        S.tt(tmp[:], xb[1][:], CW[:, 1, :], ALU.mult, eng='pool')
        S.tt(cv[:], cv[:], tmp[:], ALU.add, eng='pool')
        S.tt(tmp[:], xb[2][:], CW[:, 2, :], ALU.mult, eng='pool')
        S.tt(cv[:], cv[:], tmp[:], ALU.add, eng='pool')
        S.tt(cv[:], cv[:], CB[:], ALU.add, eng='pool')
        S.act(xc[:], cv[:], AF.Silu)
        dt, la = dt_r.next(), la_r.next()
        S.tt(dt[:], dtr[:], DTB[:], ALU.add)
        S.act(dt[:], dt[:], AF.Exp)
        S.act(dt[:], dt[:], AF.Ln, bias=1.0)
        S.tt(la[:], dt[:], NEGA[:], ALU.mult)
        vbm, kkm = vbm_r.next(), kkm_r.next()
        S.cp(vbm[:], xc[:, 0:256], eng='pool')
        a1, a2 = a1_r.next(), a2_r.next()
        g4 = lambda ap: ap.rearrange("p (g n) -> p g n", g=2).unsqueeze(2).to_broadcast([64, 2, 2, 64])
        h4 = lambda ap: ap.rearrange("p (g h) -> p g h", g=2).unsqueeze(3).to_broadcast([64, 2, 2, 64])
        o4 = lambda ap: ap.rearrange("p (g h n) -> p g h n", g=2, h=2)
        S.tt(o4(kkm[:]), g4(xc[:, 256:384]), h4(dt[:, 0:4]), ALU.mult)
        lab = la[:, 0:4].unsqueeze(2).to_broadcast([64, 4, 64])
        S.tt(a1[:], ONES.unsqueeze(1).to_broadcast([64, 4, 64]), lab, ALU.mult)
        S.tt(a2[:], NTRI.unsqueeze(1).to_broadcast([64, 4, 64]), lab, ALU.mult)
        for h in range(4):
            S.mm(psR[0:64, h * 64:(h + 1) * 64], a1[:, h, :], TRI, start=True, stop=False)
            S.mm(psR[0:64, h * 64:(h + 1) * 64], a2[:, h, :], ONES, start=False, stop=False)
            S.mm(psR[0:64, h * 64:(h + 1) * 64], IDF, NEG, start=False, stop=True)
        S.mm(psR[0:64, 256:260], M2, la[:])
        S.mm(psR[0:64, 260:264], M3, la[:])
        for p in range(2):
            S.mm(psR[:, 264 + 2 * p:266 + 2 * p], a1[:, 2 * p:2 * p + 2, :].rearrange("p a b -> p (a b)"), ONES[:, 0:2])
        dm, ec, elm = dm_r.next(), ec_r.next(), elm_r.next()
        S.act(dm[:], psR[0:64, 0:256], AF.Exp)
        S.act(ec[:], psR[0:64, 256:264], AF.Exp)
        S.act(elm[:], psR[:, 264:268], AF.Exp)
        mk = mk_r.next()
        S.cp(mk[:, 1, :], kkm[:], eng='pool')
        S.cp(o4(mk[:, 0, :]), g4(xc[:, 384:512]), eng='pool')
        S.tt(o4(mk[:, 2, :]), g4(xc[:, 384:512]), h4(ec[:, 0:4]), ALU.mult)
        S.tt(mk[:, 3, :].rearrange("p (h n) -> p h n", h=4), kkm[:].rearrange("p (h n) -> p h n", h=4),
             ec[:, 4:8].unsqueeze(2).to_broadcast([64, 4, 64]), ALU.mult)
        for a in range(3):
            for p in range(2):
                S.tr(psT[:, 640 + (a * 2 + p) * 64:640 + (a * 2 + p + 1) * 64], mk[:, a, p * 128:(p + 1) * 128], idb)
        tTm = tTm_r.next()
        Zm = [Zm_r[0].next(), Zm_r[1].next()]
        S.cp(tTm[:], psT[:, 640:768])
        S.cp(Zm[0][0:64, :], psT[0:64, 768:1024])
        S.cp(Zm[1][64:128, :], psT[64:128, 768:1024])
        for h in range(4):
            p, hf = h // 2, h % 2
            S.mm(psG[0:64, h * 64:(h + 1) * 64], Zm[hf][:, p * 64:(p + 1) * 64], tTm[:, p * 64:(p + 1) * 64])
        scmm = scmm_r.next()
        S.tt(scmm[:], psG[0:64, 0:256], dm[:], ALU.mult)
        return dict(scmm=scmm, vbm=vbm, Zm=Zm, mk=mk, elm=elm, xc=xc)

    def mB(nm, c, t):
        sd = streams[nm]
        need_out = not (l == DEPTH - 1 and nm == 'c')
        r0, r1 = c * 64, (c + 1) * 64
        PJ = sd['PJ']
        scmm, vbm, Zm, mk, elm, xc = t['scmm'], t['vbm'], t['Zm'], t['mk'], t['elm'], t['xc']
        for h in range(4):
            p, hf = h // 2, h % 2
            S.mm(psC[0:64, h * 64:(h + 1) * 64], scmm[:, h * 64:(h + 1) * 64], vbm[:, h * 64:(h + 1) * 64],
                 start=True, stop=False)
            S.mm(psC[0:64, h * 64:(h + 1) * 64], Zm[hf][:, 128 + p * 64:128 + (p + 1) * 64],
                 Smb[:, p, :], start=False, stop=True)
        for p in range(2):
            S.mm(psC[:, 256 + p * 128:256 + (p + 1) * 128], mk[:, 3, p * 128:(p + 1) * 128], vbm[:, p * 128:(p + 1) * 128])
        om = om_r.next()
        S.cp(om[:], psC[0:64, 0:256], eng='act')
        for h in range(4):
            p, hf = h // 2, h % 2
            sl = slice(hf * 64, (hf + 1) * 64)
            S.stt(Sm[sl, p, :], Sm[sl, p, :], elm[sl, 2 * p:2 * p + 1],
                  psC[sl, 256 + p * 128 + hf * 64:256 + p * 128 + (hf + 1) * 64], ALU.mult, ALU.add)
        S.cp(Smb[:], Sm[:], eng='act')
        if not fin:
            S.stv(sd['YF'][r0:r1, :], om[:])
        elif need_out:
            yf, zt = yf_r.next(), z_r.next()
            S.ld(yf[:], sd['YF'][r0:r1, :])
            S.ld(zt[:], PJ[r0:r1, 2304:2560])
            S.tt(om[:], om[:], yf[:], ALU.add)
            sq, stt_ = sq2_r.next(), st2_r.next()
            x3 = xc[:, 0:256].rearrange("p (h e) -> p h e", h=4)
            sq3 = sq[:].rearrange("p (h e) -> p h e", h=4)
            S.tt(sq3, x3, DV[:].unsqueeze(2).to_broadcast([64, 4, 64]), ALU.mult, eng='pool')
            S.tt(om[:], om[:], sq[:], ALU.add)
            S.act(zt[:], zt[:], AF.Silu)
            S.tt(om[:], om[:], zt[:], ALU.mult)
            S.tt(sq[:], om[:], om[:], ALU.mult, eng='pool')
            S.reduce(stt_[:, 0:2], sq[:].rearrange("p (g e) -> p g e", g=2), ALU.add)
            S.rstd(stt_[:, 0:2], 1.0 / 128)
            o3 = om[:].rearrange("p (g e) -> p g e", g=2)
            S.tt(o3, o3, stt_[:, 0:2].unsqueeze(2).to_broadcast([64, 2, 128]), ALU.mult)
            S.tt(om[:], om[:], SNW[:], ALU.mult, eng='pool')
            S.stv(sd['YD'][r0:r1, :], om[:])

    wjobs = []
    if dirn == 0 and SPARSE_MOE:
        wr1 = S.ring(3, [128, 8, DFF], BF16)
        wr2 = S.ring(2, [128, 4, D], BF16)
        for e in range(NEXP):
            wjobs.append(('moe_w1', 'W1S', e, wr1))
            wjobs.append(('moe_w3', 'W3S', e, wr1))
            wjobs.append(('moe_w2', 'W2S', e, wr2))
    nsteps = len(order) + 1
    wper = -(-len(wjobs) // max(1, nsteps - 1)) if wjobs else 0

    prev = None
    for step in range(len(order) + 1):
        groups = []
        cur = None
        if wjobs:
            S.begin('W')
            for _ in range(wper):
                if not wjobs:
                    break
                src, dst, e, rr = wjobs.pop(0)
                t = rr.next()
                S.ld(t[:], A[src][l, e].rearrange("(k p) n -> p k n", p=128), q='pool')
                S.stv(A[dst][e * 128:(e + 1) * 128, :], t[:].rearrange("p k n -> p (k n)"), q='sp')
            groups.append('W')
        if step < len(order):
            nm, c = order[step]
            S.begin('Ah')
            th = hA(nm, c)
            S.begin('Am')
            tm = mA(nm, c)
            cur = (nm, c, th, tm)
            groups += ['Ah', 'Am']
        if prev is not None:
            nm, c, th, tm = prev
            S.begin('Bh')
            hB(nm, c, th)
            S.begin('Bm')
            mB(nm, c, tm)
            groups += ['Bh', 'Bm']
        S.flush(groups)
        prev = cur
    S.close()


def stage_merge(nc, A, l, sd):
    nm = sd['nm']
    Ts = sd['T']
    S = St(nc, 'mg%d%s' % (l, nm))
    ident = S.sb([128, 128], BF16)
    S.ld(ident[:], A['identb'])
    wbr = S.sb([128, 8, D], BF16)
    S.ld(wbr[:], A['w_branch'][l].rearrange("k (j p) n -> p (k j) n", p=128), q='pool')
    wo = S.sb([128, 8, D], BF16)
    S.ld(wo[:], A['w_out'][l].rearrange("(k p) n -> p k n", p=128), q='pool')
    G1 = load_mod(S, A, l, sd['row'], 2)
    CAW = S.sb([128, 3, 256])
    for j in range(3):
        S.ld(CAW[:, j, :], A['conv_a_w'][l, j].partition_broadcast(128))
    CAB = S.sb([128, 256])
    S.ld(CAB[:], A['conv_a_b'][l].partition_broadcast(128))
    ab_r = S.ring(2, [128, 256])
    u_r = [S.ring(2, [128, 256]) for _ in range(3)]
    yin_r = S.ring(2, [128, 3, 256])
    gt_r = S.ring(2, [128, 4096], BF16)
    x_r = S.ring(2, [128, D])
    cv_r = S.ring(2, [128, 256])
    tmp_r = S.ring(2, [128, 256])
    br_r = S.ring(2, [128, 4, 256], BF16)
    brT_r = S.ring(2, [128, D], BF16)
    mg_r = S.ring(2, [128, D])
    mt_r = S.ring(2, [128, 512])
    mgb_r = S.ring(2, [128, D], BF16)
    mT_r = S.ring(2, [128, D], BF16)
    xo_r = S.ring(2, [128, D])
    pT = S.ps([128, D], BF16)
    pT2 = S.ps([128, D], BF16)
    ppr = S.psring(4)
    def mg1(i, _c):
        r0, r1 = i * 128, (i + 1) * 128
        ab = ab_r.next()
        S.ld(ab[:], sd['PJ'][r0:r1, 0:256])
        us = [u_r[j].next() for j in range(3)]
        for j in range(3):
            S.ld(us[j][:], sd['U'][r0 + j:r1 + j, :])
        yin = yin_r.next()
        S.ld(yin[:, 0, :], sd['YB'][r0:r1, :])
        S.ld(yin[:, 1, :], sd['YC'][r0:r1, :])
        S.ld(yin[:, 2, :], sd['YD'][r0:r1, :])
        gt = gt_r.next()
        S.ld(gt[:], sd['GT'][r0:r1, :])
        xt = x_r.next()
        S.ld(xt[:], sd['x'][r0:r1, :])
        cv, tmp = cv_r.next(), tmp_r.next()
        S.tt(cv[:], us[0][:], CAW[:, 0, :], ALU.mult, eng='pool')
        S.tt(tmp[:], us[1][:], CAW[:, 1, :], ALU.mult, eng='pool')
        S.tt(cv[:], cv[:], tmp[:], ALU.add, eng='pool')
        S.tt(tmp[:], us[2][:], CAW[:, 2, :], ALU.mult, eng='pool')
        S.tt(cv[:], cv[:], tmp[:], ALU.add, eng='pool')
        S.tt(cv[:], cv[:], CAB[:], ALU.add, eng='pool')
        br = br_r.next()
        S.tt(br[:, 0, :], cv[:], ab[:], ALU.mult, eng='pool')
        S.cp(br[:, 1:4, :], yin[:], eng='pool')
        for k in range(8):
            S.tr(pT[:, k * 128:(k + 1) * 128], br[:, k // 2, (k % 2) * 128:(k % 2 + 1) * 128], ident[:])
        brT = brT_r.next()
        S.cp(brT[:], pT[:], eng='act')
        return (brT, gt, xt)

    def mg2(i, c_):
        brT, gt, xt = c_
        r0, r1 = i * 128, (i + 1) * 128
        mg = mg_r.next()
        for kb in range(4):
            for hf in range(2):
                pp = ppr.next()
                for j in range(2):
                    S.mm(pp[:], brT[:, (2 * kb + j) * 128:(2 * kb + j + 1) * 128], wbr[:, 2 * kb + j, hf * 512:(hf + 1) * 512],
                         start=(j == 0), stop=(j == 1))
                gsl = gt[:, kb * D + hf * 512:kb * D + (hf + 1) * 512]
                if kb == 0:
                    S.tt(mg[:, hf * 512:(hf + 1) * 512], pp[:], gsl, ALU.mult)
                else:
                    mt = mt_r.next()
                    S.tt(mt[:], pp[:], gsl, ALU.mult)
                    S.tt(mg[:, hf * 512:(hf + 1) * 512], mg[:, hf * 512:(hf + 1) * 512], mt[:], ALU.add, eng='pool')
        mgb = mgb_r.next()
        S.cp(mgb[:], mg[:], eng='act')
        for k in range(8):
            S.tr(pT2[:, k * 128:(k + 1) * 128], mgb[:, k * 128:(k + 1) * 128], ident[:])
        mT = mT_r.next()
        S.cp(mT[:], pT2[:], eng='act')
        xo = xo_r.next()
        for hf in range(2):
            pp = ppr.next()
            for k in range(8):
                S.mm(pp[:], mT[:, k * 128:(k + 1) * 128], wo[:, k, hf * 512:(hf + 1) * 512], start=(k == 0), stop=(k == 7))
            mt = mt_r.next()
            S.tt(mt[:], pp[:], G1[:, hf * 512:(hf + 1) * 512], ALU.mult)
            S.tt(xo[:, hf * 512:(hf + 1) * 512], mt[:], xt[:, hf * 512:(hf + 1) * 512], ALU.add, eng='pool')
        S.stv(sd['X1'][r0:r1, :], xo[:])
    pipelined(S, list(range(Ts // 128)), [mg1, mg2])
    S.close()


def stage_moe(nc, A, l, streams, last):
    S = St(nc, 'moe%d' % l)
    identf = S.sb([128, 128])
    S.ld(identf[:], A['identf'])
    wffn = S.sb([128, D])
    S.ld(wffn[:], A['norm_ffn_w'][l].partition_broadcast(128))
    rw = S.sb([128, 8, 36])
    S.ld(rw[:], A['router_w'][l].rearrange("(k p) n -> p k n", p=128))
    rb = S.sb([128, 36])
    S.ld(rb[:], A['router_b'][l].partition_broadcast(128))
    if last:
        fnw = S.sb([128, D])
        S.ld(fnw[:], A['final_norm_w'].partition_broadcast(128))
    mods = {}
    names = ('l',) if last else ('l', 'c')
    for nm in names:
        row = streams[nm]['row']
        mods[nm] = (load_mod(S, A, l, row, 4, wffn), load_mod(S, A, l, row, 3), load_mod(S, A, l, row, 5))
    tiles = []
    for nm in names:
        sd = streams[nm]
        for i in range(sd['T'] // 128):
            tiles.append((nm, i))
    nblk = max(1, -(-len(tiles) // 8))
    per = -(-len(tiles) // nblk)
    blocks = [tiles[i * per:(i + 1) * per] for i in range(nblk)]
    MAXT = per
    fTt = S.sb([128, 8, MAXT * 128], BF16)
    acc = [S.sb([128, D]) for _ in range(MAXT)]
    wg = [S.sb([128, 32]) for _ in range(MAXT)]
    x_r = S.ring(2, [128, D])
    junk = S.sb([128, D])
    ss_r = S.ring(2, [128, 1])
    fn_r = S.ring(1, [128, D])
    fTf_r = S.ring(1, [128, D])
    pTa = S.ps()
    pTb = S.ps()
    pL = S.ps()
    sm_r = S.ring(2, [128, 64])
    w1_r = S.ring(2, [128, 8, DFF], BF16)
    w3_r = S.ring(2, [128, 8, DFF], BF16)
    w2_r = S.ring(2, [128, 4, D], BF16)
    gact_r = S.ring(2, [128, 4, 512], BF16)
    sil_r = S.ring(2, [128, 512])
    ph_r = S.psring(2)
    ph3_r = S.psring(2)
    po_r = S.psring(1)
    xo_r = S.ring(2, [128, D])
    for blk in blocks:
        nt = len(blk)
        for ti, (nm, i) in enumerate(blk):
            sd = streams[nm]
            A2, B2, G2 = mods[nm]
            r0, r1 = i * 128, (i + 1) * 128
            xt, ss, fn = x_r.next(), ss_r.next(), fn_r.next()
            S.ld(xt[:], sd['X1'][r0:r1, :])
            S.act(junk[:], xt[:], AF.Square, accum=ss[:])
            S.rstd(ss[:], 1.0 / D)
            S.stt(fn[:], xt[:], ss[:], A2[:], ALU.mult, ALU.mult)
            S.tt(fn[:], fn[:], B2[:], ALU.add)
            for k in range(8):
                pt = pTa if k < 4 else pTb
                S.tr(pt[:, (k % 4) * 128:(k % 4 + 1) * 128], fn[:, k * 128:(k + 1) * 128], identf[:])
            fTf = fTf_r.next()
            S.cp(fTf[:, 0:512], pTa[:], eng='act')
            S.cp(fTf[:, 512:1024], pTb[:], eng='dve')
            S.cp(fTt[:, :, ti * 128:(ti + 1) * 128], fTf[:].rearrange("p (k t) -> p k t", k=8), eng='pool')
            for k in range(8):
                S.mm(pL[:, 0:36], fTf[:, k * 128:(k + 1) * 128], rw[:, k, :], start=(k == 0), stop=(k == 7))
            sm = sm_r.next()
            lg = sm[:, 0:36]
            S.tt(lg, pL[:, 0:36], rb[:], ALU.add)
            gmax, gsel, gex, gsum = sm[:, 36:37], sm[:, 37:41], sm[:, 41:45], sm[:, 45:46]
            S.reduce(gmax, lg[:, 0:4], ALU.max)
            S.ts(gsel, lg[:, 0:4], gmax, None, ALU.is_equal)
            S.ts(gex, lg[:, 0:4], gmax, None, ALU.subtract)
            S.act(gex, gex, AF.Exp)
            S.reduce(gsum, gex, ALU.add)
            S.recip(gsum, gsum)
            ein, m1, sel1, e2, m2, sel2 = sm[:, 46:54], sm[:, 54:55], sm[:, 56:64], sm[:, 46:54], sm[:, 55:56], sm[:, 46:54]
            S.ts(ein, lg[:, 4:12], gsel[:, 0:1], None, ALU.mult)
            for g in range(1, 4):
                S.stt(ein, lg[:, 4 + 8 * g:12 + 8 * g], gsel[:, g:g + 1], ein, ALU.mult, ALU.add)
            S.reduce(m1, ein, ALU.max)
            S.ts(sel1, ein, m1, None, ALU.is_equal)
            S.stt(e2, sel1, -1e30, ein, ALU.mult, ALU.add)
            S.reduce(m2, e2, ALU.max)
            S.ts(sel2, e2, m2, None, ALU.is_equal)
            wa, wb_ = sm[:, 36:37], sm[:, 41:42]
            S.tt(wa, m2, m1, ALU.subtract)
            S.act(wa, wa, AF.Exp)
            S.ts(wa, wa, 1.0, None, ALU.add)
            S.recip(wa, wa)
            S.ts(wb_, wa, -1.0, 1.0, ALU.mult, ALU.add)
            S.tt(wa, wa, gsum, ALU.mult)
            S.tt(wb_, wb_, gsum, ALU.mult)
            S.ts(sel1, sel1, wa, None, ALU.mult)
            S.stt(sel1, sel2, wb_, sel1, ALU.mult, ALU.add)
            for g in range(4):
                S.ts(wg[ti][:, 8 * g:8 * g + 8], sel1, gsel[:, g:g + 1], None, ALU.mult)
            S.memset(acc[ti][:], 0.0, eng='pool')
        sbs = [list(range(s, min(s + 4, nt))) for s in range(0, nt, 4)]
        for e in range(NEXP):
            w1, w3, w2 = w1_r.next(), w3_r.next(), w2_r.next()
            S.ld(w1[:], A['moe_w1'][l, e].rearrange("(k p) n -> p k n", p=128), q='pool')
            S.ld(w3[:], A['moe_w3'][l, e].rearrange("(k p) n -> p k n", p=128), q='pool')
            S.ld(w2[:], A['moe_w2'][l, e].rearrange("(k p) n -> p k n", p=128), q='pool')
            for si, sb_ in enumerate(sbs):
                n = len(sb_) * 128
                c0 = sb_[0] * 128
                ga = gact_r.next()
                for fc in range(4):
                    ph, ph3 = ph_r.next(), ph3_r.next()
                    for k in range(8):
                        S.mm(ph[:, 0:n], w1[:, k, fc * 128:(fc + 1) * 128], fTt[:, k, c0:c0 + n],
                             start=(k == 0), stop=(k == 7))
                    for k in range(8):
                        S.mm(ph3[:, 0:n], w3[:, k, fc * 128:(fc + 1) * 128], fTt[:, k, c0:c0 + n],
                             start=(k == 0), stop=(k == 7))
                    sil = sil_r.next()
                    S.act(sil[:, 0:n], ph[:, 0:n], AF.Silu)
                    S.tt(ga[:, fc, 0:n], ph3[:, 0:n], sil[:, 0:n], ALU.mult)
                for tj, ti in enumerate(sb_):
                    for hf in range(2):
                        po = po_r.next()
                        for fc in range(4):
                            S.mm(po[:], ga[:, fc, tj * 128:(tj + 1) * 128], w2[:, fc, hf * 512:(hf + 1) * 512],
                                 start=(fc == 0), stop=(fc == 3))
                        S.stt(acc[ti][:, hf * 512:(hf + 1) * 512], po[:], wg[ti][:, e:e + 1], acc[ti][:, hf * 512:(hf + 1) * 512],
                              ALU.mult, ALU.add)
        for ti, (nm, i) in enumerate(blk):
            sd = streams[nm]
            A2, B2, G2 = mods[nm]
            r0, r1 = i * 128, (i + 1) * 128
            xt, xo = x_r.next(), xo_r.next()
            S.ld(xt[:], sd['X1'][r0:r1, :])
            S.tt(acc[ti][:], acc[ti][:], G2[:], ALU.mult, eng='pool')
            S.tt(xo[:], acc[ti][:], xt[:], ALU.add, eng='pool')
            if last:
                ss = ss_r.next()
                S.act(junk[:], xo[:], AF.Square, accum=ss[:])
                S.rstd(ss[:], 1.0 / D)
                S.stt(xo[:], xo[:], ss[:], fnw[:], ALU.mult, ALU.mult)
                S.stv(A['out'][r0:r1, :], xo[:])
            else:
                S.stv(sd['X2'][r0:r1, :], xo[:])
    S.close()


def moe_tiles(streams, last):
    names = ('l',) if last else ('l', 'c')
    tiles = []
    for nm in names:
        for i in range(streams[nm]['T'] // 128):
            tiles.append((nm, i))
    return names, tiles


def stage_moe_prep(nc, A, l):
    S = St(nc, 'mp%d' % l)
    r1 = S.ring(3, [128, 8, DFF], BF16)
    r2 = S.ring(2, [128, 4, D], BF16)
    for e in range(NEXP):
        for src, dst in (('moe_w1', 'W1S'), ('moe_w3', 'W3S')):
            t = r1.next()
            S.ld(t[:], A[src][l, e].rearrange("(k p) n -> p k n", p=128), q='pool')
            S.stv(A[dst][e * 128:(e + 1) * 128, :], t[:].rearrange("p k n -> p (k n)"), q='sp')
        t = r2.next()
        S.ld(t[:], A['moe_w2'][l, e].rearrange("(k p) n -> p k n", p=128), q='pool')
        S.stv(A['W2S'][e * 128:(e + 1) * 128, :], t[:].rearrange("p k n -> p (k n)"), q='sp')
    S.close()


def stage_moe_route(nc, A, l, streams, last, pers, NBM):
    S = St(nc, 'mr%d' % l)
    names, tiles = moe_tiles(streams, last)
    NB = 2 * len(tiles) + NEXP
    identf = S.sb([128, 128])
    S.ld(identf[:], A['identf'])
    moec = S.sb([128, 321 + NBM])
    S.ld(moec[:], A['moec'])
    ONES = moec[:, 0:128]
    TRIX = moec[0:32, 257 + NBM:289 + NBM]
    TRII = moec[0:32, 289 + NBM:321 + NBM]
    wffn = S.sb([128, D])
    S.ld(wffn[:], A['norm_ffn_w'][l].partition_broadcast(128))
    rw = S.sb([128, 8, 36])
    S.ld(rw[:], A['router_w'][l].rearrange("(k p) n -> p k n", p=128))
    rb = S.sb([128, 36])
    S.ld(rb[:], A['router_b'][l].partition_broadcast(128))
    mods = {}
    for nm in names:
        row = streams[nm]['row']
        mods[nm] = (load_mod(S, A, l, row, 4, wffn), load_mod(S, A, l, row, 3))
    OHT, WTT, PADB = pers['OHT'], pers['WTT'], pers['PADB']
    zt = S.sb([128, D], BF16)
    S.memset(zt[:], 0.0)
    for j in range(NB):
        S.stv(A['FB'][j * 128:(j + 1) * 128, :], zt[:], q='sp')
    x_r = S.ring(2, [128, D])
    junk = S.sb([128, D])
    ss_r = S.ring(2, [128, 1])
    fn_r = S.ring(2, [128, D])
    fb_r = S.ring(2, [128, D], BF16)
    fTf_r = S.ring(2, [128, D])
    pTa, pTb, pL = S.ps(), S.ps(), S.ps()
    pCnt = S.ps()
    sm_r = S.ring(2, [128, 80])
    ohs_r = S.ring(2, [128, 32])
    def rt1(it, _c):
        ti, (nm, i) = it
        sd = streams[nm]
        A2, B2 = mods[nm]
        r0, r1 = i * 128, (i + 1) * 128
        xt, ss, fn, fb = x_r.next(), ss_r.next(), fn_r.next(), fb_r.next()
        S.ld(xt[:], sd['X1'][r0:r1, :])
        S.act(junk[:], xt[:], AF.Square, accum=ss[:])
        S.rstd(ss[:], 1.0 / D)
        S.stt(fn[:], xt[:], ss[:], A2[:], ALU.mult, ALU.mult)
        S.tt(fn[:], fn[:], B2[:], ALU.add)
        S.cp(fb[:], fn[:], eng='pool')
        S.stv(A['FN'][ti * 128:(ti + 1) * 128, :], fb[:], q='sp')
        for k in range(8):
            pt = pTa if k < 4 else pTb
            S.tr(pt[:, (k % 4) * 128:(k % 4 + 1) * 128], fn[:, k * 128:(k + 1) * 128], identf[:])
        fTf = fTf_r.next()
        S.cp(fTf[:, 0:512], pTa[:], eng='act')
        S.cp(fTf[:, 512:1024], pTb[:], eng='dve')
        return fTf

    def rt2(it, fTf):
        ti, (nm, i) = it
        for k in range(8):
            S.mm(pL[:, 0:36], fTf[:, k * 128:(k + 1) * 128], rw[:, k, :], start=(k == 0), stop=(k == 7))
        sm = sm_r.next()
        lg = sm[:, 0:36]
        S.tt(lg, pL[:, 0:36], rb[:], ALU.add)
        gmax, gsel, gex, gsum = sm[:, 36:37], sm[:, 37:41], sm[:, 41:45], sm[:, 45:46]
        S.reduce(gmax, lg[:, 0:4], ALU.max)
        S.ts(gsel, lg[:, 0:4], gmax, None, ALU.is_equal)
        S.ts(gex, lg[:, 0:4], gmax, None, ALU.subtract)
        S.act(gex, gex, AF.Exp)
        S.reduce(gsum, gex, ALU.add)
        S.recip(gsum, gsum)
        ein, m1, m2 = sm[:, 46:54], sm[:, 54:55], sm[:, 55:56]
        sel1, e2, sel2 = sm[:, 56:64], sm[:, 64:72], sm[:, 72:80]
        S.ts(ein, lg[:, 4:12], gsel[:, 0:1], None, ALU.mult)
        for g in range(1, 4):
            S.stt(ein, lg[:, 4 + 8 * g:12 + 8 * g], gsel[:, g:g + 1], ein, ALU.mult, ALU.add)
        S.reduce(m1, ein, ALU.max)
        S.ts(sel1, ein, m1, None, ALU.is_equal)
        S.stt(e2, sel1, -1e30, ein, ALU.mult, ALU.add)
        S.reduce(m2, e2, ALU.max)
        S.ts(sel2, e2, m2, None, ALU.is_equal)
        wa, wb_ = sm[:, 36:37], sm[:, 41:42]
        S.tt(wa, m2, m1, ALU.subtract)
        S.act(wa, wa, AF.Exp)
        S.ts(wa, wa, 1.0, None, ALU.add)
        S.recip(wa, wa)
        S.ts(wb_, wa, -1.0, 1.0, ALU.mult, ALU.add)
        S.tt(WTT[:, ti, 0:1], wa, gsum, ALU.mult)
        S.tt(WTT[:, ti, 1:2], wb_, gsum, ALU.mult)
        for g in range(4):
            S.ts(OHT[:, ti, 8 * g:8 * g + 8], sel1, gsel[:, g:g + 1], None, ALU.mult)
            S.ts(OHT[:, ti, 32 + 8 * g:40 + 8 * g], sel2, gsel[:, g:g + 1], None, ALU.mult)
        ohs = ohs_r.next()
        S.tt(ohs[:], OHT[:, ti, 0:32], OHT[:, ti, 32:64], ALU.add)
        S.mm(pCnt[0:32, 0:2], ohs[:], ONES[:, 0:2], start=(ti == 0), stop=(ti == len(tiles) - 1))
    pipelined(S, list(enumerate(tiles)), [rt1, rt2])
    I32 = mybir.dt.int32
    cf = S.sb([32, 4])
    ci = S.sb([32, 2], I32)
    S.ts(cf[:, 0:1], pCnt[0:32, 0:1], 127.0, None, ALU.add)
    S.cp(ci[:, 0:1], cf[:, 0:1])
    S.op('dve', lambda e: e.tensor_scalar(out=ci[:, 1:2], in0=ci[:, 0:1], scalar1=7, scalar2=7,
                                          op0=ALU.arith_shift_right, op1=ALU.logical_shift_left), [ci[:, 0:1]], [ci[:, 1:2]])
    S.cp(cf[:, 1:2], ci[:, 1:2])
    pbc = S.sb([32, 128])
    S.ts(pbc[:], ONES[0:32, :], cf[:, 1:2], None, ALU.mult)
    pP = S.ps()
    S.mm(pP[:, 0:32], pbc[:], TRIX)
    S.mm(pP[:, 32:64], pbc[:], TRII)
    S.cp(PADB[:], pP[:, 0:64])
    S.close()


def stage_moe_disp(nc, A, l, streams, last, pers, NBM):
    S = St(nc, 'md%d' % l)
    names, tiles = moe_tiles(streams, last)
    NB = 2 * len(tiles) + NEXP
    I32 = mybir.dt.int32
    moec = S.sb([128, 321 + NBM])
    S.ld(moec[:], A['moec'])
    ONES = moec[:, 0:128]
    STRICT = moec[:, 128:256]
    JROW = moec[:, 256:256 + NBM]
    PIDX = moec[:, 256 + NBM:257 + NBM]
    OHT, DEST, IDXW, PADB = pers['OHT'], pers['DEST'], pers['IDXW'], pers['PADB']
    accp = S.sb([128, 32])
    S.memset(accp[:], 0.0)
    ohs_r = S.ring(2, [128, 32])
    tmp_r = S.ring(2, [128, 32])
    t2_r = S.ring(2, [128, 32])
    df_r = S.ring(2, [128, 2])
    fb_r = S.ring(3, [128, D], BF16)
    pr = S.psring(2)
    for ti in range(len(tiles)):
        ohs, tmp, t2, df, fb = ohs_r.next(), tmp_r.next(), t2_r.next(), df_r.next(), fb_r.next()
        pp = pr.next()
        S.tt(ohs[:], OHT[:, ti, 0:32], OHT[:, ti, 32:64], ALU.add)
        S.mm(pp[:, 0:32], ONES, accp[:], start=True, stop=False)
        S.mm(pp[:, 0:32], STRICT, ohs[:], start=False, stop=True)
        S.tt(tmp[:], pp[:, 0:32], PADB[:, 0:32], ALU.add)
        S.tt(accp[:], accp[:], ohs[:], ALU.add, eng='pool')
        for k in range(2):
            S.tt(t2[:], tmp[:], OHT[:, ti, 32 * k:32 * k + 32], ALU.mult)
            S.reduce(df[:, k:k + 1], t2[:], ALU.add)
        S.cp(DEST[:, ti, :], df[:])
        S.ld(fb[:], A['FN'][ti * 128:(ti + 1) * 128, :])
        for k in range(2):
            S.op('pool', lambda e, ti=ti, k=k, fb=fb: e.indirect_dma_start(
                out=A['FB'], out_offset=bass.IndirectOffsetOnAxis(ap=DEST[:, ti, k:k + 1], axis=0),
                in_=fb[:], in_offset=None), [fb[:], DEST[:]], [], dma=True)
    be = S.sb([128, NBM])
    S.memset(be[:], 0.0)
    for e in range(NEXP):
        S.stt(be[:], JROW, PADB[:, 32 + e:33 + e], be[:], ALU.is_ge, ALU.add)
    S.ts(be[:], be[:], 31.0, 128.0, ALU.min, ALU.mult)
    S.ts(be[:], be[:], PIDX, None, ALU.add)
    S.cp(IDXW[:], be[:])
    S.close()


def stage_moe_exp(nc, A, l, streams, last, pers, NBM):
    S = St(nc, 'me%d' % l)
    names, tiles = moe_tiles(streams, last)
    NB = 2 * len(tiles) + NEXP
    IDXW = pers['IDXW']
    ident = S.sb([128, 128], BF16)
    S.ld(ident[:], A['identb'])
    w1_r = S.ring(3, [128, 8 * DFF], BF16)
    w3_r = S.ring(3, [128, 8 * DFF], BF16)
    w2_r = S.ring(3, [128, 4 * D], BF16)
    xb_r = S.ring(3, [128, D], BF16)
    xT_r = S.ring(2, [128, D], BF16)
    sil_r = S.ring(2, [128, DFF])
    g_r = S.ring(2, [128, DFF], BF16)
    gT_r = S.ring(2, [128, DFF], BF16)
    yb_r = S.ring(2, [128, D])
    pT = S.ps([128, D], BF16)
    pT2 = S.ps([128, D], BF16)
    ph_r, ph3_r, po_r = S.psring(2), S.psring(2), S.psring(2)
    def ex1(j, _c):
        w1, w3, w2 = w1_r.next(), w3_r.next(), w2_r.next()
        for wt, nm in ((w1, 'W1S'), (w3, 'W3S'), (w2, 'W2S')):
            S.op('pool', lambda e, wt=wt, nm=nm, j=j: e.indirect_dma_start(
                out=wt[:], out_offset=None, in_=A[nm],
                in_offset=bass.IndirectOffsetOnAxis(ap=IDXW[:, j:j + 1], axis=0)), [IDXW[:]], [wt[:]], dma=True)
        xb = xb_r.next()
        S.ld(xb[:], A['FB'][j * 128:(j + 1) * 128, :])
        for k in range(8):
            S.tr(pT[:, k * 128:(k + 1) * 128], xb[:, k * 128:(k + 1) * 128], ident[:])
        xT = xT_r.next()
        S.cp(xT[:], pT[:])
        ph, ph3 = ph_r.next(), ph3_r.next()
        for k in range(8):
            S.mm(ph[:], xT[:, k * 128:(k + 1) * 128], w1[:, k * DFF:(k + 1) * DFF], start=(k == 0), stop=(k == 7))
        for k in range(8):
            S.mm(ph3[:], xT[:, k * 128:(k + 1) * 128], w3[:, k * DFF:(k + 1) * DFF], start=(k == 0), stop=(k == 7))
        return (ph, ph3, w2)

    def ex2(j, c_):
        ph, ph3, w2 = c_
        sil, g = sil_r.next(), g_r.next()
        S.act(sil[:], ph[:], AF.Silu)
        S.tt(g[:], ph3[:], sil[:], ALU.mult)
        for fc in range(4):
            S.tr(pT2[:, fc * 128:(fc + 1) * 128], g[:, fc * 128:(fc + 1) * 128], ident[:])
        gT = gT_r.next()
        S.cp(gT[:], pT2[:, 0:512])
        yb = yb_r.next()
        for hf in range(2):
            po = po_r.next()
            for fc in range(4):
                S.mm(po[:], gT[:, fc * 128:(fc + 1) * 128], w2[:, fc * D + hf * 512:fc * D + (hf + 1) * 512],
                     start=(fc == 0), stop=(fc == 3))
            S.cp(yb[:, hf * 512:(hf + 1) * 512], po[:], eng=('act' if hf == 0 else 'dve'))
        S.stv(A['YBUF'][j * 128:(j + 1) * 128, :], yb[:], q='sp')
    pipelined(S, list(range(NB)), [ex1, ex2])
    S.close()


def stage_moe_comb(nc, A, l, streams, last, pers):
    S = St(nc, 'mc%d' % l)
    names, tiles = moe_tiles(streams, last)
    DEST, WTT = pers['DEST'], pers['WTT']
    G2 = {nm: load_mod(S, A, l, streams[nm]['row'], 5) for nm in names}
    if last:
        fnw = S.sb([128, D])
        S.ld(fnw[:], A['final_norm_w'].partition_broadcast(128))
    x_r = S.ring(2, [128, D])
    y0_r, y1_r = S.ring(2, [128, D]), S.ring(2, [128, D])
    xo_r = S.ring(2, [128, D])
    junk = S.sb([128, D])
    ss_r = S.ring(2, [128, 1])
    def cb1(it, _c):
        ti, (nm, i) = it
        sd = streams[nm]
        r0, r1 = i * 128, (i + 1) * 128
        xt, y0, y1 = x_r.next(), y0_r.next(), y1_r.next()
        S.ld(xt[:], sd['X1'][r0:r1, :])
        for k, yt in ((0, y0), (1, y1)):
            S.op('pool', lambda e, ti=ti, k=k, yt=yt: e.indirect_dma_start(
                out=yt[:], out_offset=None, in_=A['YBUF'],
                in_offset=bass.IndirectOffsetOnAxis(ap=DEST[:, ti, k:k + 1], axis=0)), [DEST[:]], [yt[:]], dma=True)
        return (xt, y0, y1)

    def cb2(it, c_):
        ti, (nm, i) = it
        xt, y0, y1 = c_
        sd = streams[nm]
        r0, r1 = i * 128, (i + 1) * 128
        xo = xo_r.next()
        S.ts(y0[:], y0[:], WTT[:, ti, 0:1], None, ALU.mult)
        S.stt(y0[:], y1[:], WTT[:, ti, 1:2], y0[:], ALU.mult, ALU.add)
        S.tt(y0[:], y0[:], G2[nm][:], ALU.mult, eng='pool')
        S.tt(xo[:], y0[:], xt[:], ALU.add, eng='pool')
        if last:
            ss = ss_r.next()
            S.act(junk[:], xo[:], AF.Square, accum=ss[:])
            S.rstd(ss[:], 1.0 / D)
            S.stt(xo[:], xo[:], ss[:], fnw[:], ALU.mult, ALU.mult)
            S.stv(A['out'][r0:r1, :], xo[:], q='sp')
        else:
            S.stv(sd['X2'][r0:r1, :], xo[:], q='sp')
    pipelined(S, list(enumerate(tiles)), [cb1, cb2])
    S.close()


def prep_inputs(inputs, b, consts):
    f = lambda a: np.ascontiguousarray(np.asarray(a, dtype=np.float32))
    m = {}
    m['x'] = f(inputs['x'][b])
    m['ctx'] = f(inputs['ctx'][b])
    cc = np.stack([f(inputs['c'][b]).reshape(8, 128).T, f(inputs['c_ctx']).reshape(8, 128).T], axis=-1)
    m['cc'] = np.ascontiguousarray(cc.reshape(128, 16))
    for k in ('ada_w', 'ada_b', 'norm_mix_w', 'norm_ffn_w', 'w_in', 'conv_a_w', 'conv_a_b', 'hgrn_lb', 'hgrn_norm_w',
              'ssm_conv_w', 'ssm_conv_b', 'ssm_A_log', 'ssm_dt_bias', 'ssm_D', 'ssm_norm_w', 'w_branch', 'w_out',
              'moe_w1', 'moe_w3', 'moe_w2', 'final_norm_w'):
        m[k] = f(inputs[k])
    m['router_w'] = np.ascontiguousarray(np.concatenate([f(inputs['router_group_w']), f(inputs['router_expert_w'])], axis=-1))
    m['router_b'] = np.ascontiguousarray(np.concatenate([f(inputs['router_group_b']), f(inputs['router_expert_b'])], axis=-1))
    m.update(consts)
    return m


_CACHE = {}


def kernel(**inputs):
    x = np.asarray(inputs['x'])
    B, T, _ = x.shape
    if T not in _CACHE:
        _CACHE[T] = build_program(T)
    nc, consts = _CACHE[T]
    ncores = 8
    in_maps = [prep_inputs(inputs, c % B, consts) for c in range(ncores)]
    res = run_bass_kernel_spmd(nc, in_maps, core_ids=list(range(ncores)))
    out = np.stack([np.asarray(res.results[b]['out']) for b in range(B)], axis=0)
    return out.astype(np.float32)
```
